# Optimizing a Trainium2 kernel written in Bass

```python
import jax
import jax.numpy as jnp
from jax import lax
import numpy as np

D_MODEL = 1024
BATCH = 8
SEQ = 4096
DEPTH = 4

CTX_LEN = 256
GRID_W = 64
N_BRANCH = 3
BRANCH_W = D_MODEL // 2
HEAD_DIM = 64
ML_HEADS = 4
ML_HEAD_DIM = BRANCH_W // ML_HEADS
ML_CHUNK = 64
GQA_HEADS = BRANCH_W // HEAD_DIM
GQA_KV_HEADS = 2
GQA_GROUP = GQA_HEADS // GQA_KV_HEADS
NA_HEADS = BRANCH_W // HEAD_DIM
NA_WIN_R = 8
NA_WIN_C = 16
Q_BLOCK = 128
ROPE_THETA = 10000.0
FFN_DIM = ((8 * D_MODEL // 3 + 127) // 128) * 128
CONV_K = 3
EPS = 1e-6
IN_SIZES = (BRANCH_W, BRANCH_W, BRANCH_W, BRANCH_W, 4 * ML_HEADS,
            GQA_HEADS * HEAD_DIM, GQA_KV_HEADS * HEAD_DIM, GQA_KV_HEADS * HEAD_DIM,
            NA_HEADS * HEAD_DIM, NA_HEADS * HEAD_DIM, NA_HEADS * HEAD_DIM,
            N_BRANCH * D_MODEL)
IN_SPLITS = tuple(sum(IN_SIZES[:i + 1]) for i in range(len(IN_SIZES) - 1))
D_IN = sum(IN_SIZES)

kernel_name = 'hybrid_mlstm_gqa_natten_dit_block'


def rms_norm(x, g):
    xf = x.astype(jnp.float32)
    y = xf * lax.rsqrt(jnp.mean(xf * xf, axis=-1, keepdims=True) + EPS)
    return (y * g.astype(jnp.float32)).astype(x.dtype)


def modulate(h, shift, scale):
    return h * (1 + scale) + shift


def rope_tables(n_tok, dtype):
    half = HEAD_DIM // 2
    t = jnp.arange(n_tok)
    inv = ROPE_THETA ** (-jnp.arange(0, half, 2, dtype=jnp.float32) / half)
    ang_r = (t // GRID_W).astype(jnp.float32)[:, None] * inv
    ang_c = (t % GRID_W).astype(jnp.float32)[:, None] * inv
    ang = jnp.concatenate([ang_r, ang_r, ang_c, ang_c], axis=-1)
    return jnp.cos(ang).astype(dtype), jnp.sin(ang).astype(dtype)


def apply_rope(x, cos, sin):
    x1, x2, x3, x4 = jnp.split(x, 4, axis=-1)
    rot = jnp.concatenate([-x2, x1, -x4, x3], axis=-1)
    return x * cos[:, None, :] + rot * sin[:, None, :]


def split_heads(z, n_heads):
    B, T, _ = z.shape
    return z.reshape(B, T, n_heads, HEAD_DIM)


def attend(q, k, v):
    s = jnp.einsum('bqkgd,bskd->bkgqs', q, k).astype(jnp.float32) * (HEAD_DIM ** -0.5)
    p = jax.nn.softmax(s, axis=-1).astype(v.dtype)
    return jnp.einsum('bkgqs,bskd->bqkgd', p, v)


def zero_state(B):
    return (jnp.zeros((B, ML_HEADS, ML_HEAD_DIM, ML_HEAD_DIM), jnp.float32),
            jnp.zeros((B, ML_HEADS, ML_HEAD_DIM), jnp.float32),
            jnp.zeros((B, ML_HEADS), jnp.float32))


def mlstm_scan(q, k, v, ig, lf, state):
    B, H, T, d = q.shape
    nc = T // ML_CHUNK

    def to_chunks(a):
        return jnp.moveaxis(a.reshape((B, H, nc, ML_CHUNK) + a.shape[3:]), 2, 0)

    tril = jnp.tril(jnp.ones((ML_CHUNK, ML_CHUNK), dtype=bool))

    def body(carry, xs):
        C, n, m = carry
        qc, kc, vc, igc, lfc = xs
        b = jnp.cumsum(lfc, axis=-1)
        logd = jnp.where(tril, b[..., :, None] - b[..., None, :] + igc[..., None, :], -jnp.inf)
        m_t = jnp.maximum(b + m[..., None], jnp.max(logd, axis=-1))
        dw = jnp.exp(logd - m_t[..., None])
        inter = jnp.exp(b + m[..., None] - m_t)
        s = jnp.einsum('bhtd,bhsd->bhts', qc, kc) * dw
        num = jnp.einsum('bhts,bhse->bhte', s, vc) + inter[..., None] * jnp.einsum('bhtd,bhde->bhte', qc, C)
        den = jnp.sum(s, axis=-1) + inter * jnp.einsum('bhtd,bhd->bht', qc, n)
        h = num / jnp.maximum(jnp.abs(den), jnp.exp(-m_t))[..., None]
        b_last = b[..., -1]
        log_w = b_last[..., None] - b + igc
        m_new = jnp.maximum(b_last + m, jnp.max(log_w, axis=-1))
        w = jnp.exp(log_w - m_new[..., None])
        decay = jnp.exp(b_last + m - m_new)
        C_new = decay[..., None, None] * C + jnp.einsum('bhs,bhsd,bhse->bhde', w, kc, vc)
        n_new = decay[..., None] * n + jnp.einsum('bhs,bhsd->bhd', w, kc)
        return (C_new, n_new, m_new), h

    final, h = lax.scan(body, state, (to_chunks(q), to_chunks(k), to_chunks(v), to_chunks(ig), to_chunks(lf)))
    h = jnp.moveaxis(h, 0, 2).reshape(B, H, T, d)
    return h, final


def mlstm_branch(zq, zk, zv, zo, zgate, gate_bias, g_out, state_f, state_b):
    B, T, _ = zq.shape

    def heads(z):
        return z.reshape(B, T, ML_HEADS, ML_HEAD_DIM).transpose(0, 2, 1, 3)

    q, k, v = heads(zq), heads(zk) * (ML_HEAD_DIM ** -0.5), heads(zv)
    g = (zgate.astype(jnp.float32) + gate_bias.astype(jnp.float32)).reshape(B, T, 4, ML_HEADS).transpose(2, 0, 3, 1)
    i_f, f_f, i_b, f_b = g[0], g[1], g[2], g[3]
    h_f, st_f = mlstm_scan(q, k, v, i_f, jax.nn.log_sigmoid(f_f), state_f)
    rev = lambda a: jnp.flip(a, axis=2)
    h_b, st_b = mlstm_scan(rev(q), rev(k), rev(v), rev(i_b), jax.nn.log_sigmoid(rev(f_b)), state_b)
    h = (h_f + rev(h_b)).transpose(0, 2, 1, 3)
    h = rms_norm(h, g_out.reshape(ML_HEADS, ML_HEAD_DIM)).reshape(B, T, BRANCH_W)
    y = h * jax.nn.sigmoid(zo.astype(jnp.float32))
    return y.astype(zq.dtype), st_f, st_b


def gqa_qkv(zq, zk, zv, g_q, g_k):
    B, T, _ = zq.shape
    q = rms_norm(zq.reshape(B, T, GQA_HEADS, HEAD_DIM), g_q)
    k = rms_norm(zk.reshape(B, T, GQA_KV_HEADS, HEAD_DIM), g_k)
    v = zv.reshape(B, T, GQA_KV_HEADS, HEAD_DIM)
    return q, k, v


def gqa_latent(q, k, v, kc, vc):
    B, T = q.shape[:2]
    k_all = jnp.concatenate([kc, k], axis=1)
    v_all = jnp.concatenate([vc, v], axis=1)
    qb = q.reshape(B, T // Q_BLOCK, Q_BLOCK, GQA_KV_HEADS, GQA_GROUP, HEAD_DIM)
    out = lax.map(lambda qi: attend(qi, k_all, v_all), jnp.moveaxis(qb, 1, 0))
    return jnp.moveaxis(out, 0, 1).reshape(B, T, BRANCH_W)


def na_latent(q, k, v, kc, vc, rpb):
    B, T, H, d = q.shape
    rows = T // GRID_W
    wr = min(NA_WIN_R, rows)
    wc = NA_WIN_C
    qg = q.reshape(B, rows, GRID_W, H, d)
    kg = k.reshape(B, rows, GRID_W, H, d)
    vg = v.reshape(B, rows, GRID_W, H, d)
    cols = jnp.arange(GRID_W)
    c0 = jnp.clip(cols - wc // 2, 0, GRID_W - wc)
    col_idx = c0[:, None] + jnp.arange(wc)[None, :]
    dc = col_idx - cols[:, None] + (NA_WIN_C - 1)
    scale = d ** -0.5

    def row_block(r):
        r0 = jnp.clip(r - wr // 2, 0, rows - wr)
        k_win = lax.dynamic_slice_in_dim(kg, r0, wr, axis=1)[:, :, col_idx]
        v_win = lax.dynamic_slice_in_dim(vg, r0, wr, axis=1)[:, :, col_idx]
        q_row = lax.dynamic_index_in_dim(qg, r, axis=1, keepdims=False)
        dr = r0 + jnp.arange(wr) - r + (NA_WIN_R - 1)
        bias = rpb[:, dr[None, :, None], dc[:, None, :]]
        s_win = jnp.einsum('bqhd,brqjhd->bhqrj', q_row, k_win).astype(jnp.float32) * scale + bias.astype(jnp.float32)
        s_ctx = jnp.einsum('bqhd,bshd->bhqs', q_row, kc).astype(jnp.float32) * scale
        s = jnp.concatenate([s_win.reshape(B, H, GRID_W, wr * wc), s_ctx], axis=-1)
        p = jax.nn.softmax(s, axis=-1).astype(v.dtype)
        p_win = p[..., :wr * wc].reshape(B, H, GRID_W, wr, wc)
        p_ctx = p[..., wr * wc:]
        o = jnp.einsum('bhqrj,brqjhd->bqhd', p_win, v_win) + jnp.einsum('bhqs,bshd->bqhd', p_ctx, vc)
        return o.reshape(B, GRID_W, H * d)

    out = lax.map(row_block, jnp.arange(rows))
    return jnp.moveaxis(out, 0, 1).reshape(B, T, H * d)


def merge_branches(y_ml, y_ga, y_na, zg, w_branch, w_out):
    B, T, _ = zg.shape
    gates = jax.nn.sigmoid(zg.astype(jnp.float32)).astype(zg.dtype).reshape(B, T, N_BRANCH, D_MODEL)
    ys = jnp.stack([y_ml, y_ga, y_na], axis=2)
    proj = jnp.einsum('btnc,ncd->btnd', ys, w_branch)
    return jnp.sum(gates * proj, axis=2) @ w_out


def conv_ffn(h, w_up, conv_w, conv_b, w_down):
    u = h @ w_up
    up = jnp.pad(u, ((0, 0), (1, 1), (0, 0)))
    u = up[:, :-2] * conv_w[0] + up[:, 1:-1] * conv_w[1] + up[:, 2:] * conv_w[2] + conv_b
    a, g = jnp.split(u, 2, axis=-1)
    return (a * jax.nn.silu(g)) @ w_down


def setup_inputs(seed: int = 0) -> dict:
    key = jax.random.key(seed)
    ks = jax.random.split(key, 24)
    f32 = jnp.float32

    def nrm(k, shape, s):
        return jax.random.normal(k, shape, f32) * s

    x = nrm(ks[0], (BATCH, SEQ, D_MODEL), 1.0)
    c = nrm(ks[1], (BATCH, D_MODEL), 1.0)
    ctx = nrm(ks[2], (BATCH, CTX_LEN, D_MODEL), 1.0)
    c_ctx = nrm(ks[3], (D_MODEL,), 1.0)
    w_mod = nrm(ks[4], (DEPTH, D_MODEL, 6 * D_MODEL), 0.5 * D_MODEL ** -0.5)
    b_mod = nrm(ks[5], (DEPTH, 6 * D_MODEL), 0.02)
    g_pre_mix = 1.0 + nrm(ks[6], (DEPTH, D_MODEL), 0.05)
    g_post_mix = 1.0 + nrm(ks[7], (DEPTH, D_MODEL), 0.05)
    g_pre_ffn = 1.0 + nrm(ks[8], (DEPTH, D_MODEL), 0.05)
    g_post_ffn = 1.0 + nrm(ks[9], (DEPTH, D_MODEL), 0.05)
    w_in = nrm(ks[10], (DEPTH, D_MODEL, D_IN), D_MODEL ** -0.5)
    i_bias = nrm(ks[11], (DEPTH, 2, ML_HEADS), 0.1)
    f_bias = jnp.linspace(3.0, 6.0, ML_HEADS, dtype=f32) + nrm(ks[12], (DEPTH, 2, ML_HEADS), 0.1)
    b_ml_gates = jnp.stack([i_bias[:, 0], f_bias[:, 0], i_bias[:, 1], f_bias[:, 1]], axis=1).reshape(DEPTH, 4 * ML_HEADS)
    g_ml_out = 1.0 + nrm(ks[13], (DEPTH, BRANCH_W), 0.05)
    g_q = 1.0 + nrm(ks[14], (DEPTH, HEAD_DIM), 0.05)
    g_k = 1.0 + nrm(ks[15], (DEPTH, HEAD_DIM), 0.05)
    na_rpb = nrm(ks[16], (DEPTH, NA_HEADS, 2 * NA_WIN_R - 1, 2 * NA_WIN_C - 1), 0.1)
    w_branch = nrm(ks[17], (DEPTH, N_BRANCH, BRANCH_W, D_MODEL), BRANCH_W ** -0.5)
    w_out = nrm(ks[18], (DEPTH, D_MODEL, D_MODEL), D_MODEL ** -0.5)
    w_up = nrm(ks[19], (DEPTH, D_MODEL, 2 * FFN_DIM), D_MODEL ** -0.5)
    conv_w = nrm(ks[20], (DEPTH, CONV_K, 2 * FFN_DIM), 0.3) + jnp.array([0.0, 1.0, 0.0], f32)[None, :, None]
    conv_b = nrm(ks[21], (DEPTH, 2 * FFN_DIM), 0.02)
    w_down = nrm(ks[22], (DEPTH, FFN_DIM, D_MODEL), FFN_DIM ** -0.5)
    return {'x': x, 'c': c, 'ctx': ctx, 'c_ctx': c_ctx, 'w_mod': w_mod, 'b_mod': b_mod,
            'g_pre_mix': g_pre_mix, 'g_post_mix': g_post_mix, 'g_pre_ffn': g_pre_ffn, 'g_post_ffn': g_post_ffn,
            'w_in': w_in, 'b_ml_gates': b_ml_gates, 'g_ml_out': g_ml_out, 'g_q': g_q, 'g_k': g_k,
            'na_rpb': na_rpb, 'w_branch': w_branch, 'w_out': w_out, 'w_up': w_up, 'conv_w': conv_w,
            'conv_b': conv_b, 'w_down': w_down}


def reference(x, c, ctx, c_ctx, w_mod, b_mod, g_pre_mix, g_post_mix, g_pre_ffn, g_post_ffn, w_in, b_ml_gates,
              g_ml_out, g_q, g_k, na_rpb, w_branch, w_out, w_up, conv_w, conv_b, w_down):
    B, T, _ = x.shape
    cos, sin = rope_tables(T, x.dtype)
    xc = ctx
    for l in range(DEPTH):
        last = l == DEPTH - 1
        mod = (jax.nn.silu(c) @ w_mod[l] + b_mod[l])[:, None, :]
        mod_c = jax.nn.silu(c_ctx) @ w_mod[l] + b_mod[l]
        sh1, sc1, ga1, sh2, sc2, ga2 = jnp.split(mod, 6, axis=-1)
        csh1, csc1, cga1, csh2, csc2, cga2 = jnp.split(mod_c, 6, axis=-1)

        h = modulate(rms_norm(x, g_pre_mix[l]), sh1, sc1)
        hc = modulate(rms_norm(xc, g_pre_mix[l]), csh1, csc1)
        (mq, mk, mv, mo, mg, aq, ak, av, nq, nk, nv, zg) = jnp.split(h @ w_in[l], IN_SPLITS, axis=-1)
        (cmq, cmk, cmv, cmo, cmg, caq, cak, cav, cnq, cnk, cnv, czg) = jnp.split(hc @ w_in[l], IN_SPLITS, axis=-1)

        yc_ml, st_f, st_b = mlstm_branch(cmq, cmk, cmv, cmo, cmg, b_ml_gates[l], g_ml_out[l], zero_state(B), zero_state(B))
        y_ml, _, _ = mlstm_branch(mq, mk, mv, mo, mg, b_ml_gates[l], g_ml_out[l], st_f, st_b)

        qc_a, kc_a, vc_a = gqa_qkv(caq, cak, cav, g_q[l], g_k[l])
        q_a, k_a, v_a = gqa_qkv(aq, ak, av, g_q[l], g_k[l])
        y_ga = gqa_latent(apply_rope(q_a, cos, sin), apply_rope(k_a, cos, sin), v_a, kc_a, vc_a)

        kc_n = split_heads(cnk, NA_HEADS)
        vc_n = split_heads(cnv, NA_HEADS)
        y_na = na_latent(split_heads(nq, NA_HEADS), split_heads(nk, NA_HEADS), split_heads(nv, NA_HEADS), kc_n, vc_n, na_rpb[l])

        o = merge_branches(y_ml, y_ga, y_na, zg, w_branch[l], w_out[l])
        x = x + ga1 * rms_norm(o, g_post_mix[l])
        f = conv_ffn(modulate(rms_norm(x, g_pre_ffn[l]), sh2, sc2), w_up[l], conv_w[l], conv_b[l], w_down[l])
        x = x + ga2 * rms_norm(f, g_post_ffn[l])

        if not last:
            Tc = xc.shape[1]
            yc_ga = attend(qc_a.reshape(B, Tc, GQA_KV_HEADS, GQA_GROUP, HEAD_DIM), kc_a, vc_a).reshape(B, Tc, BRANCH_W)
            yc_na = attend(split_heads(cnq, NA_HEADS)[:, :, :, None, :], kc_n, vc_n).reshape(B, Tc, BRANCH_W)
            oc = merge_branches(yc_ml, yc_ga, yc_na, czg, w_branch[l], w_out[l])
            xc = xc + cga1 * rms_norm(oc, g_post_mix[l])
            fc = conv_ffn(modulate(rms_norm(xc, g_pre_ffn[l]), csh2, csc2), w_up[l], conv_w[l], conv_b[l], w_down[l])
            xc = xc + cga2 * rms_norm(fc, g_post_ffn[l])
    return x
```

```python
import numpy as np
from contextlib import ExitStack
import concourse.bass as bass
import concourse.mybir as mybir
from concourse.bass_utils import run_bass_kernel_spmd

F32 = mybir.dt.float32
BF16 = mybir.dt.bfloat16
AF = mybir.ActivationFunctionType
ALU = mybir.AluOpType
AX = mybir.AxisListType

D = 1024; KC = 8; TL = 4096; TC = 256; S = TL + TC; NT = S // 128; NCT = TC // 128
DEPTH = 4; FFN = 2816; NFT = FFN // 128; D_IN = 7440
GRID = 64; EPS = 1e-6
C_MQ, C_MK, C_MV, C_MO, C_MG = 0, 512, 1024, 1536, 2048
C_AQ, C_AK, C_AV = 2064, 2576, 2704
C_NQ, C_NK, C_NV, C_ZG = 2832, 3344, 3856, 4368
ENGS = ("pe", "act", "dve", "pool", "sp")
N_DMA_SEMS = 14
NEG = -30000.0


class Prog:
    def __init__(self, nc):
        self.nc = nc
        self.ops = {e: [] for e in ENGS}
        self.cnt = {e: 0 for e in ENGS}
        self.res = {}
        self.seen = {e: {} for e in ENGS}
        self.dma_i = 0
        self.dma_hist = {}
        self.nops = 0
        self.ep = 0
        self.allsems = set()
        self.final = []

    def sn(self, base):
        n = "%s_e%d" % (base, self.ep)
        self.allsems.add(n)
        return n

    def new_epoch(self):
        self.barrier()
        self.ep += 1
        self.cnt = {e: 0 for e in ENGS}
        self.dma_hist = {}
        self.seen = {e: {} for e in ENGS}

    def _deps(self, reads, writes):
        ev = set()
        for r in reads:
            st = self.res.get(r)
            if st and st[0]:
                ev.add(st[0])
        for w in writes:
            st = self.res.get(w)
            if st:
                if st[0]:
                    ev.add(st[0])
                ev.update(st[1])
        return ev

    def _commit(self, event, reads, writes):
        for r in reads:
            st = self.res.setdefault(r, [None, []])
            st[1].append(event)
        for w in writes:
            self.res[w] = [event, []]

    def _filter(self, eng, evs, own_sem=None):
        seen = self.seen[eng]
        best = {}
        for (s, v) in evs:
            if s == own_sem and eng == "pe":
                continue
            if seen.get(s, 0) >= v:
                continue
            if best.get(s, 0) < v:
                best[s] = v
        for s, v in best.items():
            seen[s] = v
        return list(best.items())

    def op(self, eng, fn, reads=(), writes=()):
        writes = tuple(writes) + tuple(r for r in reads if r.startswith("ps"))
        reads = tuple(r for r in reads if not r.startswith("ps"))
        evs = self._deps(reads, writes)
        own = self.sn("c_" + eng)
        waits = self._filter(eng, evs, own)
        self.cnt[eng] += 1
        event = (own, self.cnt[eng])
        self.ops[eng].append((waits, fn, (own, 1)))
        self._commit(event, reads, writes)
        self.nops += 1

    def dma(self, eng, fn, reads=(), writes=()):
        reads = tuple(reads); writes = tuple(writes)
        evs = self._deps(reads, writes)
        j = self.dma_i % N_DMA_SEMS
        sem = self.sn("d_%d" % j)
        prev = self.dma_hist.get(j, 0)
        if prev:
            evs.add((sem, prev))
        val = prev + 16
        self.dma_hist[j] = val
        self.dma_i += 1
        waits = self._filter(eng, evs, None)
        self.ops[eng].append((waits, fn, (sem, 16)))
        self._commit((sem, val), reads, writes)
        self.nops += 1

    def barrier(self):
        evs = [(self.sn("c_" + e), self.cnt[e]) for e in ENGS if self.cnt[e]]
        evs += [(self.sn("d_%d" % j), v) for j, v in self.dma_hist.items()]
        self.final = list(evs)
        for e in ENGS:
            waits = self._filter(e, evs, None)
            if waits:
                self.ops[e].append((waits, None, None))
        self.res = {}

    def emit(self):
        nc = self.nc
        with ExitStack() as st:
            sems = {}
            for n in sorted(self.allsems):
                sems[n] = st.enter_context(nc.semaphore(n))
            final = [(self.sn("c_" + e), self.cnt[e]) for e in ENGS if self.cnt[e] and e != "sp"]
            final += [(self.sn("d_%d" % j), v) for j, v in self.dma_hist.items()]
            block = st.enter_context(nc.Block())

            def run(engname):
                def body(eng):
                    for waits, fn, inc in self.ops[engname]:
                        for (ws, wv) in waits:
                            eng.wait_ge(sems[ws], wv)
                        if fn is not None:
                            fn(eng).then_inc(sems[inc[0]], inc[1])
                    if engname == "sp":
                        for (ws, wv) in final:
                            eng.wait_ge(sems[ws], wv)
                return body

            block.tensor(run("pe"))
            block.scalar(run("act"))
            block.vector(run("dve"))
            block.gpsimd(run("pool"))
            block.sync(run("sp"))


class Arena:
    def __init__(self, ap, nwords):
        self.ap = ap; self.n = nwords; self.off = 0; self.uid = 0

    def mark(self):
        return self.off

    def release(self, m):
        self.off = m

    def alloc(self, shape, dt, parts=128):
        n = int(np.prod(shape))
        words = n if dt == F32 else (n + 1) // 2
        words = (words + 7) // 8 * 8
        assert self.off + words <= self.n, ("SBUF arena overflow", self.off, words, self.n)
        a = self.ap[0:parts, self.off:self.off + words]
        self.off += words
        if dt != F32:
            a = a.bitcast(dt)
        a = a[:, 0:n]
        if len(shape) > 1:
            names = [chr(ord('a') + i) for i in range(len(shape))]
            kw = {names[i]: int(shape[i]) for i in range(len(shape))}
            a = a.rearrange("p (%s) -> p %s" % (" ".join(names), " ".join(names)), **kw)
        self.uid += 1
        return a, "b%d" % self.uid


class K:
    def __init__(self, P):
        self.P = P

    def mm(self, out, lhsT, rhs, start, stop, r, w):
        self.P.op("pe", lambda e: e.matmul(out, lhsT=lhsT, rhs=rhs, start=start, stop=stop), r, w)

    def tr(self, out, in_, ident, r, w):
        self.P.op("pe", lambda e: e.transpose(out, in_, ident), r, w)

    def act(self, out, in_, func, r, w, scale=None, bias=None, accum=None, eng="act"):
        kw = {}
        if scale is not None: kw["scale"] = scale
        if bias is not None: kw["bias"] = bias
        if accum is not None: kw["accum_out"] = accum
        self.P.op("act", lambda e: e.activation(out=out, in_=in_, func=func, **kw), r, w)

    def tt(self, out, in0, in1, op, r, w, eng="dve"):
        self.P.op(eng, lambda e: e.tensor_tensor(out=out, in0=in0, in1=in1, op=op), r, w)

    def ts(self, out, in0, s1, op0, r, w, s2=None, op1=None, eng="dve"):
        if op1 is None:
            self.P.op(eng, lambda e: e.tensor_scalar(out=out, in0=in0, scalar1=s1, scalar2=None, op0=op0), r, w)
        else:
            self.P.op(eng, lambda e: e.tensor_scalar(out=out, in0=in0, scalar1=s1, scalar2=s2, op0=op0, op1=op1), r, w)

    def stt(self, out, in0, scalar, in1, op0, op1, r, w):
        self.P.op("dve", lambda e: e.scalar_tensor_tensor(out=out, in0=in0, scalar=scalar, in1=in1, op0=op0, op1=op1), r, w)

    def copy(self, out, in_, r, w, eng="dve"):
        self.P.op(eng, lambda e: e.tensor_copy(out=out, in_=in_), r, w)

    def recip(self, out, in_, r, w):
        self.P.op("dve", lambda e: e.reciprocal(out=out, in_=in_), r, w)

    def reduce(self, out, in_, op, r, w):
        self.P.op("dve", lambda e: e.tensor_reduce(out=out, in_=in_, axis=AX.X, op=op), r, w)

    def scan(self, out, d0, d1, init, op0, op1, r, w):
        self.P.op("dve", lambda e: e.tensor_tensor_scan(out=out, data0=d0, data1=d1, initial=init, op0=op0, op1=op1), r, w)

    def ttr(self, out, in0, in1, accum, r, w):
        self.P.op("dve", lambda e: e.tensor_tensor_reduce(out=out, in0=in0, in1=in1, scale=1.0, scalar=0.0,
                                                          op0=ALU.mult, op1=ALU.add, accum_out=accum), r, w)

    def memset(self, ap, v, w, eng="pool"):
        self.P.op(eng, lambda e: e.memset(ap, v), (), w)

    def dma(self, out, in_, r, w, eng="sp"):
        self.P.dma(eng, lambda e: e.dma_start(out=out, in_=in_), r, w)


def r0_of(r):
    return min(max(r - 4, 0), GRID - 8)


def build(n_layers=DEPTH, debug=False, arena_words=51200, phases=None, force_last=False):
    nc = bass.Bass("TRN2", target_bir_lowering=False)

    def din(name, shape, dt=F32):
        return nc.dram_tensor(name, list(shape), dt, kind="ExternalInput").ap()

    x_in = din("x_in", [TL, D]); ctx_in = din("ctx_in", [TC, D])
    cT = din("cT", [128, 2, 8])
    w_mod = din("w_mod", [DEPTH, D, 6 * D]); b_mod = din("b_mod", [DEPTH, 6 * D])
    gfm = din("gfm", [DEPTH, 128, 2, 8])
    gpost = din("gpost", [DEPTH, 2, D])
    w_in = din("w_in", [DEPTH, D, D_IN])
    bgate = din("bgate", [DEPTH, 4, 4])
    gml = din("gml", [DEPTH, 512])
    gqk = din("gqk", [DEPTH, 384])
    rpbT = din("rpbT", [DEPTH, 4, 128, 2 * 7 * 128])
    w_branch = din("w_branch", [DEPTH, 3, 512, D]); w_out = din("w_out", [DEPTH, D, D])
    w_up = din("w_up", [DEPTH, D, 2 * FFN]); w_down = din("w_down", [DEPTH, FFN, D])
    cwfm = din("cwfm", [DEPTH, 128, 2 * NFT * 3]); cbfm = din("cbfm", [DEPTH, 128, 2 * NFT])
    ropec = din("ropec", [128, 32 * 64]); ropes = din("ropes", [128, 32 * 64])
    cst = din("cst", [128, 5 * 128])
    yout = nc.dram_tensor("y", [TL, D], F32, kind="ExternalOutput").ap()
    okind = "ExternalOutput" if debug else "Internal"
    X = nc.dram_tensor("Xres", [S, D], F32, kind=okind).ap()
    YT = [nc.dram_tensor("YT%d" % n, [512, S], BF16, kind=okind).ap() for n in range(3)]
    MTm = nc.dram_tensor("MTm", [D, S], BF16, kind="Internal").ap()
    MTf = nc.dram_tensor("MTf", [FFN, S], BF16, kind="Internal").ap()

    with ExitStack() as st:
        arena_t = st.enter_context(nc.sbuf_tensor("arena", [128, arena_words], F32))
        A = Arena(arena_t, arena_words)
        PS = [st.enter_context(nc.psum_tensor("ps%d" % i, [128, 512], F32)) for i in range(8)]
        PK = ["ps%d" % i for i in range(8)]
        P = Prog(nc)
        k = K(P)

        cstt, kc = A.alloc([5 * 128], F32)
        identF = cstt[:, 0:128]; onesF = cstt[:, 128:256]
        maskd = [cstt[:, 256:384], cstt[:, 384:512]]
        eselt, kes = A.alloc([4 * 128], F32, parts=4)
        identB, kib = A.alloc([128], BF16)
        srep, ksr = A.alloc([2, 8, 128], F32)
        hT, khT = A.alloc([KC, S], BF16)
        Gbc, kG = A.alloc([2, 2, D], F32)
        fmv, kfm = A.alloc([2, 4, 8], F32)
        k.dma(cstt, cst, (), [kc])
        k.copy(identB, identF, [kc], [kib])

        eselin = din("esel", [4, 4 * 128])
        k.dma(eselt, eselin, (), [kes])

        k.dma(X[0:TC, :], ctx_in, (), ["Xi"])
        for q in range(8):
            k.dma(X[TC + q * 512:TC + (q + 1) * 512, :], x_in[q * 512:(q + 1) * 512, :], (), ["Xi%d" % q])

        m0 = A.mark()
        ct_t, kct = A.alloc([2, 8], F32)
        k.dma(ct_t, cT, (), [kct])
        k.act(ct_t, ct_t, AF.Silu, [kct], [kct])
        for r in range(2):
            for kk in range(8):
                k.ts(srep[:, r, kk, :], onesF, ct_t[:, r, kk:kk + 1], ALU.mult, [kct, kc], [ksr])
        P.barrier(); A.release(m0)

        on = lambda nm: phases is None or nm in phases

        def phase_mod(l):
            m = A.mark()
            wm = [A.alloc([8, 512], F32) for _ in range(2)]
            brow = [A.alloc([512], F32, parts=1) for _ in range(2)]
            gp = [A.alloc([512], F32) for _ in range(2)]
            tmp = [A.alloc([512], F32) for _ in range(2)]
            junk, kj = A.alloc([4, 128], F32)
            gf, kgf = A.alloc([2, 8], F32)
            k.dma(gf, gfm[l], (), [kgf])
            for cb in range(12):
                b = cb % 2
                seg = cb // 2; half = cb % 2
                k.dma(wm[b][0], w_mod[l][:, cb * 512:(cb + 1) * 512].rearrange("(k p) c -> p k c", p=128), (), [wm[b][1]])
                k.dma(brow[b][0], b_mod[l:l + 1, cb * 512:(cb + 1) * 512], (), [brow[b][1]])
                if seg in (2, 5):
                    which = 0 if seg == 2 else 1
                    k.dma(gp[b][0], gpost[l, which:which + 1, half * 512:(half + 1) * 512].to_broadcast([128, 512]), (), [gp[b][1]])
                for r in range(2):
                    ps = PS[r + 2 * b]; pk = PK[r + 2 * b]
                    for kk in range(8):
                        k.mm(ps[:], srep[:, r, kk, :], wm[b][0][:, kk, :], kk == 0, False, [ksr, wm[b][1]], [pk])
                    k.mm(ps[:], onesF[0:1, :], brow[b][0], False, True, [kc, brow[b][1]], [pk])
                    if seg in (2, 5):
                        which = 0 if seg == 2 else 1
                        k.tt(Gbc[:, r, which, half * 512:(half + 1) * 512], ps[:], gp[b][0], ALU.mult, [pk, gp[b][1]], [kG])
                    else:
                        slot = {0: 1, 1: 0, 3: 3, 4: 2}[seg]
                        tb = tmp[r]
                        k.copy(tb[0], ps[:], [pk], [tb[1]])
                        k.tt(junk, tb[0].rearrange("p (a b) -> p a b", b=128), identF.unsqueeze(1).to_broadcast([128, 4, 128]),
                             ALU.mult, [tb[1], kc], [kj])
                        k.reduce(fmv[:, r, slot, half * 4:half * 4 + 4], junk, ALU.add, [kj], [kfm])
            for r in range(2):
                for (slot, gi) in ((0, 0), (2, 1)):
                    k.stt(fmv[:, r, slot, :], fmv[:, r, slot, :], 1.0, gf[:, gi, :], ALU.add, ALU.mult, [kfm, kgf], [kfm])
            P.barrier(); A.release(m)

        def phase_norm(which):
            m = A.mark()
            xt = [A.alloc([D], F32) for _ in range(2)]
            xn = [A.alloc([D], BF16) for _ in range(2)]
            junk, kj = A.alloc([D], F32)
            ss, kss = A.alloc([NT], F32)
            for i in range(NT):
                b = i % 2
                k.dma(xt[b][0], X[i * 128:(i + 1) * 128, :], ["X"], [xt[b][1]])
                k.act(junk, xt[b][0], AF.Square, [xt[b][1]], [kj, kss], accum=ss[:, i:i + 1])
            k.ts(ss, ss, 1.0 / D, ALU.mult, [kss], [kss], s2=EPS, op1=ALU.add)
            k.act(ss, ss, AF.Sqrt, [kss], [kss])
            k.recip(ss, ss, [kss], [kss])
            for i in range(NT):
                b = i % 2
                r = 1 if i < NCT else 0
                k.dma(xt[b][0], X[i * 128:(i + 1) * 128, :], ["X"], [xt[b][1]])
                k.ts(xn[b][0], xt[b][0], ss[:, i:i + 1], ALU.mult, [xt[b][1], kss], [xn[b][1]])
                ps = PS[b]; pk = PK[b]
                psb = ps[:].bitcast(BF16).rearrange("p (a b) -> p a b", a=8)
                for kk in range(8):
                    k.tr(psb[:, kk, :], xn[b][0][:, kk * 128:(kk + 1) * 128], identB, [xn[b][1], kib], [pk])
                for kk in range(8):
                    k.act(hT[:, kk, i * 128:(i + 1) * 128], psb[:, kk, :], AF.Identity, [pk, kfm], [khT],
                          scale=fmv[:, r, 2 * which, kk:kk + 1], bias=fmv[:, r, 2 * which + 1, kk:kk + 1])
            P.barrier(); A.release(m)

        def load_w(dst, key, src_cols_ap):
            k.dma(dst, src_cols_ap.rearrange("(k p) c -> p k c", p=128), (), [key], eng="pool")

        TOKCH = [(c * 512, min(512, S - c * 512)) for c in range((S + 511) // 512)]

        def phase_mlstm(l):
            m = A.mark()
            kwT = [A.alloc([NT, 4], F32) for _ in range(2)]
            thT = [A.alloc([NT, 4], F32) for _ in range(2)]
            gbc = [A.alloc([4, NT], F32) for _ in range(2)]
            m1 = A.mark()
            wg, kwg = A.alloc([8, 16], BF16)
            load_w(wg, kwg, w_in[l][:, C_MG:C_MG + 16])
            bg, kbg = A.alloc([4], F32, parts=4)
            k.dma(bg, bgate[l], (), [kbg])
            X0, k0 = A.alloc([S], F32, parts=4); X1, k1 = A.alloc([S], F32, parts=4)
            X2, k2 = A.alloc([S], F32, parts=4); X3, k3 = A.alloc([S], F32, parts=4)
            X4, k4 = A.alloc([S], F32, parts=4)
            cm, kcm = A.alloc([NT], F32, parts=4); ri, kri = A.alloc([NT], F32, parts=4)
            rr, krr = A.alloc([NT], F32, parts=4); gd, kgd = A.alloc([NT], F32, parts=4)
            for d in range(2):
                for (j, dst, kd) in ((2 * d, X0, k0), (2 * d + 1, X1, k1)):
                    for ci, (t0, tn) in enumerate(TOKCH):
                        ps = PS[ci % 2]; pk = PK[ci % 2]
                        for kk in range(8):
                            k.mm(ps[0:4, 0:tn], wg[:, kk, 4 * j:4 * j + 4], hT[:, kk, t0:t0 + tn], kk == 0, kk == 7, [kwg, khT], [pk])
                        k.act(dst[:, t0:t0 + tn], ps[0:4, 0:tn], AF.Identity, [pk, kbg], [kd], bias=bg[:, j:j + 1], scale=1.0)
                k.act(X1, X1, AF.Exp, [k1], [k1], scale=-1.0)
                k.ts(X2, X1, 2.0, ALU.add, [k1], [k2])
                k.recip(X2, X2, [k2], [k2])
                k.tt(X2, X2, X1, ALU.mult, [k2, k1], [k2])
                k.tt(X3, X2, X2, ALU.mult, [k2], [k3])
                k.ts(X4, X3, 0.2, ALU.mult, [k3], [k4], s2=1.0 / 3.0, op1=ALU.add)
                k.tt(X4, X4, X3, ALU.mult, [k4, k3], [k4])
                k.ts(X4, X4, 1.0, ALU.add, [k4], [k4], s2=-2.0, op1=ALU.mult)
                k.tt(X4, X4, X2, ALU.mult, [k4, k2], [k4])
                if d == 0:
                    k.scan(X1, X4, X4, 0.0, ALU.add, ALU.min, [k4], [k1])
                else:
                    k.scan(X1[:, 0:TC][:, ::-1], X4[:, 0:TC][:, ::-1], X4[:, 0:TC][:, ::-1], 0.0,
                           ALU.add, ALU.min, [k4], [k1])
                    k.scan(X1[:, TC:S][:, ::-1], X4[:, TC:S][:, ::-1], X4[:, TC:S][:, ::-1], X1[:, 0:1],
                           ALU.add, ALU.min, [k4, k1], [k1])
                k.tt(X2, X0, X1, ALU.subtract, [k0, k1], [k2])
                k.reduce(cm, X2.rearrange("p (c t) -> p c t", t=128), ALU.max, [k2], [kcm])
                if d == 0:
                    k.scan(ri, cm, cm, 0.0, ALU.max, ALU.max, [kcm], [kri])
                    k.memset(rr[:, 0:1], 0.0, [krr], eng="dve")
                    k.copy(rr[:, 1:NT], ri[:, 0:NT - 1], [kri], [krr])
                else:
                    k.scan(ri[:, 0:NCT][:, ::-1], cm[:, 0:NCT][:, ::-1], cm[:, 0:NCT][:, ::-1], 0.0, ALU.max, ALU.max, [kcm], [kri])
                    k.scan(ri[:, NCT:NT][:, ::-1], cm[:, NCT:NT][:, ::-1], cm[:, NCT:NT][:, ::-1], ri[:, 0:1], ALU.max, ALU.max,
                           [kcm, kri], [kri])
                    k.memset(rr[:, NCT - 1:NCT], 0.0, [krr], eng="dve")
                    k.copy(rr[:, 0:NCT - 1], ri[:, 1:NCT], [kri], [krr])
                    k.copy(rr[:, NT - 1:NT], ri[:, 0:1], [kri], [krr])
                    k.copy(rr[:, NCT:NT - 1], ri[:, NCT + 1:NT], [kri], [krr])
                rrb = rr.unsqueeze(2).to_broadcast([4, NT, 128])
                k.tt(X3.rearrange("p (c t) -> p c t", t=128), X2.rearrange("p (c t) -> p c t", t=128), rrb, ALU.subtract, [k2, krr], [k3])
                k.act(X3, X3, AF.Exp, [k3], [k3])
                k.tt(X0.rearrange("p (c t) -> p c t", t=128), X1.rearrange("p (c t) -> p c t", t=128), rrb, ALU.add, [k1, krr], [k0])
                k.act(X0, X0, AF.Exp, [k0], [k0], scale=-1.0)
                k.tt(gd, rr, ri, ALU.subtract, [krr, kri], [kgd])
                k.act(gd, gd, AF.Exp, [kgd], [kgd])
                for (src, ks, dstp) in ((X3, k3, kwT[d]), (X0, k0, thT[d])):
                    ps = PS[2]; pk = PK[2]
                    for c in range(NT):
                        k.tr(ps[:, c * 4:(c + 1) * 4], src[:, c * 128:(c + 1) * 128], identF[0:4, 0:4], [ks, kc], [pk])
                    k.copy(dstp[0], ps[:, 0:NT * 4].rearrange("p (c h) -> p c h", h=4), [pk], [dstp[1]])
                ps = PS[3]; pk = PK[3]
                for h in range(4):
                    k.mm(ps[:, h * NT:(h + 1) * NT], eselt[:, h * 128:(h + 1) * 128], gd, True, True, [kes, kgd], [pk])
                k.copy(gbc[d][0], ps[:, 0:4 * NT].rearrange("p (h c) -> p h c", h=4), [pk], [gbc[d][1]])
            P.barrier(); A.release(m1)

            gmlb, kgm = A.alloc([512], F32)
            k.dma(gmlb, gml[l:l + 1, :].to_broadcast([128, 512]), (), [kgm])
            for h in range(4 if on("ml_heads") else 0):
                m2 = A.mark()
                wq, kwq = A.alloc([8, 128], BF16); wk_, kwk = A.alloc([8, 128], BF16)
                wkvo, kwkvo = A.alloc([8, 384], BF16)
                load_w(wq, kwq, w_in[l][:, C_MQ + h * 128:C_MQ + (h + 1) * 128])
                load_w(wk_, kwk, w_in[l][:, C_MK + h * 128:C_MK + (h + 1) * 128])
                load_w(wkvo[:, :, 0:128], kwkvo, w_in[l][:, C_MK + h * 128:C_MK + (h + 1) * 128])
                load_w(wkvo[:, :, 128:256], kwkvo, w_in[l][:, C_MV + h * 128:C_MV + (h + 1) * 128])
                load_w(wkvo[:, :, 256:384], kwkvo, w_in[l][:, C_MO + h * 128:C_MO + (h + 1) * 128])
                QT, kQT = A.alloc([S], BF16); KT, kKT = A.alloc([S], BF16)
                Ktm, kKtm = A.alloc([NT, 128], BF16); Va, kVa = A.alloc([NT, 130], BF16)
                Osg, kOs = A.alloc([NT, 128], BF16); Hacc, kH = A.alloc([NT, 128], F32)
                k.memset(Va[:, :, 128:129], 1.0, [kVa])
                k.memset(Hacc, 0.0, [kH + ".%d" % c for c in range(NT)])
                for ci, (t0, tn) in enumerate(TOKCH if on("ml_fm") else []):
                    for (wt, kwt, dst, kd, sc, pi) in ((wq, kwq, QT, kQT, 1.0, 0), (wk_, kwk, KT, kKT, 128.0 ** -0.5, 1)):
                        ps = PS[pi + 2 * (ci % 2)]; pk = PK[pi + 2 * (ci % 2)]
                        for kk in range(8):
                            k.mm(ps[:, 0:tn], wt[:, kk, :], hT[:, kk, t0:t0 + tn], kk == 0, kk == 7, [kwt, khT], [pk])
                        k.act(dst[:, t0:t0 + tn], ps[:, 0:tn], AF.Identity, [pk], [kd], scale=sc)
                for i in range(NT if on("ml_tm") else 0):
                    ps = PS[4 + i % 2]; pk = PK[4 + i % 2]
                    for kk in range(8):
                        k.mm(ps[:, 0:384], hT[:, kk, i * 128:(i + 1) * 128], wkvo[:, kk, :], kk == 0, kk == 7, [khT, kwkvo], [pk])
                    k.act(Ktm[:, i, :], ps[:, 0:128], AF.Identity, [pk], [kKtm], scale=128.0 ** -0.5)
                    k.copy(Va[:, i, 0:128], ps[:, 128:256], [pk], [kVa])
                    k.act(Osg[:, i, :], ps[:, 256:384], AF.Sigmoid, [pk], [kOs])
                Cf = [A.alloc([129], F32) for _ in range(2)]; Cb = [A.alloc([130], BF16) for _ in range(2)]
                PmT = [A.alloc([128], BF16) for _ in range(2)]; Kp = [A.alloc([128], BF16) for _ in range(2)]
                dd = [A.alloc([2], F32) for _ in range(2)]
                for d in range(2):
                    k.memset(Cf[d][0], 0.0, [Cf[d][1]]); k.memset(Cb[d][0], 0.0, [Cb[d][1]])
                order = [list(range(NT)), [1, 0] + list(range(NT - 1, NCT - 1, -1))]
                for step in range(NT if on("ml_rec") else 0):
                    for d in range(2):
                        c = order[d][step]
                        cs = slice(c * 128, (c + 1) * 128)
                        pS, kS = PS[0 + d], PK[0 + d]; pO, kO = PS[2 + d], PK[2 + d]; pU, kU = PS[6 + d], PK[6 + d]
                        k.mm(pS[:, 0:128], KT[:, cs], QT[:, cs], True, True, [kKT, kQT], [kS])
                        k.stt(PmT[d][0], pS[:, 0:128], kwT[d][0][:, c, h:h + 1], maskd[d], ALU.mult, ALU.mult,
                              [kS, kwT[d][1], kc], [PmT[d][1]])
                        k.mm(pO[:, 0:129], PmT[d][0], Va[:, c, 0:129], True, False, [PmT[d][1], kVa], [kO])
                        k.mm(pO[:, 0:129], QT[:, cs], Cb[d][0][:, 0:129], False, True, [kQT, Cb[d][1]], [kO])
                        k.act(dd[d][0][:, 0:1], pO[:, 128:129], AF.Abs, [kO], [dd[d][1]])
                        k.tt(dd[d][0][:, 0:1], dd[d][0][:, 0:1], thT[d][0][:, c, h:h + 1], ALU.max, [dd[d][1], thT[d][1]], [dd[d][1]])
                        k.recip(dd[d][0][:, 1:2], dd[d][0][:, 0:1], [dd[d][1]], [dd[d][1]])
                        if True:
                            k.stt(Hacc[:, c, :], pO[:, 0:128], dd[d][0][:, 1:2], Hacc[:, c, :], ALU.mult, ALU.add,
                                  [kO, dd[d][1], kH + ".%d" % c], [kH + ".%d" % c])
                        if step < NT - 1:
                            k.ts(Kp[d][0], Ktm[:, c, :], kwT[d][0][:, c, h:h + 1], ALU.mult, [kKtm, kwT[d][1]], [Kp[d][1]], eng="pool")
                            k.mm(pU[:, 0:129], Kp[d][0], Va[:, c, 0:129], True, True, [Kp[d][1], kVa], [kU])
                            k.tt(Cf[d][0], pU[:, 0:129], Cf[d][0], ALU.add, [kU, Cf[d][1]], [Cf[d][1]])
                            k.ts(Cf[d][0], Cf[d][0], gbc[d][0][:, h, c:c + 1], ALU.mult, [Cf[d][1], gbc[d][1]], [Cf[d][1]])
                            k.act(Cb[d][0][:, 0:129], Cf[d][0], AF.Identity, [Cf[d][1]], [Cb[d][1]])
                ssq, kssq = A.alloc([NT], F32)
                sq, ksq = A.alloc([8, 128], F32); yb, kyb = A.alloc([8, 128], BF16); ys, kys = A.alloc([8 * 128], BF16)
                hk_all = [kH + ".%d" % c for c in range(NT)]
                for g0 in range(0, NT if on("ml_fin") else 0, 8):
                    gn = min(8, NT - g0)
                    hv = Hacc[:, g0:g0 + gn, :]
                    k.tt(sq[:, 0:gn, :], hv, hv, ALU.mult, hk_all, [ksq])
                    k.reduce(ssq[:, g0:g0 + gn], sq[:, 0:gn, :], ALU.add, [ksq], [kssq])
                if on("ml_fin"):
                    k.ts(ssq, ssq, 1.0 / 128, ALU.mult, [kssq], [kssq], s2=EPS, op1=ALU.add)
                    k.act(ssq, ssq, AF.Sqrt, [kssq], [kssq])
                    k.recip(ssq, ssq, [kssq], [kssq])
                for g0 in range(0, NT if on("ml_fin") else 0, 8):
                    gn = min(8, NT - g0)
                    hv = Hacc[:, g0:g0 + gn, :]
                    k.tt(sq[:, 0:gn, :], hv, ssq[:, g0:g0 + gn].unsqueeze(2).to_broadcast([128, gn, 128]), ALU.mult, hk_all + [kssq], [ksq])
                    k.tt(sq[:, 0:gn, :], sq[:, 0:gn, :], gmlb[:, h * 128:(h + 1) * 128].unsqueeze(1).to_broadcast([128, gn, 128]),
                         ALU.mult, [ksq, kgm], [ksq])
                    k.tt(yb[:, 0:gn, :], sq[:, 0:gn, :], Osg[:, g0:g0 + gn, :], ALU.mult, [ksq, kOs], [kyb])
                    ps = PS[4 + (g0 // 8) % 2]; pk = PK[4 + (g0 // 8) % 2]
                    psb = ps[:].bitcast(BF16)
                    for q in range(gn):
                        k.tr(psb[:, q * 128:(q + 1) * 128], yb[:, q, :], identB, [kyb, kib], [pk])
                    k.copy(ys[:, 0:gn * 128], psb[:, 0:gn * 128], [pk], [kys])
                    k.dma(YT[0][h * 128:(h + 1) * 128, g0 * 128:(g0 + gn) * 128], ys[:, 0:gn * 128], [kys], ["YT0"])
                P.barrier(); A.release(m2)
            P.barrier(); A.release(m)

        def attn_finalize(pO, kO, n, yb_ap, kyb, tmpf, pB, kB):
            rden, krd, osb, kosb = tmpf
            k.recip(rden[64:65, 0:n], pO[64:65, 0:n], [kO], [krd])
            k.mm(pB[0:64, 0:n], onesF[64:65, 0:64], rden[64:65, 0:n], True, True, [kc, krd], [kB])
            k.act(osb[0:64, 0:n], pO[0:64, 0:n], AF.Identity, [kO], [kosb])
            k.tt(yb_ap, osb[0:64, 0:n], pB[0:64, 0:n], ALU.mult, [kosb, kB], [kyb])

        def phase_gqa(l, last):
            m = A.mark()
            rc, krc = A.alloc([32, 64], F32); rs, krs = A.alloc([32, 64], F32)
            k.dma(rc, ropec.rearrange("p (a b) -> p a b", b=64), (), [krc])
            k.dma(rs, ropes.rearrange("p (a b) -> p a b", b=64), (), [krs])
            gqf, kgq = A.alloc([384], F32)
            k.dma(gqf, gqk[l:l + 1, :].to_broadcast([128, 384]), (), [kgq])
            gq = gqf.rearrange("p (a b) -> p a b", b=64)
            rden, krd = A.alloc([512], F32); osb, kosb = A.alloc([512], F32)
            for g in range(2):
                m2 = A.mark()
                w, kw_ = A.alloc([8, 448], BF16)
                load_w(w[:, :, 0:256], kw_, w_in[l][:, C_AQ + g * 256:C_AQ + (g + 1) * 256])
                load_w(w[:, :, 256:320], kw_, w_in[l][:, C_AK + g * 64:C_AK + (g + 1) * 64])
                load_w(w[:, :, 320:384], kw_, w_in[l][:, C_AK + g * 64:C_AK + (g + 1) * 64])
                load_w(w[:, :, 384:448], kw_, w_in[l][:, C_AV + g * 64:C_AV + (g + 1) * 64])
                QTK, kQ = A.alloc([3, S], BF16); Vg, kV = A.alloc([NT, 66], BF16)
                k.memset(Vg[:, :, 64:65], 1.0, [kV])
                sq, ksq = A.alloc([6, 64], F32); t1, kt1 = A.alloc([6, 64], F32); xr, kxr = A.alloc([6, 64], F32)
                sw, ksw = A.alloc([6, 64], F32); qr, kqr = A.alloc([384], BF16); st5, kst = A.alloc([6], F32)
                for i in range(NT):
                    ps = PS[i % 2]; pk = PK[i % 2]
                    for kk in range(8):
                        k.mm(ps[:, 0:448], hT[:, kk, i * 128:(i + 1) * 128], w[:, kk, :], kk == 0, kk == 7, [khT, kw_], [pk])
                    psv = ps[:, 0:384].rearrange("p (a b) -> p a b", b=64)
                    k.act(sq, psv, AF.Square, [pk], [ksq])
                    k.reduce(st5, sq, ALU.add, [ksq], [kst])
                    k.ts(st5, st5, 1.0 / 64, ALU.mult, [kst], [kst], s2=EPS, op1=ALU.add)
                    k.act(st5, st5, AF.Ln, [kst], [kst])
                    k.act(st5, st5, AF.Exp, [kst], [kst], scale=-0.5)
                    k.tt(t1, psv, st5.unsqueeze(2).to_broadcast([128, 6, 64]), ALU.mult, [pk, kst], [kt1])
                    k.tt(t1, t1, gq, ALU.mult, [kt1, kgq], [kt1])
                    k.copy(Vg[:, i, 0:64], ps[:, 384:448], [pk], [kV], eng="dve")
                    if i >= NCT:
                        lt = i - NCT
                        cb_ = rc[:, lt, :].unsqueeze(1).to_broadcast([128, 6, 64])
                        k.tt(xr, t1, cb_, ALU.mult, [kt1, krc], [kxr])
                        t1v = t1.rearrange("p h (a q e) -> p h a q e", a=2, q=2)
                        swv = sw.rearrange("p h (a q e) -> p h a q e", a=2, q=2)
                        rsv = rs[:, lt, :].rearrange("p (a q e) -> p a q e", a=2, q=2)
                        for qd in range(2):
                            k.tt(swv[:, :, :, qd, :], t1v[:, :, :, 1 - qd, :],
                                 rsv[:, :, qd, :].unsqueeze(1).to_broadcast([128, 6, 2, 16]), ALU.mult, [kt1, krs], [ksw])
                        k.tt(qr.rearrange("p (a b) -> p a b", b=64), xr, sw, ALU.add, [kxr, ksw], [kqr])
                    else:
                        k.copy(qr.rearrange("p (a b) -> p a b", b=64), t1, [kt1], [kqr])
                    pt = PS[2 + i % 2]; pkt = PK[2 + i % 2]
                    ptb = pt[:].bitcast(BF16)
                    for q in range(3):
                        k.tr(ptb[:, q * 128:(q + 1) * 128], qr[:, q * 128:(q + 1) * 128], identB, [kqr, kib], [pkt])
                    k.act(QTK[:, :, i * 128:(i + 1) * 128], ptb[:, 0:384].rearrange("p (a b) -> p a b", b=128), AF.Identity, [pkt], [kQ])
                PT = [A.alloc([512], BF16) for _ in range(3)]
                yb, kyb = A.alloc([512], BF16)
                jobs = []
                if not last:
                    for qh in range(4):
                        jobs.append((qh, 0, TC, list(range(NCT))))
                for qh in range(4):
                    for qc in range(TL // 512):
                        jobs.append((qh, TC + qc * 512, 512, list(range(NT))))
                it = 0
                for ji, (qh, q0, qn, kts) in enumerate(jobs):
                    j = qh // 2; hb = 64 * (qh % 2)
                    pO, kO = PS[4 + ji % 2], PK[4 + ji % 2]
                    for ki, kt in enumerate(kts):
                        pS, kS = PS[it % 3], PK[it % 3]
                        ptile = PT[it % 3]
                        it += 1
                        k.mm(pS[:, 0:qn], QTK[hb:hb + 64, 2, kt * 128:(kt + 1) * 128], QTK[hb:hb + 64, j, q0:q0 + qn], True, True, [kQ], [kS])
                        k.act(ptile[0][:, 0:qn], pS[:, 0:qn], AF.Exp, [kS], [ptile[1]], scale=0.125)
                        k.mm(pO[0:65, 0:qn], Vg[:, kt, 0:65], ptile[0][:, 0:qn], ki == 0, ki == len(kts) - 1, [kV, ptile[1]], [kO])
                    attn_finalize(pO, kO, qn, yb[0:64, 0:qn], kyb, (rden, krd, osb, kosb), PS[6 + ji % 2], PK[6 + ji % 2])
                    hd = 4 * g + qh
                    k.dma(YT[1][hd * 64:(hd + 1) * 64, q0:q0 + qn], yb[0:64, 0:qn], [kyb], ["YT1"])
                P.barrier(); A.release(m2)
            P.barrier(); A.release(m)

        def phase_na(l, last):
            m = A.mark()
            rden, krd = A.alloc([512], F32); osb, kosb = A.alloc([512], F32)
            for hp in range(4):
                m2 = A.mark()
                wq, kwq = A.alloc([8, 128], BF16); wk_, kwk = A.alloc([8, 128], BF16); wv, kwv = A.alloc([8, 128], BF16)
                load_w(wq, kwq, w_in[l][:, C_NQ + hp * 128:C_NQ + (hp + 1) * 128])
                load_w(wk_, kwk, w_in[l][:, C_NK + hp * 128:C_NK + (hp + 1) * 128])
                load_w(wv, kwv, w_in[l][:, C_NV + hp * 128:C_NV + (hp + 1) * 128])
                Tb, kTb = A.alloc([2, 7, 128], F32)
                k.dma(Tb, rpbT[l, hp].rearrange("p (a b c) -> p a b c", a=2, b=7), (), [kTb])
                QT, kQT = A.alloc([S], BF16); KT, kKT = A.alloc([S], BF16); Vn, kVn = A.alloc([NT, 2, 66], BF16)
                k.memset(Vn[:, :, :, 64:65], 1.0, [kVn])
                for ci, (t0, tn) in enumerate(TOKCH):
                    for (wt, kwt, dst, kd, sc, pi) in ((wq, kwq, QT, kQT, 0.125, 0), (wk_, kwk, KT, kKT, 1.0, 1)):
                        ps = PS[pi + 2 * (ci % 2)]; pk = PK[pi + 2 * (ci % 2)]
                        for kk in range(8):
                            k.mm(ps[:, 0:tn], wt[:, kk, :], hT[:, kk, t0:t0 + tn], kk == 0, kk == 7, [kwt, khT], [pk])
                        k.act(dst[:, t0:t0 + tn], ps[:, 0:tn], AF.Identity, [pk], [kd], scale=sc)
                for i in range(NT):
                    ps = PS[4 + i % 2]; pk = PK[4 + i % 2]
                    for kk in range(8):
                        k.mm(ps[:, 0:128], hT[:, kk, i * 128:(i + 1) * 128], wv[:, kk, :], kk == 0, kk == 7, [khT, kwv], [pk])
                    k.copy(Vn[:, i, :, 0:64], ps[:, 0:128].rearrange("p (a b) -> p a b", b=64), [pk], [kVn])
                sbs = [A.alloc([640], F32) for _ in range(2)]
                PT = [A.alloc([896], BF16) for _ in range(2)]
                ynb, kyn = A.alloc([2, TL], BF16, parts=64)
                ycb, kyc = A.alloc([2, TC], BF16, parts=64)
                it = 0
                if not last:
                    for hh in range(2):
                        hb = 64 * hh
                        pS, kS = PS[0], PK[0]; pO, kO = PS[4 + hh], PK[4 + hh]
                        for kt in range(NCT):
                            k.mm(pS[:, kt * 256:(kt + 1) * 256], KT[hb:hb + 64, kt * 128:(kt + 1) * 128], QT[hb:hb + 64, 0:TC], True, True, [kKT, kQT], [kS])
                        ptile = PT[it % 2]; it += 1
                        k.act(ptile[0][:, 0:512], pS[:, 0:512], AF.Exp, [kS], [ptile[1]])
                        for kt in range(NCT):
                            k.mm(pO[0:65, 0:TC], Vn[:, kt, hh, 0:65], ptile[0][:, kt * 256:(kt + 1) * 256], kt == 0, kt == NCT - 1, [kVn, ptile[1]], [kO])
                        attn_finalize(pO, kO, TC, ycb[:, hh, :], kyc, (rden, krd, osb, kosb), PS[6 + hh], PK[6 + hh])
                    for hh in range(2):
                        hd = 2 * hp + hh
                        k.dma(YT[2][hd * 64:(hd + 1) * 64, 0:TC], ycb[:, hh, :], [kyc], ["YT2"])
                for rb in range(32):
                    q0 = TC + rb * 128
                    r0a, r0b = r0_of(2 * rb), r0_of(2 * rb + 1)
                    tiles = list(range(r0a // 2, (r0b + 7) // 2 + 1))
                    nw = len(tiles)
                    di0 = (2 * tiles[0] - 2 * rb + 6) // 2
                    assert 0 <= di0 and di0 + nw <= 7 and nw <= 5
                    for hh in range(2):
                        hb = 64 * hh
                        ji = rb * 2 + hh
                        pSa, kSa = PS[2 * (ji % 2)], PK[2 * (ji % 2)]
                        pSb, kSb = PS[2 * (ji % 2) + 1], PK[2 * (ji % 2) + 1]
                        pO, kO = PS[4 + ji % 2], PK[4 + ji % 2]
                        sb_, ptile = sbs[ji % 2], PT[ji % 2]
                        def sdst(jw):
                            return (pSa, kSa, jw * 128) if jw < 4 else (pSb, kSb, (jw - 4) * 128)
                        for jw, t in enumerate(tiles):
                            pp, kp, co = sdst(jw)
                            kt = NCT + t
                            k.mm(pp[:, co:co + 128], KT[hb:hb + 64, kt * 128:(kt + 1) * 128], QT[hb:hb + 64, q0:q0 + 128], True, True, [kKT, kQT], [kp])
                        for kt in range(NCT):
                            k.mm(pSb[:, 128 + kt * 128:256 + kt * 128], KT[hb:hb + 64, kt * 128:(kt + 1) * 128], QT[hb:hb + 64, q0:q0 + 128], True, True, [kKT, kQT], [kSb])
                        n4 = min(nw, 4)
                        k.tt(sb_[0][:, 0:n4 * 128], pSa[:, 0:n4 * 128], Tb[:, hh, di0:di0 + n4, :].rearrange("p a b -> p (a b)"), ALU.add, [kSa, kTb], [sb_[1]])
                        if nw > 4:
                            k.tt(sb_[0][:, 512:640], pSb[:, 0:128], Tb[:, hh, di0 + 4, :], ALU.add, [kSb, kTb], [sb_[1]])
                        k.act(ptile[0][:, 0:nw * 128], sb_[0][:, 0:nw * 128], AF.Exp, [sb_[1]], [ptile[1]])
                        k.act(ptile[0][:, 640:896], pSb[:, 128:384], AF.Exp, [kSb], [ptile[1]])
                        for jw, t in enumerate(tiles):
                            for a in range(2):
                                for b in range(2):
                                    kr = 2 * t + a; qrow = 2 * rb + b
                                    if not (r0_of(qrow) <= kr <= r0_of(qrow) + 7):
                                        k.memset(ptile[0][64 * a:64 * a + 64, jw * 128 + 64 * b:jw * 128 + 64 * b + 64], 0.0, [ptile[1]])
                        for jw, t in enumerate(tiles):
                            k.mm(pO[0:65, 0:128], Vn[:, NCT + t, hh, 0:65], ptile[0][:, jw * 128:(jw + 1) * 128], jw == 0, False, [kVn, ptile[1]], [kO])
                        for kt in range(NCT):
                            k.mm(pO[0:65, 0:128], Vn[:, kt, hh, 0:65], ptile[0][:, 640 + kt * 128:768 + kt * 128], False, kt == NCT - 1, [kVn, ptile[1]], [kO])
                        attn_finalize(pO, kO, 128, ynb[:, hh, rb * 128:(rb + 1) * 128], kyn, (rden, krd, osb, kosb), PS[6 + ji % 2], PK[6 + ji % 2])
                for hh in range(2):
                    hd = 2 * hp + hh
                    k.dma(YT[2][hd * 64:(hd + 1) * 64, TC:S], ynb[:, hh, :], [kyn], ["YT2"])
                P.barrier(); A.release(m2)
            P.barrier(); A.release(m)

        def phase_merge(l):
            m = A.mark()
            wz = [A.alloc([8, 3, 128], BF16) for _ in range(2)]
            wb = [A.alloc([3, 4, 128], BF16) for _ in range(2)]
            yt = [A.alloc([3, 4, 512], BF16) for _ in range(2)]
            sg = [A.alloc([512], F32) for _ in range(2)]
            acc, kacc = A.alloc([512], F32); tmp, ktmp = A.alloc([512], F32)
            mo = [A.alloc([512], BF16) for _ in range(2)]
            it = 0
            for ct in range(8):
                b = ct % 2
                for n in range(3):
                    load_w(wz[b][0][:, :, n, :], wz[b][1], w_in[l][:, C_ZG + n * D + ct * 128:C_ZG + n * D + (ct + 1) * 128])
                    load_w(wb[b][0][:, n, :, :], wb[b][1], w_branch[l, n][:, ct * 128:(ct + 1) * 128])
                for ci, (t0, tn) in enumerate(TOKCH):
                    yb_ = yt[it % 2]; mo_ = mo[it % 2]; it += 1
                    for n in range(3):
                        k.dma(yb_[0][:, n, :, 0:tn], YT[n][:, t0:t0 + tn].rearrange("(k p) c -> p k c", p=128), ["YT%d" % n], [yb_[1]])
                    for n in range(3):
                        pg, kg = PS[2 * (n % 2)], PK[2 * (n % 2)]
                        pp, kp = PS[2 * (n % 2) + 1], PK[2 * (n % 2) + 1]
                        for kk in range(8):
                            k.mm(pg[:, 0:tn], wz[b][0][:, kk, n, :], hT[:, kk, t0:t0 + tn], kk == 0, kk == 7, [wz[b][1], khT], [kg])
                        for kk in range(4):
                            k.mm(pp[:, 0:tn], wb[b][0][:, n, kk, :], yb_[0][:, n, kk, 0:tn], kk == 0, kk == 3, [wb[b][1], yb_[1]], [kp])
                        sg_ = sg[n % 2]
                        k.act(sg_[0][:, 0:tn], pg[:, 0:tn], AF.Sigmoid, [kg], [sg_[1]])
                        if n == 0:
                            k.tt(acc[:, 0:tn], sg_[0][:, 0:tn], pp[:, 0:tn], ALU.mult, [sg_[1], kp], [kacc])
                        else:
                            k.tt(tmp[:, 0:tn], sg_[0][:, 0:tn], pp[:, 0:tn], ALU.mult, [sg_[1], kp], [ktmp])
                            if n == 1:
                                k.tt(acc[:, 0:tn], acc[:, 0:tn], tmp[:, 0:tn], ALU.add, [kacc, ktmp], [kacc])
                            else:
                                k.tt(mo_[0][:, 0:tn], acc[:, 0:tn], tmp[:, 0:tn], ALU.add, [kacc, ktmp], [mo_[1]])
                    k.dma(MTm[ct * 128:(ct + 1) * 128, t0:t0 + tn], mo_[0][:, 0:tn], [mo_[1]], ["MTm"])
            P.barrier(); A.release(m)

        def phase_proj_res(MT, mtkey, nk, wsrc, which, tiles):
            m = A.mark()
            wd, kwd = A.alloc([nk, D], BF16)
            for kk0 in range(0, nk, 8):
                kn = min(8, nk - kk0)
                load_w(wd[:, kk0:kk0 + kn, :], kwd, wsrc[kk0 * 128:(kk0 + kn) * 128, :])
            mt = [A.alloc([nk, 128], BF16) for _ in range(2)]
            xt = [A.alloc([D], F32) for _ in range(2)]
            tt_ = [A.alloc([D], F32) for _ in range(2)]
            junk, kj = A.alloc([512], F32)
            ssv = [A.alloc([4], F32) for _ in range(2)]
            for ii, i in enumerate(tiles):
                b = ii % 2
                r = 1 if i < NCT else 0
                k.dma(mt[b][0], MT[:, i * 128:(i + 1) * 128].rearrange("(k p) c -> p k c", p=128), [mtkey], [mt[b][1]])
                k.dma(xt[b][0], X[i * 128:(i + 1) * 128, :], ["X.%d" % i], [xt[b][1]])
                ph = [(PS[4 * b + hf], PK[4 * b + hf]) for hf in range(2)]
                for hf in range(2):
                    for kk in range(nk):
                        k.mm(ph[hf][0][:], mt[b][0][:, kk, :], wd[:, kk, hf * 512:(hf + 1) * 512], kk == 0, kk == nk - 1, [mt[b][1], kwd], [ph[hf][1]])
                sv, ksv = ssv[b]
                for hf in range(2):
                    k.act(junk, ph[hf][0][:], AF.Square, [ph[hf][1]], [kj, ksv], accum=sv[:, hf:hf + 1])
                k.tt(sv[:, 2:3], sv[:, 0:1], sv[:, 1:2], ALU.add, [ksv], [ksv])
                k.ts(sv[:, 2:3], sv[:, 2:3], 1.0 / D, ALU.mult, [ksv], [ksv], s2=EPS, op1=ALU.add)
                k.act(sv[:, 3:4], sv[:, 2:3], AF.Ln, [ksv], [ksv])
                k.act(sv[:, 3:4], sv[:, 3:4], AF.Exp, [ksv], [ksv], scale=-0.5)
                for hf in range(2):
                    cs = slice(hf * 512, (hf + 1) * 512)
                    k.stt(tt_[b][0][:, cs], ph[hf][0][:], sv[:, 3:4], Gbc[:, r, which, cs], ALU.mult, ALU.mult, [ph[hf][1], ksv, kG], [tt_[b][1]])
                k.tt(tt_[b][0], tt_[b][0], xt[b][0], ALU.add, [tt_[b][1], xt[b][1]], [tt_[b][1]], eng="pool")
                k.dma(X[i * 128:(i + 1) * 128, :], tt_[b][0], [tt_[b][1]], ["X.%d" % i, "X"])
            P.barrier(); A.release(m)

        def phase_ffn_up(l, lo_tok):
            m = A.mark()
            LB = S + 4
            cw, kcw = A.alloc([2 * NFT, 3], F32); cbb, kcb = A.alloc([2 * NFT], F32)
            k.dma(cw, cwfm[l].rearrange("p (a b) -> p a b", b=3), (), [kcw])
            k.dma(cbb, cbfm[l], (), [kcb])
            U = [A.alloc([LB], F32) for _ in range(2)]
            T = [A.alloc([LB], F32) for _ in range(2)]
            mb, kmb = A.alloc([LB], BF16)
            w = [A.alloc([8, 2, 128], BF16) for _ in range(2)]
            for z in range(2):
                k.memset(U[z][0], 0.0, [U[z][1]])
            chunks = []
            if lo_tok == 0:
                chunks.append((0, TC, 1))
            for qc in range(TL // 512):
                chunks.append((TC + qc * 512, 512, 3 + TC + qc * 512))
            lo_c = 1 if lo_tok == 0 else 3 + TC
            hi_c = LB - 1
            it = 0
            for j in range(NFT):
                b = j % 2
                load_w(w[b][0][:, :, 0, :], w[b][1], w_up[l][:, j * 128:(j + 1) * 128])
                load_w(w[b][0][:, :, 1, :], w[b][1], w_up[l][:, FFN + j * 128:FFN + (j + 1) * 128])
                for (t0, tn, c0) in chunks:
                    for z in range(2):
                        ps, pk = PS[it % 4], PK[it % 4]; it += 1
                        for kk in range(8):
                            k.mm(ps[:, 0:tn], w[b][0][:, kk, z, :], hT[:, kk, t0:t0 + tn], kk == 0, kk == 7, [w[b][1], khT], [pk])
                        k.act(U[z][0][:, c0:c0 + tn], ps[:, 0:tn], AF.Identity, [pk], [U[z][1]])
                n = hi_c - lo_c
                for z in range(2):
                    ch = z * NFT + j
                    k.act(T[z][0][:, lo_c:hi_c], U[z][0][:, lo_c:hi_c], AF.Identity, [U[z][1], kcw, kcb], [T[z][1]],
                          scale=cw[:, ch, 1:2], bias=cbb[:, ch:ch + 1])
                    k.stt(T[z][0][:, lo_c:hi_c], U[z][0][:, lo_c - 1:hi_c - 1], cw[:, ch, 0:1], T[z][0][:, lo_c:hi_c], ALU.mult, ALU.add,
                          [U[z][1], kcw, T[z][1]], [T[z][1]])
                    k.stt(T[z][0][:, lo_c:hi_c], U[z][0][:, lo_c + 1:hi_c + 1], cw[:, ch, 2:3], T[z][0][:, lo_c:hi_c], ALU.mult, ALU.add,
                          [U[z][1], kcw, T[z][1]], [T[z][1]])
                k.act(T[1][0][:, lo_c:hi_c], T[1][0][:, lo_c:hi_c], AF.Silu, [T[1][1]], [T[1][1]])
                k.tt(mb[:, lo_c:hi_c], T[0][0][:, lo_c:hi_c], T[1][0][:, lo_c:hi_c], ALU.mult, [T[0][1], T[1][1]], [kmb])
                if lo_tok == 0:
                    k.dma(MTf[j * 128:(j + 1) * 128, 0:TC], mb[:, 1:1 + TC], [kmb], ["MTf"])
                k.dma(MTf[j * 128:(j + 1) * 128, TC:S], mb[:, 3 + TC:3 + S], [kmb], ["MTf"])
            P.barrier(); A.release(m)

        for l in range(n_layers):
            last = (l == DEPTH - 1) or force_last
            P.new_epoch()
            if on("mod"): phase_mod(l)
            if on("norm"): phase_norm(0)
            if on("mlstm"): phase_mlstm(l)
            if on("gqa"): phase_gqa(l, last)
            if on("na"): phase_na(l, last)
            if on("merge"): phase_merge(l)
            tiles = list(range(NCT if last else 0, NT))
            if on("res1"): phase_proj_res(MTm, "MTm", 8, w_out[l], 0, tiles)
            if on("norm2"): phase_norm(1)
            if on("ffn"): phase_ffn_up(l, TC if last else 0)
            if on("res2"): phase_proj_res(MTf, "MTf", NFT, w_down[l], 1, tiles)
        P.barrier()
        for q in range(8):
            k.dma(yout[q * 512:(q + 1) * 512, :], X[TC + q * 512:TC + (q + 1) * 512, :], ["X"], ["yout%d" % q])
        P.emit()
        print("ops emitted:", P.nops, {e: len(v) for e, v in P.ops.items()})
    return nc


def _consts():
    ident = np.eye(128, dtype=np.float32)
    ones = np.ones((128, 128), np.float32)
    s = np.arange(128)[:, None]; t = np.arange(128)[None, :]
    mf = (s <= t).astype(np.float32); mb = (s >= t).astype(np.float32)
    cst = np.concatenate([ident, ones, mf, mb, np.zeros((128, 128), np.float32)], axis=1)
    esel = np.zeros((4, 4, 128), np.float32)
    for h in range(4):
        esel[h, h, :] = 1.0
    half = 32
    tpos = np.arange(TL)
    inv = (10000.0 ** (-np.arange(0, half, 2, dtype=np.float32) / half)).astype(np.float32)
    ang_r = (tpos // GRID).astype(np.float32)[:, None] * inv
    ang_c = (tpos % GRID).astype(np.float32)[:, None] * inv
    ang = np.concatenate([ang_r, ang_r, ang_c, ang_c], axis=-1)
    cos = np.cos(ang).astype(np.float32); sin = np.sin(ang).astype(np.float32)
    sgn = np.concatenate([-np.ones(16), np.ones(16), -np.ones(16), np.ones(16)]).astype(np.float32)
    sins = sin * sgn[None, :]
    ropec = cos.reshape(32, 128, 64).transpose(1, 0, 2).reshape(128, 32 * 64)
    ropes = sins.reshape(32, 128, 64).transpose(1, 0, 2).reshape(128, 32 * 64)
    return cst, esel.reshape(4, 512), np.ascontiguousarray(ropec), np.ascontiguousarray(ropes)


def _rpb_tiles(na_rpb):
    L = na_rpb.shape[0]
    cols = np.arange(GRID)
    c0 = np.clip(cols - 8, 0, GRID - 16)
    kc = np.arange(GRID)[:, None]; qc = np.arange(GRID)[None, :]
    inwin = (kc >= c0[None, :]) & (kc < c0[None, :] + 16)
    dc = np.clip(kc - qc + 15, 0, 30)
    out = np.full((L, 4, 128, 2, 7, 128), NEG, np.float32)
    for di in range(7):
        delta = 2 * di - 6
        for a in range(2):
            for b in range(2):
                dr = delta + a - b + 7
                if not (0 <= dr <= 14):
                    continue
                vals = na_rpb[:, :, dr, :][:, :, dc]
                vals = np.where(inwin[None, None], vals, np.float32(NEG))
                v = vals.reshape(L, 4, 2, GRID, GRID).transpose(0, 1, 3, 2, 4)
                out[:, :, 64 * a:64 * a + 64, :, di, 64 * b:64 * b + 64] = v
    return out.reshape(L, 4, 128, 2 * 7 * 128)


_NC_CACHE = {}


def prep_inputs(inp):
    f = lambda a: np.ascontiguousarray(np.asarray(a, dtype=np.float32))
    cst, esel, ropec, ropes = _consts()
    L = DEPTH
    gfm = np.stack([f(inp["g_pre_mix"]).reshape(L, 8, 128), f(inp["g_pre_ffn"]).reshape(L, 8, 128)], axis=1)
    gfm = np.ascontiguousarray(gfm.transpose(0, 3, 1, 2))
    gpost = np.ascontiguousarray(np.stack([f(inp["g_post_mix"]), f(inp["g_post_ffn"])], axis=1))
    bgate = np.ascontiguousarray(f(inp["b_ml_gates"]).reshape(L, 4, 4).transpose(0, 2, 1))
    gq = f(inp["g_q"]); gk = f(inp["g_k"])
    gqk = np.ascontiguousarray(np.concatenate([gq, gq, gq, gq, gk, gk], axis=1))
    cw = f(inp["conv_w"])
    cwfm = np.ascontiguousarray(cw.reshape(L, 3, 2 * NFT, 128).transpose(0, 3, 2, 1).reshape(L, 128, 2 * NFT * 3))
    cbfm = np.ascontiguousarray(f(inp["conv_b"]).reshape(L, 2 * NFT, 128).transpose(0, 2, 1))
    shared = {
        "w_mod": f(inp["w_mod"]), "b_mod": f(inp["b_mod"]), "gfm": gfm, "gpost": gpost, "w_in": f(inp["w_in"]),
        "bgate": bgate, "gml": f(inp["g_ml_out"]), "gqk": gqk, "rpbT": _rpb_tiles(f(inp["na_rpb"])),
        "w_branch": f(inp["w_branch"]), "w_out": f(inp["w_out"]), "w_up": f(inp["w_up"]), "w_down": f(inp["w_down"]),
        "cwfm": cwfm, "cbfm": cbfm, "ropec": ropec, "ropes": ropes, "cst": cst, "esel": esel,
    }
    x = f(inp["x"]); c = f(inp["c"]); ctx = f(inp["ctx"]); cc = f(inp["c_ctx"])
    maps = []
    for b in range(x.shape[0]):
        cv = np.stack([c[b], cc], axis=0)
        cT = np.ascontiguousarray(cv.reshape(2, 8, 128).transpose(2, 0, 1))
        mp = dict(shared)
        mp.update({"x_in": x[b], "ctx_in": ctx[b], "cT": cT})
        maps.append(mp)
    return maps


def kernel(**inputs):
    maps = prep_inputs(inputs)
    if "nc" not in _NC_CACHE:
        _NC_CACHE["nc"] = build()
    nc = _NC_CACHE["nc"]
    res = run_bass_kernel_spmd(nc, maps, core_ids=list(range(len(maps))))
    return np.stack([np.asarray(r["y"], dtype=np.float32) for r in res.results], axis=0)
```

```python
import numpy as np
from contextlib import ExitStack
import concourse.bass as bass
import concourse.mybir as mybir
from concourse.bass_utils import run_bass_kernel_spmd

F32 = mybir.dt.float32
BF16 = mybir.dt.bfloat16
AF = mybir.ActivationFunctionType
ALU = mybir.AluOpType
AX = mybir.AxisListType

D = 1024; KC = 8; TL = 4096; TC = 256; S = TL + TC; NT = S // 128; NCT = TC // 128
DEPTH = 4; FFN = 2816; NFT = FFN // 128; D_IN = 7440
GRID = 64; EPS = 1e-6
C_MQ, C_MK, C_MV, C_MO, C_MG = 0, 512, 1024, 1536, 2048
C_AQ, C_AK, C_AV = 2064, 2576, 2704
C_NQ, C_NK, C_NV, C_ZG = 2832, 3344, 3856, 4368
ENGS = ("pe", "act", "dve", "pool", "sp")
N_DMA_SEMS = 14
NEG = -30000.0


class Prog:
    def __init__(self, nc):
        self.nc = nc
        self.ops = {e: [] for e in ENGS}
        self.cnt = {e: 0 for e in ENGS}
        self.res = {}
        self.seen = {e: {} for e in ENGS}
        self.dma_i = 0
        self.dma_hist = {}
        self.nops = 0
        self.ep = 0
        self.label = "init"
        self.name2label = {}
        self.allsems = set()
        self.final = []

    def sn(self, base):
        n = "%s_e%d" % (base, self.ep)
        self.allsems.add(n)
        return n

    def new_epoch(self):
        self.barrier()
        self.ep += 1
        self.cnt = {e: 0 for e in ENGS}
        self.dma_hist = {}
        self.seen = {e: {} for e in ENGS}

    def _deps(self, reads, writes):
        ev = set()
        for r in reads:
            st = self.res.get(r)
            if st and st[0]:
                ev.add(st[0])
        for w in writes:
            st = self.res.get(w)
            if st:
                if st[0]:
                    ev.add(st[0])
                ev.update(st[1])
        return ev

    def _commit(self, event, reads, writes):
        for r in reads:
            st = self.res.setdefault(r, [None, []])
            st[1].append(event)
        for w in writes:
            self.res[w] = [event, []]

    def _filter(self, eng, evs, own_sem=None):
        seen = self.seen[eng]
        best = {}
        for (s, v) in evs:
            if s == own_sem and eng == "pe":
                continue
            if seen.get(s, 0) >= v:
                continue
            if best.get(s, 0) < v:
                best[s] = v
        for s, v in best.items():
            seen[s] = v
        return list(best.items())

    def op(self, eng, fn, reads=(), writes=()):
        writes = tuple(writes) + tuple(r for r in reads if r.startswith("ps"))
        reads = tuple(r for r in reads if not r.startswith("ps"))
        evs = self._deps(reads, writes)
        own = self.sn("c_" + eng)
        waits = self._filter(eng, evs, own)
        self.cnt[eng] += 1
        event = (own, self.cnt[eng])
        self.ops[eng].append((waits, fn, (own, 1), self.label))
        self._commit(event, reads, writes)
        self.nops += 1

    def dma(self, eng, fn, reads=(), writes=()):
        reads = tuple(reads); writes = tuple(writes)
        evs = self._deps(reads, writes)
        j = self.dma_i % N_DMA_SEMS
        sem = self.sn("d_%d" % j)
        prev = self.dma_hist.get(j, 0)
        if prev:
            evs.add((sem, prev))
        val = prev + 16
        self.dma_hist[j] = val
        self.dma_i += 1
        waits = self._filter(eng, evs, None)
        self.ops[eng].append((waits, fn, (sem, 16), self.label))
        self._commit((sem, val), reads, writes)
        self.nops += 1

    def barrier(self):
        evs = [(self.sn("c_" + e), self.cnt[e]) for e in ENGS if self.cnt[e]]
        evs += [(self.sn("d_%d" % j), v) for j, v in self.dma_hist.items()]
        self.final = list(evs)
        for e in ENGS:
            waits = self._filter(e, evs, None)
            if waits:
                self.ops[e].append((waits, None, None, self.label))
        self.res = {}

    def emit(self):
        nc = self.nc
        with ExitStack() as st:
            sems = {}
            for n in sorted(self.allsems):
                sems[n] = st.enter_context(nc.semaphore(n))
            final = [(self.sn("c_" + e), self.cnt[e]) for e in ENGS if self.cnt[e] and e != "sp"]
            final += [(self.sn("d_%d" % j), v) for j, v in self.dma_hist.items()]
            block = st.enter_context(nc.Block())

            def run(engname):
                def body(eng):
                    for waits, fn, inc, lab in self.ops[engname]:
                        for (ws, wv) in waits:
                            eng.wait_ge(sems[ws], wv)
                        if fn is not None:
                            ins = fn(eng)
                            ins.then_inc(sems[inc[0]], inc[1])
                            try:
                                self.name2label[ins.ins.name] = lab
                            except Exception:
                                pass
                    if engname == "sp":
                        for (ws, wv) in final:
                            eng.wait_ge(sems[ws], wv)
                return body

            block.tensor(run("pe"))
            block.scalar(run("act"))
            block.vector(run("dve"))
            block.gpsimd(run("pool"))
            block.sync(run("sp"))


class Arena:
    def __init__(self, ap, nwords):
        self.ap = ap; self.n = nwords; self.off = 0; self.uid = 0

    def mark(self):
        return self.off

    def release(self, m):
        self.off = m

    def alloc(self, shape, dt, parts=128):
        n = int(np.prod(shape))
        words = n if dt == F32 else (n + 1) // 2
        words = (words + 7) // 8 * 8
        assert self.off + words <= self.n, ("SBUF arena overflow", self.off, words, self.n)
        a = self.ap[0:parts, self.off:self.off + words]
        self.off += words
        if dt != F32:
            a = a.bitcast(dt)
        a = a[:, 0:n]
        if len(shape) > 1:
            names = [chr(ord('a') + i) for i in range(len(shape))]
            kw = {names[i]: int(shape[i]) for i in range(len(shape))}
            a = a.rearrange("p (%s) -> p %s" % (" ".join(names), " ".join(names)), **kw)
        self.uid += 1
        return a, "b%d" % self.uid


class K:
    def __init__(self, P):
        self.P = P

    def mm(self, out, lhsT, rhs, start, stop, r, w):
        self.P.op("pe", lambda e: e.matmul(out, lhsT=lhsT, rhs=rhs, start=start, stop=stop), r, w)

    def tr(self, out, in_, ident, r, w):
        self.P.op("pe", lambda e: e.transpose(out, in_, ident), r, w)

    def act(self, out, in_, func, r, w, scale=None, bias=None, accum=None, eng="act"):
        kw = {}
        if scale is not None: kw["scale"] = scale
        if bias is not None: kw["bias"] = bias
        if accum is not None: kw["accum_out"] = accum
        self.P.op("act", lambda e: e.activation(out=out, in_=in_, func=func, **kw), r, w)

    def tt(self, out, in0, in1, op, r, w, eng="dve"):
        self.P.op(eng, lambda e: e.tensor_tensor(out=out, in0=in0, in1=in1, op=op), r, w)

    def ts(self, out, in0, s1, op0, r, w, s2=None, op1=None, eng="dve"):
        if op1 is None:
            self.P.op(eng, lambda e: e.tensor_scalar(out=out, in0=in0, scalar1=s1, scalar2=None, op0=op0), r, w)
        else:
            self.P.op(eng, lambda e: e.tensor_scalar(out=out, in0=in0, scalar1=s1, scalar2=s2, op0=op0, op1=op1), r, w)

    def stt(self, out, in0, scalar, in1, op0, op1, r, w):
        self.P.op("dve", lambda e: e.scalar_tensor_tensor(out=out, in0=in0, scalar=scalar, in1=in1, op0=op0, op1=op1), r, w)

    def copy(self, out, in_, r, w, eng="dve"):
        self.P.op(eng, lambda e: e.tensor_copy(out=out, in_=in_), r, w)

    def recip(self, out, in_, r, w):
        self.P.op("dve", lambda e: e.reciprocal(out=out, in_=in_), r, w)

    def reduce(self, out, in_, op, r, w):
        self.P.op("dve", lambda e: e.tensor_reduce(out=out, in_=in_, axis=AX.X, op=op), r, w)

    def scan(self, out, d0, d1, init, op0, op1, r, w):
        self.P.op("dve", lambda e: e.tensor_tensor_scan(out=out, data0=d0, data1=d1, initial=init, op0=op0, op1=op1), r, w)

    def ttr(self, out, in0, in1, accum, r, w):
        self.P.op("dve", lambda e: e.tensor_tensor_reduce(out=out, in0=in0, in1=in1, scale=1.0, scalar=0.0,
                                                          op0=ALU.mult, op1=ALU.add, accum_out=accum), r, w)

    def memset(self, ap, v, w, eng="pool"):
        self.P.op(eng, lambda e: e.memset(ap, v), (), w)

    def dma(self, out, in_, r, w, eng="sp"):
        self.P.dma(eng, lambda e: e.dma_start(out=out, in_=in_), r, w)


def r0_of(r):
    return min(max(r - 4, 0), GRID - 8)


def build(n_layers=DEPTH, debug=False, arena_words=51200, phases=None, force_last=False):
    nc = bass.Bass("TRN2", target_bir_lowering=False)

    def din(name, shape, dt=F32):
        return nc.dram_tensor(name, list(shape), dt, kind="ExternalInput").ap()

    x_in = din("x_in", [TL, D]); ctx_in = din("ctx_in", [TC, D])
    cT = din("cT", [128, 2, 8])
    w_mod = din("w_mod", [DEPTH, D, 6 * D]); b_mod = din("b_mod", [DEPTH, 6 * D])
    gfm = din("gfm", [DEPTH, 128, 2, 8])
    gpost = din("gpost", [DEPTH, 2, D])
    w_in = din("w_in", [DEPTH, D, D_IN])
    bgate = din("bgate", [DEPTH, 4, 4])
    gml = din("gml", [DEPTH, 512])
    gqk = din("gqk", [DEPTH, 384])
    rpbT = din("rpbT", [DEPTH, 4, 128, 2 * 7 * 128])
    w_branch = din("w_branch", [DEPTH, 3, 512, D]); w_out = din("w_out", [DEPTH, D, D])
    w_up = din("w_up", [DEPTH, D, 2 * FFN]); w_down = din("w_down", [DEPTH, FFN, D])
    cwfm = din("cwfm", [DEPTH, 128, 2 * NFT * 3]); cbfm = din("cbfm", [DEPTH, 128, 2 * NFT])
    ropec = din("ropec", [128, 32 * 64]); ropes = din("ropes", [128, 32 * 64])
    cst = din("cst", [128, 5 * 128])
    yout = nc.dram_tensor("y", [TL, D], F32, kind="ExternalOutput").ap()
    okind = "ExternalOutput" if debug else "Internal"
    X = nc.dram_tensor("Xres", [S, D], F32, kind=okind).ap()
    YT = [nc.dram_tensor("YT%d" % n, [512, S], BF16, kind=okind).ap() for n in range(3)]
    MTm = nc.dram_tensor("MTm", [D, S], BF16, kind="Internal").ap()
    MTf = nc.dram_tensor("MTf", [FFN, S], BF16, kind="Internal").ap()

    with ExitStack() as st:
        arena_t = st.enter_context(nc.sbuf_tensor("arena", [128, arena_words], F32))
        A = Arena(arena_t, arena_words)
        PS = [st.enter_context(nc.psum_tensor("ps%d" % i, [128, 512], F32)) for i in range(8)]
        PK = ["ps%d" % i for i in range(8)]
        P = Prog(nc)
        k = K(P)

        cstt, kc = A.alloc([5 * 128], F32)
        identF = cstt[:, 0:128]; onesF = cstt[:, 128:256]
        maskd = [cstt[:, 256:384], cstt[:, 384:512]]
        eselt, kes = A.alloc([4 * 128], F32, parts=4)
        identB, kib = A.alloc([128], BF16)
        srep, ksr = A.alloc([2, 8, 128], F32)
        hT, khT = A.alloc([KC, S], BF16)
        Gbc, kG = A.alloc([2, 2, D], F32)
        fmv, kfm = A.alloc([2, 4, 8], F32)
        k.dma(cstt, cst, (), [kc])
        k.copy(identB, identF, [kc], [kib])

        eselin = din("esel", [4, 4 * 128])
        k.dma(eselt, eselin, (), [kes])

        k.dma(X[0:TC, :], ctx_in, (), ["Xi"])
        for q in range(8):
            k.dma(X[TC + q * 512:TC + (q + 1) * 512, :], x_in[q * 512:(q + 1) * 512, :], (), ["Xi%d" % q])

        m0 = A.mark()
        ct_t, kct = A.alloc([2, 8], F32)
        k.dma(ct_t, cT, (), [kct])
        k.act(ct_t, ct_t, AF.Silu, [kct], [kct])
        for r in range(2):
            for kk in range(8):
                k.ts(srep[:, r, kk, :], onesF, ct_t[:, r, kk:kk + 1], ALU.mult, [kct, kc], [ksr])
        P.barrier(); A.release(m0)

        on = lambda nm: phases is None or nm in phases

        def phase_mod(l):
            m = A.mark()
            wm = [A.alloc([8, 512], F32) for _ in range(2)]
            brow = [A.alloc([512], F32, parts=1) for _ in range(2)]
            gp = [A.alloc([512], F32) for _ in range(2)]
            tmp = [A.alloc([512], F32) for _ in range(2)]
            junk, kj = A.alloc([4, 128], F32)
            gf, kgf = A.alloc([2, 8], F32)
            k.dma(gf, gfm[l], (), [kgf])
            for cb in range(12):
                b = cb % 2
                seg = cb // 2; half = cb % 2
                k.dma(wm[b][0], w_mod[l][:, cb * 512:(cb + 1) * 512].rearrange("(k p) c -> p k c", p=128), (), [wm[b][1]])
                k.dma(brow[b][0], b_mod[l:l + 1, cb * 512:(cb + 1) * 512], (), [brow[b][1]])
                if seg in (2, 5):
                    which = 0 if seg == 2 else 1
                    k.dma(gp[b][0], gpost[l, which:which + 1, half * 512:(half + 1) * 512].to_broadcast([128, 512]), (), [gp[b][1]])
                for r in range(2):
                    ps = PS[r + 2 * b]; pk = PK[r + 2 * b]
                    for kk in range(8):
                        k.mm(ps[:], srep[:, r, kk, :], wm[b][0][:, kk, :], kk == 0, False, [ksr, wm[b][1]], [pk])
                    k.mm(ps[:], onesF[0:1, :], brow[b][0], False, True, [kc, brow[b][1]], [pk])
                    if seg in (2, 5):
                        which = 0 if seg == 2 else 1
                        k.tt(Gbc[:, r, which, half * 512:(half + 1) * 512], ps[:], gp[b][0], ALU.mult, [pk, gp[b][1]], [kG])
                    else:
                        slot = {0: 1, 1: 0, 3: 3, 4: 2}[seg]
                        tb = tmp[r]
                        k.copy(tb[0], ps[:], [pk], [tb[1]])
                        k.tt(junk, tb[0].rearrange("p (a b) -> p a b", b=128), identF.unsqueeze(1).to_broadcast([128, 4, 128]),
                             ALU.mult, [tb[1], kc], [kj])
                        k.reduce(fmv[:, r, slot, half * 4:half * 4 + 4], junk, ALU.add, [kj], [kfm])
            for r in range(2):
                for (slot, gi) in ((0, 0), (2, 1)):
                    k.stt(fmv[:, r, slot, :], fmv[:, r, slot, :], 1.0, gf[:, gi, :], ALU.add, ALU.mult, [kfm, kgf], [kfm])
            P.barrier(); A.release(m)

        def phase_norm(which):
            m = A.mark()
            xt = [A.alloc([D], F32) for _ in range(2)]
            xn = [A.alloc([D], BF16) for _ in range(2)]
            junk, kj = A.alloc([D], F32)
            ss, kss = A.alloc([NT], F32)
            for i in range(NT):
                b = i % 2
                k.dma(xt[b][0], X[i * 128:(i + 1) * 128, :], ["X"], [xt[b][1]])
                k.act(junk, xt[b][0], AF.Square, [xt[b][1]], [kj, kss], accum=ss[:, i:i + 1])
            k.ts(ss, ss, 1.0 / D, ALU.mult, [kss], [kss], s2=EPS, op1=ALU.add)
            k.act(ss, ss, AF.Sqrt, [kss], [kss])
            k.recip(ss, ss, [kss], [kss])
            for i in range(NT):
                b = i % 2
                r = 1 if i < NCT else 0
                k.dma(xt[b][0], X[i * 128:(i + 1) * 128, :], ["X"], [xt[b][1]])
                k.ts(xn[b][0], xt[b][0], ss[:, i:i + 1], ALU.mult, [xt[b][1], kss], [xn[b][1]])
                ps = PS[b]; pk = PK[b]
                psb = ps[:].bitcast(BF16).rearrange("p (a b) -> p a b", a=8)
                for kk in range(8):
                    k.tr(psb[:, kk, :], xn[b][0][:, kk * 128:(kk + 1) * 128], identB, [xn[b][1], kib], [pk])
                for kk in range(8):
                    k.act(hT[:, kk, i * 128:(i + 1) * 128], psb[:, kk, :], AF.Identity, [pk, kfm], [khT],
                          scale=fmv[:, r, 2 * which, kk:kk + 1], bias=fmv[:, r, 2 * which + 1, kk:kk + 1])
            P.barrier(); A.release(m)

        def load_w(dst, key, src_cols_ap):
            k.dma(dst, src_cols_ap.rearrange("(k p) c -> p k c", p=128), (), [key], eng="pool")

        TOKCH = [(c * 512, min(512, S - c * 512)) for c in range((S + 511) // 512)]

        def phase_mlstm(l):
            m = A.mark()
            kwT = [A.alloc([NT, 4], F32) for _ in range(2)]
            thT = [A.alloc([NT, 4], F32) for _ in range(2)]
            gbc = [A.alloc([4, NT], F32) for _ in range(2)]
            m1 = A.mark()
            wg, kwg = A.alloc([8, 16], BF16)
            load_w(wg, kwg, w_in[l][:, C_MG:C_MG + 16])
            bg, kbg = A.alloc([4], F32, parts=4)
            k.dma(bg, bgate[l], (), [kbg])
            X0, k0 = A.alloc([S], F32, parts=4); X1, k1 = A.alloc([S], F32, parts=4)
            X2, k2 = A.alloc([S], F32, parts=4); X3, k3 = A.alloc([S], F32, parts=4)
            X4, k4 = A.alloc([S], F32, parts=4)
            cm, kcm = A.alloc([NT], F32, parts=4); ri, kri = A.alloc([NT], F32, parts=4)
            rr, krr = A.alloc([NT], F32, parts=4); gd, kgd = A.alloc([NT], F32, parts=4)
            for d in range(2):
                for (j, dst, kd) in ((2 * d, X0, k0), (2 * d + 1, X1, k1)):
                    for ci, (t0, tn) in enumerate(TOKCH):
                        ps = PS[ci % 2]; pk = PK[ci % 2]
                        for kk in range(8):
                            k.mm(ps[0:4, 0:tn], wg[:, kk, 4 * j:4 * j + 4], hT[:, kk, t0:t0 + tn], kk == 0, kk == 7, [kwg, khT], [pk])
                        k.act(dst[:, t0:t0 + tn], ps[0:4, 0:tn], AF.Identity, [pk, kbg], [kd], bias=bg[:, j:j + 1], scale=1.0)
                k.act(X1, X1, AF.Exp, [k1], [k1], scale=-1.0)
                k.ts(X2, X1, 2.0, ALU.add, [k1], [k2])
                k.recip(X2, X2, [k2], [k2])
                k.tt(X2, X2, X1, ALU.mult, [k2, k1], [k2])
                k.tt(X3, X2, X2, ALU.mult, [k2], [k3])
                k.ts(X4, X3, 0.2, ALU.mult, [k3], [k4], s2=1.0 / 3.0, op1=ALU.add)
                k.tt(X4, X4, X3, ALU.mult, [k4, k3], [k4])
                k.ts(X4, X4, 1.0, ALU.add, [k4], [k4], s2=-2.0, op1=ALU.mult)
                k.tt(X4, X4, X2, ALU.mult, [k4, k2], [k4])
                if d == 0:
                    k.scan(X1, X4, X4, 0.0, ALU.add, ALU.min, [k4], [k1])
                else:
                    k.scan(X1[:, 0:TC][:, ::-1], X4[:, 0:TC][:, ::-1], X4[:, 0:TC][:, ::-1], 0.0,
                           ALU.add, ALU.min, [k4], [k1])
                    k.scan(X1[:, TC:S][:, ::-1], X4[:, TC:S][:, ::-1], X4[:, TC:S][:, ::-1], X1[:, 0:1],
                           ALU.add, ALU.min, [k4, k1], [k1])
                k.tt(X2, X0, X1, ALU.subtract, [k0, k1], [k2])
                k.reduce(cm, X2.rearrange("p (c t) -> p c t", t=128), ALU.max, [k2], [kcm])
                if d == 0:
                    k.scan(ri, cm, cm, 0.0, ALU.max, ALU.max, [kcm], [kri])
                    k.memset(rr[:, 0:1], 0.0, [krr], eng="dve")
                    k.copy(rr[:, 1:NT], ri[:, 0:NT - 1], [kri], [krr])
                else:
                    k.scan(ri[:, 0:NCT][:, ::-1], cm[:, 0:NCT][:, ::-1], cm[:, 0:NCT][:, ::-1], 0.0, ALU.max, ALU.max, [kcm], [kri])
                    k.scan(ri[:, NCT:NT][:, ::-1], cm[:, NCT:NT][:, ::-1], cm[:, NCT:NT][:, ::-1], ri[:, 0:1], ALU.max, ALU.max,
                           [kcm, kri], [kri])
                    k.memset(rr[:, NCT - 1:NCT], 0.0, [krr], eng="dve")
                    k.copy(rr[:, 0:NCT - 1], ri[:, 1:NCT], [kri], [krr])
                    k.copy(rr[:, NT - 1:NT], ri[:, 0:1], [kri], [krr])
                    k.copy(rr[:, NCT:NT - 1], ri[:, NCT + 1:NT], [kri], [krr])
                rrb = rr.unsqueeze(2).to_broadcast([4, NT, 128])
                k.tt(X3.rearrange("p (c t) -> p c t", t=128), X2.rearrange("p (c t) -> p c t", t=128), rrb, ALU.subtract, [k2, krr], [k3])
                k.act(X3, X3, AF.Exp, [k3], [k3])
                k.tt(X0.rearrange("p (c t) -> p c t", t=128), X1.rearrange("p (c t) -> p c t", t=128), rrb, ALU.add, [k1, krr], [k0])
                k.act(X0, X0, AF.Exp, [k0], [k0], scale=-1.0)
                k.tt(gd, rr, ri, ALU.subtract, [krr, kri], [kgd])
                k.act(gd, gd, AF.Exp, [kgd], [kgd])
                for (src, ks, dstp) in ((X3, k3, kwT[d]), (X0, k0, thT[d])):
                    ps = PS[2]; pk = PK[2]
                    for c in range(NT):
                        k.tr(ps[:, c * 4:(c + 1) * 4], src[:, c * 128:(c + 1) * 128], identF[0:4, 0:4], [ks, kc], [pk])
                    k.copy(dstp[0], ps[:, 0:NT * 4].rearrange("p (c h) -> p c h", h=4), [pk], [dstp[1]])
                ps = PS[3]; pk = PK[3]
                for h in range(4):
                    k.mm(ps[:, h * NT:(h + 1) * NT], eselt[:, h * 128:(h + 1) * 128], gd, True, True, [kes, kgd], [pk])
                k.copy(gbc[d][0], ps[:, 0:4 * NT].rearrange("p (h c) -> p h c", h=4), [pk], [gbc[d][1]])
            P.barrier(); A.release(m1)

            gmlb, kgm = A.alloc([512], F32)
            k.dma(gmlb, gml[l:l + 1, :].to_broadcast([128, 512]), (), [kgm])
            for h in range(4 if on("ml_heads") else 0):
                m2 = A.mark()
                wq, kwq = A.alloc([8, 128], BF16); wk_, kwk = A.alloc([8, 128], BF16)
                wkvo, kwkvo = A.alloc([8, 384], BF16)
                load_w(wq, kwq, w_in[l][:, C_MQ + h * 128:C_MQ + (h + 1) * 128])
                load_w(wk_, kwk, w_in[l][:, C_MK + h * 128:C_MK + (h + 1) * 128])
                load_w(wkvo[:, :, 0:128], kwkvo, w_in[l][:, C_MK + h * 128:C_MK + (h + 1) * 128])
                load_w(wkvo[:, :, 128:256], kwkvo, w_in[l][:, C_MV + h * 128:C_MV + (h + 1) * 128])
                load_w(wkvo[:, :, 256:384], kwkvo, w_in[l][:, C_MO + h * 128:C_MO + (h + 1) * 128])
                QT, kQT = A.alloc([S], BF16); KT, kKT = A.alloc([S], BF16)
                Ktm, kKtm = A.alloc([NT, 128], BF16); Va, kVa = A.alloc([NT, 130], BF16)
                Osg, kOs = A.alloc([NT, 128], BF16); Hacc, kH = A.alloc([NT, 128], F32)
                k.memset(Va[:, :, 128:129], 1.0, [kVa])
                k.memset(Hacc, 0.0, [kH + ".%d" % c for c in range(NT)])
                for ci, (t0, tn) in enumerate(TOKCH if on("ml_fm") else []):
                    for (wt, kwt, dst, kd, sc, pi) in ((wq, kwq, QT, kQT, 1.0, 0), (wk_, kwk, KT, kKT, 128.0 ** -0.5, 1)):
                        ps = PS[pi + 2 * (ci % 2)]; pk = PK[pi + 2 * (ci % 2)]
                        for kk in range(8):
                            k.mm(ps[:, 0:tn], wt[:, kk, :], hT[:, kk, t0:t0 + tn], kk == 0, kk == 7, [kwt, khT], [pk])
                        k.act(dst[:, t0:t0 + tn], ps[:, 0:tn], AF.Identity, [pk], [kd], scale=sc)
                for i in range(NT if on("ml_tm") else 0):
                    ps = PS[4 + i % 2]; pk = PK[4 + i % 2]
                    for kk in range(8):
                        k.mm(ps[:, 0:384], hT[:, kk, i * 128:(i + 1) * 128], wkvo[:, kk, :], kk == 0, kk == 7, [khT, kwkvo], [pk])
                    k.act(Ktm[:, i, :], ps[:, 0:128], AF.Identity, [pk], [kKtm], scale=128.0 ** -0.5)
                    k.copy(Va[:, i, 0:128], ps[:, 128:256], [pk], [kVa])
                    k.act(Osg[:, i, :], ps[:, 256:384], AF.Sigmoid, [pk], [kOs])
                Cf = [A.alloc([129], F32) for _ in range(2)]; Cb = [A.alloc([130], BF16) for _ in range(2)]
                PmT = [A.alloc([128], BF16) for _ in range(2)]; Kp = [A.alloc([128], BF16) for _ in range(2)]
                dd = [A.alloc([2], F32) for _ in range(2)]
                for d in range(2):
                    k.memset(Cf[d][0], 0.0, [Cf[d][1]]); k.memset(Cb[d][0], 0.0, [Cb[d][1]])
                order = [list(range(NT)), [1, 0] + list(range(NT - 1, NCT - 1, -1))]
                for step in range(NT if on("ml_rec") else 0):
                    cc = [order[d][step] for d in range(2)]
                    notlast = step < NT - 1
                    for d in range(2):
                        c = cc[d]
                        if notlast:
                            k.act(Kp[d][0], Ktm[:, c, :], AF.Identity, [kKtm, kwT[d][1]], [Kp[d][1]], scale=kwT[d][0][:, c, h:h + 1])
                    for d in range(2):
                        c = cc[d]; cs = slice(c * 128, (c + 1) * 128)
                        k.mm(PS[0 + d][:, 0:128], KT[:, cs], QT[:, cs], True, True, [kKT, kQT], [PK[0 + d]])
                    for d in range(2):
                        c = cc[d]
                        if notlast:
                            k.mm(PS[6 + d][:, 0:129], Kp[d][0], Va[:, c, 0:129], True, True, [Kp[d][1], kVa], [PK[6 + d]])
                    for d in range(2):
                        c = cc[d]
                        k.stt(PmT[d][0], PS[0 + d][:, 0:128], kwT[d][0][:, c, h:h + 1], maskd[d], ALU.mult, ALU.mult,
                              [PK[0 + d], kwT[d][1], kc], [PmT[d][1]])
                    for d in range(2):
                        c = cc[d]; cs = slice(c * 128, (c + 1) * 128)
                        pO, kO = PS[2 + d], PK[2 + d]
                        k.mm(pO[:, 0:129], PmT[d][0], Va[:, c, 0:129], True, False, [PmT[d][1], kVa], [kO])
                        k.mm(pO[:, 0:129], QT[:, cs], Cb[d][0][:, 0:129], False, True, [kQT, Cb[d][1]], [kO])
                    for d in range(2):
                        c = cc[d]
                        pO, kO = PS[2 + d], PK[2 + d]
                        k.act(dd[d][0][:, 0:1], pO[:, 128:129], AF.Abs, [kO], [dd[d][1]])
                        k.tt(dd[d][0][:, 0:1], dd[d][0][:, 0:1], thT[d][0][:, c, h:h + 1], ALU.max, [dd[d][1], thT[d][1]], [dd[d][1]])
                        k.recip(dd[d][0][:, 1:2], dd[d][0][:, 0:1], [dd[d][1]], [dd[d][1]])
                        k.stt(Hacc[:, c, :], pO[:, 0:128], dd[d][0][:, 1:2], Hacc[:, c, :], ALU.mult, ALU.add,
                              [kO, dd[d][1], kH + ".%d" % c], [kH + ".%d" % c])
                    for d in range(2):
                        c = cc[d]
                        if notlast:
                            pU, kU = PS[6 + d], PK[6 + d]
                            k.tt(Cf[d][0], pU[:, 0:129], Cf[d][0], ALU.add, [kU, Cf[d][1]], [Cf[d][1]])
                            k.ts(Cf[d][0], Cf[d][0], gbc[d][0][:, h, c:c + 1], ALU.mult, [Cf[d][1], gbc[d][1]], [Cf[d][1]])
                            k.act(Cb[d][0][:, 0:129], Cf[d][0], AF.Identity, [Cf[d][1]], [Cb[d][1]])
                ssq, kssq = A.alloc([NT], F32)
                sq, ksq = A.alloc([8, 128], F32); yb, kyb = A.alloc([8, 128], BF16); ys, kys = A.alloc([8 * 128], BF16)
                hk_all = [kH + ".%d" % c for c in range(NT)]
                for g0 in range(0, NT if on("ml_fin") else 0, 8):
                    gn = min(8, NT - g0)
                    hv = Hacc[:, g0:g0 + gn, :]
                    k.tt(sq[:, 0:gn, :], hv, hv, ALU.mult, hk_all, [ksq])
                    k.reduce(ssq[:, g0:g0 + gn], sq[:, 0:gn, :], ALU.add, [ksq], [kssq])
                if on("ml_fin"):
                    k.ts(ssq, ssq, 1.0 / 128, ALU.mult, [kssq], [kssq], s2=EPS, op1=ALU.add)
                    k.act(ssq, ssq, AF.Sqrt, [kssq], [kssq])
                    k.recip(ssq, ssq, [kssq], [kssq])
                for g0 in range(0, NT if on("ml_fin") else 0, 8):
                    gn = min(8, NT - g0)
                    hv = Hacc[:, g0:g0 + gn, :]
                    k.tt(sq[:, 0:gn, :], hv, ssq[:, g0:g0 + gn].unsqueeze(2).to_broadcast([128, gn, 128]), ALU.mult, hk_all + [kssq], [ksq])
                    k.tt(sq[:, 0:gn, :], sq[:, 0:gn, :], gmlb[:, h * 128:(h + 1) * 128].unsqueeze(1).to_broadcast([128, gn, 128]),
                         ALU.mult, [ksq, kgm], [ksq])
                    k.tt(yb[:, 0:gn, :], sq[:, 0:gn, :], Osg[:, g0:g0 + gn, :], ALU.mult, [ksq, kOs], [kyb])
                    ps = PS[4 + (g0 // 8) % 2]; pk = PK[4 + (g0 // 8) % 2]
                    psb = ps[:].bitcast(BF16)
                    for q in range(gn):
                        k.tr(psb[:, q * 128:(q + 1) * 128], yb[:, q, :], identB, [kyb, kib], [pk])
                    k.copy(ys[:, 0:gn * 128], psb[:, 0:gn * 128], [pk], [kys])
                    k.dma(YT[0][h * 128:(h + 1) * 128, g0 * 128:(g0 + gn) * 128], ys[:, 0:gn * 128], [kys], ["YT0"])
                P.barrier(); A.release(m2)
            P.barrier(); A.release(m)

        def attn_finalize(pO, kO, n, yb_ap, kyb, tmpf, pB, kB):
            rden, krd, osb, kosb = tmpf
            k.act(rden[64:65, 0:n], pO[64:65, 0:n], AF.Ln, [kO], [krd])
            k.act(rden[64:65, 0:n], rden[64:65, 0:n], AF.Exp, [krd], [krd], scale=-1.0)
            k.mm(pB[0:64, 0:n], onesF[64:65, 0:64], rden[64:65, 0:n], True, True, [kc, krd], [kB])
            k.act(osb[0:64, 0:n], pO[0:64, 0:n], AF.Identity, [kO], [kosb])
            k.tt(yb_ap, osb[0:64, 0:n], pB[0:64, 0:n], ALU.mult, [kosb, kB], [kyb])

        def phase_gqa(l, last):
            m = A.mark()
            rc, krc = A.alloc([32, 64], F32); rs, krs = A.alloc([32, 64], F32)
            k.dma(rc, ropec.rearrange("p (a b) -> p a b", b=64), (), [krc])
            k.dma(rs, ropes.rearrange("p (a b) -> p a b", b=64), (), [krs])
            gqf, kgq = A.alloc([384], F32)
            k.dma(gqf, gqk[l:l + 1, :].to_broadcast([128, 384]), (), [kgq])
            gq = gqf.rearrange("p (a b) -> p a b", b=64)
            rden, krd = A.alloc([512], F32); osb, kosb = A.alloc([512], F32)
            for g in range(2):
                m2 = A.mark()
                w, kw_ = A.alloc([8, 448], BF16)
                load_w(w[:, :, 0:256], kw_, w_in[l][:, C_AQ + g * 256:C_AQ + (g + 1) * 256])
                load_w(w[:, :, 256:320], kw_, w_in[l][:, C_AK + g * 64:C_AK + (g + 1) * 64])
                load_w(w[:, :, 320:384], kw_, w_in[l][:, C_AK + g * 64:C_AK + (g + 1) * 64])
                load_w(w[:, :, 384:448], kw_, w_in[l][:, C_AV + g * 64:C_AV + (g + 1) * 64])
                QTK, kQ = A.alloc([3, S], BF16); Vg, kV = A.alloc([NT, 66], BF16)
                k.memset(Vg[:, :, 64:65], 1.0, [kV])
                sq, ksq = A.alloc([6, 64], F32); t1, kt1 = A.alloc([6, 64], F32); xr, kxr = A.alloc([6, 64], F32)
                sw, ksw = A.alloc([6, 64], F32); qr, kqr = A.alloc([384], BF16); st5, kst = A.alloc([6], F32)
                for i in range(NT):
                    ps = PS[i % 2]; pk = PK[i % 2]
                    for kk in range(8):
                        k.mm(ps[:, 0:448], hT[:, kk, i * 128:(i + 1) * 128], w[:, kk, :], kk == 0, kk == 7, [khT, kw_], [pk])
                    psv = ps[:, 0:384].rearrange("p (a b) -> p a b", b=64)
                    k.act(sq, psv, AF.Square, [pk], [ksq])
                    k.reduce(st5, sq, ALU.add, [ksq], [kst])
                    k.ts(st5, st5, 1.0 / 64, ALU.mult, [kst], [kst], s2=EPS, op1=ALU.add)
                    k.act(st5, st5, AF.Ln, [kst], [kst])
                    k.act(st5, st5, AF.Exp, [kst], [kst], scale=-0.5)
                    k.tt(t1, psv, st5.unsqueeze(2).to_broadcast([128, 6, 64]), ALU.mult, [pk, kst], [kt1])
                    k.tt(t1, t1, gq, ALU.mult, [kt1, kgq], [kt1])
                    k.copy(Vg[:, i, 0:64], ps[:, 384:448], [pk], [kV], eng="dve")
                    if i >= NCT:
                        lt = i - NCT
                        cb_ = rc[:, lt, :].unsqueeze(1).to_broadcast([128, 6, 64])
                        k.tt(xr, t1, cb_, ALU.mult, [kt1, krc], [kxr])
                        t1v = t1.rearrange("p h (a q e) -> p h a q e", a=2, q=2)
                        swv = sw.rearrange("p h (a q e) -> p h a q e", a=2, q=2)
                        rsv = rs[:, lt, :].rearrange("p (a q e) -> p a q e", a=2, q=2)
                        for qd in range(2):
                            k.tt(swv[:, :, :, qd, :], t1v[:, :, :, 1 - qd, :],
                                 rsv[:, :, qd, :].unsqueeze(1).to_broadcast([128, 6, 2, 16]), ALU.mult, [kt1, krs], [ksw])
                        k.tt(qr.rearrange("p (a b) -> p a b", b=64), xr, sw, ALU.add, [kxr, ksw], [kqr])
                    else:
                        k.copy(qr.rearrange("p (a b) -> p a b", b=64), t1, [kt1], [kqr])
                    pt = PS[2 + i % 2]; pkt = PK[2 + i % 2]
                    ptb = pt[:].bitcast(BF16)
                    for q in range(3):
                        k.tr(ptb[:, q * 128:(q + 1) * 128], qr[:, q * 128:(q + 1) * 128], identB, [kqr, kib], [pkt])
                    k.act(QTK[:, :, i * 128:(i + 1) * 128], ptb[:, 0:384].rearrange("p (a b) -> p a b", b=128), AF.Identity, [pkt], [kQ])
                PT = [A.alloc([512], BF16) for _ in range(3)]
                yb, kyb = A.alloc([512], BF16)
                jobs = []
                if not last:
                    for qh in range(4):
                        jobs.append((qh, 0, TC, list(range(NCT))))
                for qh in range(4):
                    for qc in range(TL // 512):
                        jobs.append((qh, TC + qc * 512, 512, list(range(NT))))
                items = []
                for ji, (qh, q0, qn, kts) in enumerate(jobs):
                    for ki, kt in enumerate(kts):
                        items.append((ji, ki, kt))

                def g_qk(idx):
                    ji, ki, kt = items[idx]
                    qh, q0, qn, kts = jobs[ji]
                    j = qh // 2; hb = 64 * (qh % 2)
                    pS, kS = PS[idx % 3], PK[idx % 3]
                    k.mm(pS[:, 0:qn], QTK[hb:hb + 64, 2, kt * 128:(kt + 1) * 128], QTK[hb:hb + 64, j, q0:q0 + qn], True, True, [kQ], [kS])

                def g_rest(idx):
                    ji, ki, kt = items[idx]
                    qh, q0, qn, kts = jobs[ji]
                    pS, kS = PS[idx % 3], PK[idx % 3]
                    ptile = PT[idx % 3]
                    pO, kO = PS[4 + ji % 2], PK[4 + ji % 2]
                    k.act(ptile[0][:, 0:qn], pS[:, 0:qn], AF.Exp, [kS], [ptile[1]], scale=0.125)
                    k.mm(pO[0:65, 0:qn], Vg[:, kt, 0:65], ptile[0][:, 0:qn], ki == 0, ki == len(kts) - 1, [kV, ptile[1]], [kO])

                def g_fin(ji):
                    qh, q0, qn, kts = jobs[ji]
                    pO, kO = PS[4 + ji % 2], PK[4 + ji % 2]
                    attn_finalize(pO, kO, qn, yb[0:64, 0:qn], kyb, (rden, krd, osb, kosb), PS[6 + ji % 2], PK[6 + ji % 2])
                    hd = 4 * g + qh
                    k.dma(YT[1][hd * 64:(hd + 1) * 64, q0:q0 + qn], yb[0:64, 0:qn], [kyb], ["YT1"])

                pending = None
                g_qk(0)
                for idx in range(len(items)):
                    if idx + 1 < len(items):
                        g_qk(idx + 1)
                    g_rest(idx)
                    ji, ki, kt = items[idx]
                    if pending is not None and pending[1] == idx:
                        g_fin(pending[0]); pending = None
                    if ki == len(jobs[ji][3]) - 1:
                        if idx + 2 < len(items):
                            pending = (ji, idx + 2)
                        else:
                            if pending is not None:
                                g_fin(pending[0]); pending = None
                            g_fin(ji)
                if pending is not None:
                    g_fin(pending[0]); pending = None
                P.barrier(); A.release(m2)
            P.barrier(); A.release(m)

        def phase_na(l, last):
            m = A.mark()
            rden, krd = A.alloc([512], F32); osb, kosb = A.alloc([512], F32)
            for hp in range(4):
                m2 = A.mark()
                wq, kwq = A.alloc([8, 128], BF16); wk_, kwk = A.alloc([8, 128], BF16); wv, kwv = A.alloc([8, 128], BF16)
                load_w(wq, kwq, w_in[l][:, C_NQ + hp * 128:C_NQ + (hp + 1) * 128])
                load_w(wk_, kwk, w_in[l][:, C_NK + hp * 128:C_NK + (hp + 1) * 128])
                load_w(wv, kwv, w_in[l][:, C_NV + hp * 128:C_NV + (hp + 1) * 128])
                Tb, kTb = A.alloc([2, 7, 128], F32)
                k.dma(Tb, rpbT[l, hp].rearrange("p (a b c) -> p a b c", a=2, b=7), (), [kTb])
                QT, kQT = A.alloc([S], BF16); KT, kKT = A.alloc([S], BF16); Vn, kVn = A.alloc([NT, 2, 66], BF16)
                k.memset(Vn[:, :, :, 64:65], 1.0, [kVn])
                for ci, (t0, tn) in enumerate(TOKCH):
                    for (wt, kwt, dst, kd, sc, pi) in ((wq, kwq, QT, kQT, 0.125, 0), (wk_, kwk, KT, kKT, 1.0, 1)):
                        ps = PS[pi + 2 * (ci % 2)]; pk = PK[pi + 2 * (ci % 2)]
                        for kk in range(8):
                            k.mm(ps[:, 0:tn], wt[:, kk, :], hT[:, kk, t0:t0 + tn], kk == 0, kk == 7, [kwt, khT], [pk])
                        k.act(dst[:, t0:t0 + tn], ps[:, 0:tn], AF.Identity, [pk], [kd], scale=sc)
                for i in range(NT):
                    ps = PS[4 + i % 2]; pk = PK[4 + i % 2]
                    for kk in range(8):
                        k.mm(ps[:, 0:128], hT[:, kk, i * 128:(i + 1) * 128], wv[:, kk, :], kk == 0, kk == 7, [khT, kwv], [pk])
                    k.copy(Vn[:, i, :, 0:64], ps[:, 0:128].rearrange("p (a b) -> p a b", b=64), [pk], [kVn])
                sbs = [A.alloc([640], F32) for _ in range(2)]
                PT = [A.alloc([896], BF16) for _ in range(2)]
                ynb, kyn = A.alloc([2, TL], BF16, parts=64)
                ycb, kyc = A.alloc([2, TC], BF16, parts=64)
                it = 0
                if not last:
                    for hh in range(2):
                        hb = 64 * hh
                        pS, kS = PS[0], PK[0]; pO, kO = PS[4 + hh], PK[4 + hh]
                        for kt in range(NCT):
                            k.mm(pS[:, kt * 256:(kt + 1) * 256], KT[hb:hb + 64, kt * 128:(kt + 1) * 128], QT[hb:hb + 64, 0:TC], True, True, [kKT, kQT], [kS])
                        ptile = PT[it % 2]; it += 1
                        k.act(ptile[0][:, 0:512], pS[:, 0:512], AF.Exp, [kS], [ptile[1]])
                        for kt in range(NCT):
                            k.mm(pO[0:65, 0:TC], Vn[:, kt, hh, 0:65], ptile[0][:, kt * 256:(kt + 1) * 256], kt == 0, kt == NCT - 1, [kVn, ptile[1]], [kO])
                        attn_finalize(pO, kO, TC, ycb[:, hh, :], kyc, (rden, krd, osb, kosb), PS[6 + hh], PK[6 + hh])
                    for hh in range(2):
                        hd = 2 * hp + hh
                        k.dma(YT[2][hd * 64:(hd + 1) * 64, 0:TC], ycb[:, hh, :], [kyc], ["YT2"])
                def blk(ji):
                    rb, hh = ji // 2, ji % 2
                    q0 = TC + rb * 128
                    r0a, r0b = r0_of(2 * rb), r0_of(2 * rb + 1)
                    tiles = list(range(r0a // 2, (r0b + 7) // 2 + 1))
                    nw = len(tiles)
                    di0 = (2 * tiles[0] - 2 * rb + 6) // 2
                    assert 0 <= di0 and di0 + nw <= 7 and nw <= 5
                    return rb, hh, q0, tiles, nw, di0

                def bufs(ji):
                    return (PS[2 * (ji % 2)], PK[2 * (ji % 2)], PS[2 * (ji % 2) + 1], PK[2 * (ji % 2) + 1],
                            PS[4 + ji % 2], PK[4 + ji % 2], sbs[ji % 2], PT[ji % 2])

                def n_qk(ji):
                    rb, hh, q0, tiles, nw, di0 = blk(ji)
                    hb = 64 * hh
                    pSa, kSa, pSb, kSb, pO, kO, sb_, ptile = bufs(ji)
                    for jw, t in enumerate(tiles):
                        pp, kp, co = (pSa, kSa, jw * 128) if jw < 4 else (pSb, kSb, (jw - 4) * 128)
                        kt = NCT + t
                        k.mm(pp[:, co:co + 128], KT[hb:hb + 64, kt * 128:(kt + 1) * 128], QT[hb:hb + 64, q0:q0 + 128], True, True, [kKT, kQT], [kp])
                    for kt in range(NCT):
                        k.mm(pSb[:, 128 + kt * 128:256 + kt * 128], KT[hb:hb + 64, kt * 128:(kt + 1) * 128], QT[hb:hb + 64, q0:q0 + 128], True, True, [kKT, kQT], [kSb])

                def n_soft(ji):
                    rb, hh, q0, tiles, nw, di0 = blk(ji)
                    pSa, kSa, pSb, kSb, pO, kO, sb_, ptile = bufs(ji)
                    n4 = min(nw, 4)
                    k.tt(sb_[0][:, 0:n4 * 128], pSa[:, 0:n4 * 128], Tb[:, hh, di0:di0 + n4, :].rearrange("p a b -> p (a b)"), ALU.add, [kSa, kTb], [sb_[1]])
                    if nw > 4:
                        k.tt(sb_[0][:, 512:640], pSb[:, 0:128], Tb[:, hh, di0 + 4, :], ALU.add, [kSb, kTb], [sb_[1]])
                    k.act(ptile[0][:, 0:nw * 128], sb_[0][:, 0:nw * 128], AF.Exp, [sb_[1]], [ptile[1]])
                    k.act(ptile[0][:, 640:896], pSb[:, 128:384], AF.Exp, [kSb], [ptile[1]])
                    for jw, t in enumerate(tiles):
                        for a in range(2):
                            for b in range(2):
                                kr = 2 * t + a; qrow = 2 * rb + b
                                if not (r0_of(qrow) <= kr <= r0_of(qrow) + 7):
                                    k.memset(ptile[0][64 * a:64 * a + 64, jw * 128 + 64 * b:jw * 128 + 64 * b + 64], 0.0, [ptile[1]])

                def n_pv(ji):
                    rb, hh, q0, tiles, nw, di0 = blk(ji)
                    pSa, kSa, pSb, kSb, pO, kO, sb_, ptile = bufs(ji)
                    for jw, t in enumerate(tiles):
                        k.mm(pO[0:65, 0:128], Vn[:, NCT + t, hh, 0:65], ptile[0][:, jw * 128:(jw + 1) * 128], jw == 0, False, [kVn, ptile[1]], [kO])
                    for kt in range(NCT):
                        k.mm(pO[0:65, 0:128], Vn[:, kt, hh, 0:65], ptile[0][:, 640 + kt * 128:768 + kt * 128], False, kt == NCT - 1, [kVn, ptile[1]], [kO])

                def n_fin(ji):
                    rb, hh, q0, tiles, nw, di0 = blk(ji)
                    pSa, kSa, pSb, kSb, pO, kO, sb_, ptile = bufs(ji)
                    attn_finalize(pO, kO, 128, ynb[:, hh, rb * 128:(rb + 1) * 128], kyn, (rden, krd, osb, kosb), PS[6 + ji % 2], PK[6 + ji % 2])

                NJ = 64
                n_qk(0)
                for ji in range(NJ):
                    if ji + 1 < NJ:
                        n_qk(ji + 1)
                    n_soft(ji)
                    n_pv(ji)
                    if ji >= 1:
                        n_fin(ji - 1)
                n_fin(NJ - 1)
                for hh in range(2):
                    hd = 2 * hp + hh
                    k.dma(YT[2][hd * 64:(hd + 1) * 64, TC:S], ynb[:, hh, :], [kyn], ["YT2"])
                P.barrier(); A.release(m2)
            P.barrier(); A.release(m)

        def phase_merge(l):
            m = A.mark()
            wz = [A.alloc([8, 3, 128], BF16) for _ in range(2)]
            wb = [A.alloc([3, 4, 128], BF16) for _ in range(2)]
            yt = [A.alloc([3, 4, 512], BF16) for _ in range(2)]
            sg = [A.alloc([512], F32) for _ in range(2)]
            acc, kacc = A.alloc([512], F32); tmp, ktmp = A.alloc([512], F32)
            mo = [A.alloc([512], BF16) for _ in range(2)]
            it = 0
            for ct in range(8):
                b = ct % 2
                for n in range(3):
                    load_w(wz[b][0][:, :, n, :], wz[b][1], w_in[l][:, C_ZG + n * D + ct * 128:C_ZG + n * D + (ct + 1) * 128])
                    load_w(wb[b][0][:, n, :, :], wb[b][1], w_branch[l, n][:, ct * 128:(ct + 1) * 128])
                for ci, (t0, tn) in enumerate(TOKCH):
                    yb_ = yt[it % 2]; mo_ = mo[it % 2]; it += 1
                    for n in range(3):
                        k.dma(yb_[0][:, n, :, 0:tn], YT[n][:, t0:t0 + tn].rearrange("(k p) c -> p k c", p=128), ["YT%d" % n], [yb_[1]])
                    for n in range(3):
                        pg, kg = PS[2 * (n % 2)], PK[2 * (n % 2)]
                        pp, kp = PS[2 * (n % 2) + 1], PK[2 * (n % 2) + 1]
                        for kk in range(8):
                            k.mm(pg[:, 0:tn], wz[b][0][:, kk, n, :], hT[:, kk, t0:t0 + tn], kk == 0, kk == 7, [wz[b][1], khT], [kg])
                        for kk in range(4):
                            k.mm(pp[:, 0:tn], wb[b][0][:, n, kk, :], yb_[0][:, n, kk, 0:tn], kk == 0, kk == 3, [wb[b][1], yb_[1]], [kp])
                        sg_ = sg[n % 2]
                        k.act(sg_[0][:, 0:tn], pg[:, 0:tn], AF.Sigmoid, [kg], [sg_[1]])
                        if n == 0:
                            k.tt(acc[:, 0:tn], sg_[0][:, 0:tn], pp[:, 0:tn], ALU.mult, [sg_[1], kp], [kacc])
                        else:
                            k.tt(tmp[:, 0:tn], sg_[0][:, 0:tn], pp[:, 0:tn], ALU.mult, [sg_[1], kp], [ktmp])
                            if n == 1:
                                k.tt(acc[:, 0:tn], acc[:, 0:tn], tmp[:, 0:tn], ALU.add, [kacc, ktmp], [kacc])
                            else:
                                k.tt(mo_[0][:, 0:tn], acc[:, 0:tn], tmp[:, 0:tn], ALU.add, [kacc, ktmp], [mo_[1]])
                    k.dma(MTm[ct * 128:(ct + 1) * 128, t0:t0 + tn], mo_[0][:, 0:tn], [mo_[1]], ["MTm"])
            P.barrier(); A.release(m)

        def phase_proj_res(MT, mtkey, nk, wsrc, which, tiles):
            m = A.mark()
            wd, kwd = A.alloc([nk, D], BF16)
            for kk0 in range(0, nk, 8):
                kn = min(8, nk - kk0)
                load_w(wd[:, kk0:kk0 + kn, :], kwd, wsrc[kk0 * 128:(kk0 + kn) * 128, :])
            mt = [A.alloc([nk, 128], BF16) for _ in range(2)]
            xt = [A.alloc([D], F32) for _ in range(2)]
            tt_ = [A.alloc([D], F32) for _ in range(2)]
            junk, kj = A.alloc([512], F32)
            ssv = [A.alloc([4], F32) for _ in range(2)]
            for ii, i in enumerate(tiles):
                b = ii % 2
                r = 1 if i < NCT else 0
                k.dma(mt[b][0], MT[:, i * 128:(i + 1) * 128].rearrange("(k p) c -> p k c", p=128), [mtkey], [mt[b][1]])
                k.dma(xt[b][0], X[i * 128:(i + 1) * 128, :], ["X.%d" % i], [xt[b][1]])
                ph = [(PS[4 * b + hf], PK[4 * b + hf]) for hf in range(2)]
                for hf in range(2):
                    for kk in range(nk):
                        k.mm(ph[hf][0][:], mt[b][0][:, kk, :], wd[:, kk, hf * 512:(hf + 1) * 512], kk == 0, kk == nk - 1, [mt[b][1], kwd], [ph[hf][1]])
                sv, ksv = ssv[b]
                for hf in range(2):
                    k.act(junk, ph[hf][0][:], AF.Square, [ph[hf][1]], [kj, ksv], accum=sv[:, hf:hf + 1])
                k.tt(sv[:, 2:3], sv[:, 0:1], sv[:, 1:2], ALU.add, [ksv], [ksv])
                k.ts(sv[:, 2:3], sv[:, 2:3], 1.0 / D, ALU.mult, [ksv], [ksv], s2=EPS, op1=ALU.add)
                k.act(sv[:, 3:4], sv[:, 2:3], AF.Ln, [ksv], [ksv])
                k.act(sv[:, 3:4], sv[:, 3:4], AF.Exp, [ksv], [ksv], scale=-0.5)
                for hf in range(2):
                    cs = slice(hf * 512, (hf + 1) * 512)
                    k.stt(tt_[b][0][:, cs], ph[hf][0][:], sv[:, 3:4], Gbc[:, r, which, cs], ALU.mult, ALU.mult, [ph[hf][1], ksv, kG], [tt_[b][1]])
                k.tt(tt_[b][0], tt_[b][0], xt[b][0], ALU.add, [tt_[b][1], xt[b][1]], [tt_[b][1]], eng="pool")
                k.dma(X[i * 128:(i + 1) * 128, :], tt_[b][0], [tt_[b][1]], ["X.%d" % i, "X"])
            P.barrier(); A.release(m)

        def phase_ffn_up(l, lo_tok):
            m = A.mark()
            LB = S + 4
            cw, kcw = A.alloc([2 * NFT, 3], F32); cbb, kcb = A.alloc([2 * NFT], F32)
            k.dma(cw, cwfm[l].rearrange("p (a b) -> p a b", b=3), (), [kcw])
            k.dma(cbb, cbfm[l], (), [kcb])
            U = [A.alloc([LB], F32) for _ in range(2)]
            T = [A.alloc([LB], F32) for _ in range(2)]
            mb, kmb = A.alloc([LB], BF16)
            w = [A.alloc([8, 2, 128], BF16) for _ in range(2)]
            for z in range(2):
                k.memset(U[z][0], 0.0, [U[z][1]])
            chunks = []
            if lo_tok == 0:
                chunks.append((0, TC, 1))
            for qc in range(TL // 512):
                chunks.append((TC + qc * 512, 512, 3 + TC + qc * 512))
            lo_c = 1 if lo_tok == 0 else 3 + TC
            hi_c = LB - 1
            it = 0
            for j in range(NFT):
                b = j % 2
                load_w(w[b][0][:, :, 0, :], w[b][1], w_up[l][:, j * 128:(j + 1) * 128])
                load_w(w[b][0][:, :, 1, :], w[b][1], w_up[l][:, FFN + j * 128:FFN + (j + 1) * 128])
                for (t0, tn, c0) in chunks:
                    for z in range(2):
                        ps, pk = PS[it % 4], PK[it % 4]; it += 1
                        for kk in range(8):
                            k.mm(ps[:, 0:tn], w[b][0][:, kk, z, :], hT[:, kk, t0:t0 + tn], kk == 0, kk == 7, [w[b][1], khT], [pk])
                        k.act(U[z][0][:, c0:c0 + tn], ps[:, 0:tn], AF.Identity, [pk], [U[z][1]])
                n = hi_c - lo_c
                for z in range(2):
                    ch = z * NFT + j
                    k.act(T[z][0][:, lo_c:hi_c], U[z][0][:, lo_c:hi_c], AF.Identity, [U[z][1], kcw, kcb], [T[z][1]],
                          scale=cw[:, ch, 1:2], bias=cbb[:, ch:ch + 1])
                    k.stt(T[z][0][:, lo_c:hi_c], U[z][0][:, lo_c - 1:hi_c - 1], cw[:, ch, 0:1], T[z][0][:, lo_c:hi_c], ALU.mult, ALU.add,
                          [U[z][1], kcw, T[z][1]], [T[z][1]])
                    k.stt(T[z][0][:, lo_c:hi_c], U[z][0][:, lo_c + 1:hi_c + 1], cw[:, ch, 2:3], T[z][0][:, lo_c:hi_c], ALU.mult, ALU.add,
                          [U[z][1], kcw, T[z][1]], [T[z][1]])
                k.act(T[1][0][:, lo_c:hi_c], T[1][0][:, lo_c:hi_c], AF.Silu, [T[1][1]], [T[1][1]])
                k.tt(mb[:, lo_c:hi_c], T[0][0][:, lo_c:hi_c], T[1][0][:, lo_c:hi_c], ALU.mult, [T[0][1], T[1][1]], [kmb])
                if lo_tok == 0:
                    k.dma(MTf[j * 128:(j + 1) * 128, 0:TC], mb[:, 1:1 + TC], [kmb], ["MTf"])
                k.dma(MTf[j * 128:(j + 1) * 128, TC:S], mb[:, 3 + TC:3 + S], [kmb], ["MTf"])
            P.barrier(); A.release(m)

        for l in range(n_layers):
            last = (l == DEPTH - 1) or force_last
            P.new_epoch()
            if on("mod"):
                P.label = "mod%d" % l; phase_mod(l)
            if on("norm"):
                P.label = "norm%d" % l; phase_norm(0)
            if on("mlstm"):
                P.label = "mlstm%d" % l; phase_mlstm(l)
            if on("gqa"):
                P.label = "gqa%d" % l; phase_gqa(l, last)
            if on("na"):
                P.label = "na%d" % l; phase_na(l, last)
            if on("merge"):
                P.label = "merge%d" % l; phase_merge(l)
            tiles = list(range(NCT if last else 0, NT))
            if on("res1"):
                P.label = "res1_%d" % l; phase_proj_res(MTm, "MTm", 8, w_out[l], 0, tiles)
            if on("norm2"):
                P.label = "norm2%d" % l; phase_norm(1)
            if on("ffn"):
                P.label = "ffn%d" % l; phase_ffn_up(l, TC if last else 0)
            if on("res2"):
                P.label = "res2_%d" % l; phase_proj_res(MTf, "MTf", NFT, w_down[l], 1, tiles)
        P.barrier()
        for q in range(8):
            k.dma(yout[q * 512:(q + 1) * 512, :], X[TC + q * 512:TC + (q + 1) * 512, :], ["X"], ["yout%d" % q])
        P.emit()
        build.last_prog = P
        print("ops emitted:", P.nops, {e: len(v) for e, v in P.ops.items()})
    return nc


def _consts():
    ident = np.eye(128, dtype=np.float32)
    ones = np.ones((128, 128), np.float32)
    s = np.arange(128)[:, None]; t = np.arange(128)[None, :]
    mf = (s <= t).astype(np.float32); mb = (s >= t).astype(np.float32)
    cst = np.concatenate([ident, ones, mf, mb, np.zeros((128, 128), np.float32)], axis=1)
    esel = np.zeros((4, 4, 128), np.float32)
    for h in range(4):
        esel[h, h, :] = 1.0
    half = 32
    tpos = np.arange(TL)
    inv = (10000.0 ** (-np.arange(0, half, 2, dtype=np.float32) / half)).astype(np.float32)
    ang_r = (tpos // GRID).astype(np.float32)[:, None] * inv
    ang_c = (tpos % GRID).astype(np.float32)[:, None] * inv
    ang = np.concatenate([ang_r, ang_r, ang_c, ang_c], axis=-1)
    cos = np.cos(ang).astype(np.float32); sin = np.sin(ang).astype(np.float32)
    sgn = np.concatenate([-np.ones(16), np.ones(16), -np.ones(16), np.ones(16)]).astype(np.float32)
    sins = sin * sgn[None, :]
    ropec = cos.reshape(32, 128, 64).transpose(1, 0, 2).reshape(128, 32 * 64)
    ropes = sins.reshape(32, 128, 64).transpose(1, 0, 2).reshape(128, 32 * 64)
    return cst, esel.reshape(4, 512), np.ascontiguousarray(ropec), np.ascontiguousarray(ropes)


def _rpb_tiles(na_rpb):
    L = na_rpb.shape[0]
    cols = np.arange(GRID)
    c0 = np.clip(cols - 8, 0, GRID - 16)
    kc = np.arange(GRID)[:, None]; qc = np.arange(GRID)[None, :]
    inwin = (kc >= c0[None, :]) & (kc < c0[None, :] + 16)
    dc = np.clip(kc - qc + 15, 0, 30)
    out = np.full((L, 4, 128, 2, 7, 128), NEG, np.float32)
    for di in range(7):
        delta = 2 * di - 6
        for a in range(2):
            for b in range(2):
                dr = delta + a - b + 7
                if not (0 <= dr <= 14):
                    continue
                vals = na_rpb[:, :, dr, :][:, :, dc]
                vals = np.where(inwin[None, None], vals, np.float32(NEG))
                v = vals.reshape(L, 4, 2, GRID, GRID).transpose(0, 1, 3, 2, 4)
                out[:, :, 64 * a:64 * a + 64, :, di, 64 * b:64 * b + 64] = v
    return out.reshape(L, 4, 128, 2 * 7 * 128)


_NC_CACHE = {}


def prep_inputs(inp):
    f = lambda a: np.ascontiguousarray(np.asarray(a, dtype=np.float32))
    cst, esel, ropec, ropes = _consts()
    L = DEPTH
    gfm = np.stack([f(inp["g_pre_mix"]).reshape(L, 8, 128), f(inp["g_pre_ffn"]).reshape(L, 8, 128)], axis=1)
    gfm = np.ascontiguousarray(gfm.transpose(0, 3, 1, 2))
    gpost = np.ascontiguousarray(np.stack([f(inp["g_post_mix"]), f(inp["g_post_ffn"])], axis=1))
    bgate = np.ascontiguousarray(f(inp["b_ml_gates"]).reshape(L, 4, 4).transpose(0, 2, 1))
    gq = f(inp["g_q"]); gk = f(inp["g_k"])
    gqk = np.ascontiguousarray(np.concatenate([gq, gq, gq, gq, gk, gk], axis=1))
    cw = f(inp["conv_w"])
    cwfm = np.ascontiguousarray(cw.reshape(L, 3, 2 * NFT, 128).transpose(0, 3, 2, 1).reshape(L, 128, 2 * NFT * 3))
    cbfm = np.ascontiguousarray(f(inp["conv_b"]).reshape(L, 2 * NFT, 128).transpose(0, 2, 1))
    shared = {
        "w_mod": f(inp["w_mod"]), "b_mod": f(inp["b_mod"]), "gfm": gfm, "gpost": gpost, "w_in": f(inp["w_in"]),
        "bgate": bgate, "gml": f(inp["g_ml_out"]), "gqk": gqk, "rpbT": _rpb_tiles(f(inp["na_rpb"])),
        "w_branch": f(inp["w_branch"]), "w_out": f(inp["w_out"]), "w_up": f(inp["w_up"]), "w_down": f(inp["w_down"]),
        "cwfm": cwfm, "cbfm": cbfm, "ropec": ropec, "ropes": ropes, "cst": cst, "esel": esel,
    }
    x = f(inp["x"]); c = f(inp["c"]); ctx = f(inp["ctx"]); cc = f(inp["c_ctx"])
    maps = []
    for b in range(x.shape[0]):
        cv = np.stack([c[b], cc], axis=0)
        cT = np.ascontiguousarray(cv.reshape(2, 8, 128).transpose(2, 0, 1))
        mp = dict(shared)
        mp.update({"x_in": x[b], "ctx_in": ctx[b], "cT": cT})
        maps.append(mp)
    return maps


def kernel(**inputs):
    maps = prep_inputs(inputs)
    if "nc" not in _NC_CACHE:
        _NC_CACHE["nc"] = build()
    nc = _NC_CACHE["nc"]
    res = run_bass_kernel_spmd(nc, maps, core_ids=list(range(len(maps))))
    return np.stack([np.asarray(r["y"], dtype=np.float32) for r in res.results], axis=0)
```

```python
import numpy as np
from contextlib import ExitStack
import concourse.bass as bass
import concourse.mybir as mybir
from concourse.bass_utils import run_bass_kernel_spmd

F32 = mybir.dt.float32
BF16 = mybir.dt.bfloat16
AF = mybir.ActivationFunctionType
ALU = mybir.AluOpType
AX = mybir.AxisListType

D = 1024; KC = 8; TL = 4096; TC = 256; S = TL + TC; NT = S // 128; NCT = TC // 128
DEPTH = 4; FFN = 2816; NFT = FFN // 128; D_IN = 7440
GRID = 64; EPS = 1e-6
C_MQ, C_MK, C_MV, C_MO, C_MG = 0, 512, 1024, 1536, 2048
C_AQ, C_AK, C_AV = 2064, 2576, 2704
C_NQ, C_NK, C_NV, C_ZG = 2832, 3344, 3856, 4368
ENGS = ("pe", "act", "dve", "pool", "sp")
N_DMA_SEMS = 14
NEG = -30000.0


class Prog:
    def __init__(self, nc):
        self.nc = nc
        self.ops = {e: [] for e in ENGS}
        self.cnt = {e: 0 for e in ENGS}
        self.res = {}
        self.seen = {e: {} for e in ENGS}
        self.dma_i = 0
        self.dma_hist = {}
        self.nops = 0
        self.ep = 0
        self.label = "init"
        self.name2label = {}
        self.allsems = set()
        self.final = []

    def sn(self, base):
        n = "%s_e%d" % (base, self.ep)
        self.allsems.add(n)
        return n

    def new_epoch(self):
        self.barrier()
        self.ep += 1
        self.cnt = {e: 0 for e in ENGS}
        self.dma_hist = {}
        self.seen = {e: {} for e in ENGS}

    def _deps(self, reads, writes):
        ev = set()
        for r in reads:
            st = self.res.get(r)
            if st and st[0]:
                ev.add(st[0])
        for w in writes:
            st = self.res.get(w)
            if st:
                if st[0]:
                    ev.add(st[0])
                ev.update(st[1])
        return ev

    def _commit(self, event, reads, writes):
        for r in reads:
            st = self.res.setdefault(r, [None, []])
            st[1].append(event)
        for w in writes:
            self.res[w] = [event, []]

    def _filter(self, eng, evs, own_sem=None):
        seen = self.seen[eng]
        best = {}
        for (s, v) in evs:
            if s == own_sem and eng == "pe":
                continue
            if seen.get(s, 0) >= v:
                continue
            if best.get(s, 0) < v:
                best[s] = v
        for s, v in best.items():
            seen[s] = v
        return list(best.items())

    def op(self, eng, fn, reads=(), writes=()):
        writes = tuple(writes) + tuple(r for r in reads if r.startswith("ps"))
        reads = tuple(r for r in reads if not r.startswith("ps"))
        evs = self._deps(reads, writes)
        own = self.sn("c_" + eng)
        waits = self._filter(eng, evs, own)
        self.cnt[eng] += 1
        event = (own, self.cnt[eng])
        self.ops[eng].append((waits, fn, (own, 1), self.label))
        self._commit(event, reads, writes)
        self.nops += 1

    def dma(self, eng, fn, reads=(), writes=()):
        reads = tuple(reads); writes = tuple(writes)
        evs = self._deps(reads, writes)
        j = self.dma_i % N_DMA_SEMS
        sem = self.sn("d_%d" % j)
        prev = self.dma_hist.get(j, 0)
        if prev:
            evs.add((sem, prev))
        val = prev + 16
        self.dma_hist[j] = val
        self.dma_i += 1
        waits = self._filter(eng, evs, None)
        self.ops[eng].append((waits, fn, (sem, 16), self.label))
        self._commit((sem, val), reads, writes)
        self.nops += 1

    def barrier(self):
        evs = [(self.sn("c_" + e), self.cnt[e]) for e in ENGS if self.cnt[e]]
        evs += [(self.sn("d_%d" % j), v) for j, v in self.dma_hist.items()]
        self.final = list(evs)
        for e in ENGS:
            waits = self._filter(e, evs, None)
            if waits:
                self.ops[e].append((waits, None, None, self.label))
        self.res = {}

    def emit(self):
        nc = self.nc
        with ExitStack() as st:
            sems = {}
            for n in sorted(self.allsems):
                sems[n] = st.enter_context(nc.semaphore(n))
            final = [(self.sn("c_" + e), self.cnt[e]) for e in ENGS if self.cnt[e] and e != "sp"]
            final += [(self.sn("d_%d" % j), v) for j, v in self.dma_hist.items()]
            block = st.enter_context(nc.Block())

            def run(engname):
                def body(eng):
                    for waits, fn, inc, lab in self.ops[engname]:
                        for (ws, wv) in waits:
                            eng.wait_ge(sems[ws], wv)
                        if fn is not None:
                            ins = fn(eng)
                            ins.then_inc(sems[inc[0]], inc[1])
                            try:
                                self.name2label[ins.ins.name] = lab
                            except Exception:
                                pass
                    if engname == "sp":
                        for (ws, wv) in final:
                            eng.wait_ge(sems[ws], wv)
                return body

            block.tensor(run("pe"))
            block.scalar(run("act"))
            block.vector(run("dve"))
            block.gpsimd(run("pool"))
            block.sync(run("sp"))


class Arena:
    def __init__(self, ap, nwords):
        self.ap = ap; self.n = nwords; self.off = 0; self.uid = 0

    def mark(self):
        return self.off

    def release(self, m):
        self.off = m

    def alloc(self, shape, dt, parts=128):
        n = int(np.prod(shape))
        words = n if dt == F32 else (n + 1) // 2
        words = (words + 7) // 8 * 8
        assert self.off + words <= self.n, ("SBUF arena overflow", self.off, words, self.n)
        a = self.ap[0:parts, self.off:self.off + words]
        self.off += words
        if dt != F32:
            a = a.bitcast(dt)
        a = a[:, 0:n]
        if len(shape) > 1:
            names = [chr(ord('a') + i) for i in range(len(shape))]
            kw = {names[i]: int(shape[i]) for i in range(len(shape))}
            a = a.rearrange("p (%s) -> p %s" % (" ".join(names), " ".join(names)), **kw)
        self.uid += 1
        return a, "b%d" % self.uid


class K:
    def __init__(self, P):
        self.P = P

    def mm(self, out, lhsT, rhs, start, stop, r, w):
        self.P.op("pe", lambda e: e.matmul(out, lhsT=lhsT, rhs=rhs, start=start, stop=stop), r, w)

    def tr(self, out, in_, ident, r, w):
        self.P.op("pe", lambda e: e.transpose(out, in_, ident), r, w)

    def act(self, out, in_, func, r, w, scale=None, bias=None, accum=None, eng="act"):
        kw = {}
        if scale is not None: kw["scale"] = scale
        if bias is not None: kw["bias"] = bias
        if accum is not None: kw["accum_out"] = accum
        self.P.op("act", lambda e: e.activation(out=out, in_=in_, func=func, **kw), r, w)

    def tt(self, out, in0, in1, op, r, w, eng="dve"):
        self.P.op(eng, lambda e: e.tensor_tensor(out=out, in0=in0, in1=in1, op=op), r, w)

    def ts(self, out, in0, s1, op0, r, w, s2=None, op1=None, eng="dve"):
        if op1 is None:
            self.P.op(eng, lambda e: e.tensor_scalar(out=out, in0=in0, scalar1=s1, scalar2=None, op0=op0), r, w)
        else:
            self.P.op(eng, lambda e: e.tensor_scalar(out=out, in0=in0, scalar1=s1, scalar2=s2, op0=op0, op1=op1), r, w)

    def stt(self, out, in0, scalar, in1, op0, op1, r, w):
        self.P.op("dve", lambda e: e.scalar_tensor_tensor(out=out, in0=in0, scalar=scalar, in1=in1, op0=op0, op1=op1), r, w)

    def copy(self, out, in_, r, w, eng="dve"):
        self.P.op(eng, lambda e: e.tensor_copy(out=out, in_=in_), r, w)

    def recip(self, out, in_, r, w):
        self.P.op("dve", lambda e: e.reciprocal(out=out, in_=in_), r, w)

    def reduce(self, out, in_, op, r, w):
        self.P.op("dve", lambda e: e.tensor_reduce(out=out, in_=in_, axis=AX.X, op=op), r, w)

    def scan(self, out, d0, d1, init, op0, op1, r, w):
        self.P.op("dve", lambda e: e.tensor_tensor_scan(out=out, data0=d0, data1=d1, initial=init, op0=op0, op1=op1), r, w)

    def ttr(self, out, in0, in1, accum, r, w):
        self.P.op("dve", lambda e: e.tensor_tensor_reduce(out=out, in0=in0, in1=in1, scale=1.0, scalar=0.0,
                                                          op0=ALU.mult, op1=ALU.add, accum_out=accum), r, w)

    def memset(self, ap, v, w, eng="pool"):
        self.P.op(eng, lambda e: e.memset(ap, v), (), w)

    def dma(self, out, in_, r, w, eng="sp"):
        self.P.dma(eng, lambda e: e.dma_start(out=out, in_=in_), r, w)


def r0_of(r):
    return min(max(r - 4, 0), GRID - 8)


def build(n_layers=DEPTH, debug=False, arena_words=51200, phases=None, force_last=False):
    nc = bass.Bass("TRN2", target_bir_lowering=False)

    def din(name, shape, dt=F32):
        return nc.dram_tensor(name, list(shape), dt, kind="ExternalInput").ap()

    x_in = din("x_in", [TL, D]); ctx_in = din("ctx_in", [TC, D])
    cT = din("cT", [128, 2, 8])
    w_mod = din("w_mod", [DEPTH, D, 6 * D]); b_mod = din("b_mod", [DEPTH, 6 * D])
    gfm = din("gfm", [DEPTH, 128, 2, 8])
    gpost = din("gpost", [DEPTH, 2, D])
    w_in = din("w_in", [DEPTH, D, D_IN])
    bgate = din("bgate", [DEPTH, 4, 4])
    gml = din("gml", [DEPTH, 512])
    gqk = din("gqk", [DEPTH, 384])
    rpbT = din("rpbT", [DEPTH, 4, 128, 2 * 7 * 128])
    w_branch = din("w_branch", [DEPTH, 3, 512, D]); w_out = din("w_out", [DEPTH, D, D])
    w_up = din("w_up", [DEPTH, D, 2 * FFN]); w_down = din("w_down", [DEPTH, FFN, D])
    cwfm = din("cwfm", [DEPTH, 128, 2 * NFT * 3]); cbfm = din("cbfm", [DEPTH, 128, 2 * NFT])
    ropec = din("ropec", [128, 32 * 64]); ropes = din("ropes", [128, 32 * 64])
    cst = din("cst", [128, 5 * 128])
    yout = nc.dram_tensor("y", [TL, D], F32, kind="ExternalOutput").ap()
    okind = "ExternalOutput" if debug else "Internal"
    X = nc.dram_tensor("Xres", [S, D], F32, kind=okind).ap()
    YT = [nc.dram_tensor("YT%d" % n, [512, S], BF16, kind=okind).ap() for n in range(3)]
    MTm = nc.dram_tensor("MTm", [D, S], BF16, kind="Internal").ap()
    MTf = nc.dram_tensor("MTf", [FFN, S], BF16, kind="Internal").ap()

    with ExitStack() as st:
        arena_t = st.enter_context(nc.sbuf_tensor("arena", [128, arena_words], F32))
        A = Arena(arena_t, arena_words)
        PS = [st.enter_context(nc.psum_tensor("ps%d" % i, [128, 512], F32)) for i in range(8)]
        PK = ["ps%d" % i for i in range(8)]
        P = Prog(nc)
        k = K(P)

        cstt, kc = A.alloc([5 * 128], F32)
        identF = cstt[:, 0:128]; onesF = cstt[:, 128:256]
        maskd = [cstt[:, 256:384], cstt[:, 384:512]]
        eselt, kes = A.alloc([4 * 128], F32, parts=4)
        identB, kib = A.alloc([128], BF16)
        srep, ksr = A.alloc([2, 8, 128], F32)
        hT, khT = A.alloc([KC, S], BF16)
        Gbc, kG = A.alloc([2, 2, D], F32)
        fmv, kfm = A.alloc([2, 4, 8], F32)
        k.dma(cstt, cst, (), [kc])
        k.copy(identB, identF, [kc], [kib])

        eselin = din("esel", [4, 4 * 128])
        k.dma(eselt, eselin, (), [kes])

        k.dma(X[0:TC, :], ctx_in, (), ["Xi"])
        for q in range(8):
            k.dma(X[TC + q * 512:TC + (q + 1) * 512, :], x_in[q * 512:(q + 1) * 512, :], (), ["Xi%d" % q])

        m0 = A.mark()
        ct_t, kct = A.alloc([2, 8], F32)
        k.dma(ct_t, cT, (), [kct])
        k.act(ct_t, ct_t, AF.Silu, [kct], [kct])
        for r in range(2):
            for kk in range(8):
                k.ts(srep[:, r, kk, :], onesF, ct_t[:, r, kk:kk + 1], ALU.mult, [kct, kc], [ksr])
        P.barrier(); A.release(m0)

        on = lambda nm: phases is None or nm in phases

        def phase_mod(l):
            m = A.mark()
            wm = [A.alloc([8, 512], F32) for _ in range(2)]
            brow = [A.alloc([512], F32, parts=1) for _ in range(2)]
            gp = [A.alloc([512], F32) for _ in range(2)]
            tmp = [A.alloc([512], F32) for _ in range(2)]
            junk, kj = A.alloc([4, 128], F32)
            gf, kgf = A.alloc([2, 8], F32)
            k.dma(gf, gfm[l], (), [kgf])
            for cb in range(12):
                b = cb % 2
                seg = cb // 2; half = cb % 2
                k.dma(wm[b][0], w_mod[l][:, cb * 512:(cb + 1) * 512].rearrange("(k p) c -> p k c", p=128), (), [wm[b][1]])
                k.dma(brow[b][0], b_mod[l:l + 1, cb * 512:(cb + 1) * 512], (), [brow[b][1]])
                if seg in (2, 5):
                    which = 0 if seg == 2 else 1
                    k.dma(gp[b][0], gpost[l, which:which + 1, half * 512:(half + 1) * 512].to_broadcast([128, 512]), (), [gp[b][1]])
                for r in range(2):
                    ps = PS[r + 2 * b]; pk = PK[r + 2 * b]
                    for kk in range(8):
                        k.mm(ps[:], srep[:, r, kk, :], wm[b][0][:, kk, :], kk == 0, False, [ksr, wm[b][1]], [pk])
                    k.mm(ps[:], onesF[0:1, :], brow[b][0], False, True, [kc, brow[b][1]], [pk])
                    if seg in (2, 5):
                        which = 0 if seg == 2 else 1
                        k.tt(Gbc[:, r, which, half * 512:(half + 1) * 512], ps[:], gp[b][0], ALU.mult, [pk, gp[b][1]], [kG])
                    else:
                        slot = {0: 1, 1: 0, 3: 3, 4: 2}[seg]
                        tb = tmp[r]
                        k.copy(tb[0], ps[:], [pk], [tb[1]])
                        k.tt(junk, tb[0].rearrange("p (a b) -> p a b", b=128), identF.unsqueeze(1).to_broadcast([128, 4, 128]),
                             ALU.mult, [tb[1], kc], [kj])
                        k.reduce(fmv[:, r, slot, half * 4:half * 4 + 4], junk, ALU.add, [kj], [kfm])
            for r in range(2):
                for (slot, gi) in ((0, 0), (2, 1)):
                    k.stt(fmv[:, r, slot, :], fmv[:, r, slot, :], 1.0, gf[:, gi, :], ALU.add, ALU.mult, [kfm, kgf], [kfm])
            P.barrier(); A.release(m)

        def phase_norm(which):
            m = A.mark()
            xt = [A.alloc([D], F32) for _ in range(2)]
            xn = [A.alloc([D], BF16) for _ in range(2)]
            junk, kj = A.alloc([D], F32)
            ss, kss = A.alloc([NT], F32)
            for i in range(NT):
                b = i % 2
                k.dma(xt[b][0], X[i * 128:(i + 1) * 128, :], ["X"], [xt[b][1]])
                k.act(junk, xt[b][0], AF.Square, [xt[b][1]], [kj, kss], accum=ss[:, i:i + 1])
            k.ts(ss, ss, 1.0 / D, ALU.mult, [kss], [kss], s2=EPS, op1=ALU.add)
            k.act(ss, ss, AF.Sqrt, [kss], [kss])
            k.recip(ss, ss, [kss], [kss])
            for i in range(NT):
                b = i % 2
                r = 1 if i < NCT else 0
                k.dma(xt[b][0], X[i * 128:(i + 1) * 128, :], ["X"], [xt[b][1]])
                k.ts(xn[b][0], xt[b][0], ss[:, i:i + 1], ALU.mult, [xt[b][1], kss], [xn[b][1]])
                ps = PS[b]; pk = PK[b]
                psb = ps[:].bitcast(BF16).rearrange("p (a b) -> p a b", a=8)
                for kk in range(8):
                    k.tr(psb[:, kk, :], xn[b][0][:, kk * 128:(kk + 1) * 128], identB, [xn[b][1], kib], [pk])
                for kk in range(8):
                    k.act(hT[:, kk, i * 128:(i + 1) * 128], psb[:, kk, :], AF.Identity, [pk, kfm], [khT],
                          scale=fmv[:, r, 2 * which, kk:kk + 1], bias=fmv[:, r, 2 * which + 1, kk:kk + 1])
            P.barrier(); A.release(m)

        def load_w(dst, key, src_cols_ap):
            k.dma(dst, src_cols_ap.rearrange("(k p) c -> p k c", p=128), (), [key], eng="pool")

        TOKCH = [(c * 512, min(512, S - c * 512)) for c in range((S + 511) // 512)]

        def phase_mlstm(l):
            m = A.mark()
            kwT = [A.alloc([NT, 4], F32) for _ in range(2)]
            thT = [A.alloc([NT, 4], F32) for _ in range(2)]
            gbc = [A.alloc([4, NT], F32) for _ in range(2)]
            m1 = A.mark()
            wg, kwg = A.alloc([8, 16], BF16)
            load_w(wg, kwg, w_in[l][:, C_MG:C_MG + 16])
            bg, kbg = A.alloc([4], F32, parts=4)
            k.dma(bg, bgate[l], (), [kbg])
            X0, k0 = A.alloc([S], F32, parts=4); X1, k1 = A.alloc([S], F32, parts=4)
            X2, k2 = A.alloc([S], F32, parts=4); X3, k3 = A.alloc([S], F32, parts=4)
            X4, k4 = A.alloc([S], F32, parts=4)
            cm, kcm = A.alloc([NT], F32, parts=4); ri, kri = A.alloc([NT], F32, parts=4)
            rr, krr = A.alloc([NT], F32, parts=4); gd, kgd = A.alloc([NT], F32, parts=4)
            for d in range(2):
                for (j, dst, kd) in ((2 * d, X0, k0), (2 * d + 1, X1, k1)):
                    for ci, (t0, tn) in enumerate(TOKCH):
                        ps = PS[ci % 2]; pk = PK[ci % 2]
                        for kk in range(8):
                            k.mm(ps[0:4, 0:tn], wg[:, kk, 4 * j:4 * j + 4], hT[:, kk, t0:t0 + tn], kk == 0, kk == 7, [kwg, khT], [pk])
                        k.act(dst[:, t0:t0 + tn], ps[0:4, 0:tn], AF.Identity, [pk, kbg], [kd], bias=bg[:, j:j + 1], scale=1.0)
                k.act(X1, X1, AF.Exp, [k1], [k1], scale=-1.0)
                k.ts(X2, X1, 2.0, ALU.add, [k1], [k2])
                k.recip(X2, X2, [k2], [k2])
                k.tt(X2, X2, X1, ALU.mult, [k2, k1], [k2])
                k.tt(X3, X2, X2, ALU.mult, [k2], [k3])
                k.ts(X4, X3, 0.2, ALU.mult, [k3], [k4], s2=1.0 / 3.0, op1=ALU.add)
                k.tt(X4, X4, X3, ALU.mult, [k4, k3], [k4])
                k.ts(X4, X4, 1.0, ALU.add, [k4], [k4], s2=-2.0, op1=ALU.mult)
                k.tt(X4, X4, X2, ALU.mult, [k4, k2], [k4])
                if d == 0:
                    k.scan(X1, X4, X4, 0.0, ALU.add, ALU.min, [k4], [k1])
                else:
                    k.scan(X1[:, 0:TC][:, ::-1], X4[:, 0:TC][:, ::-1], X4[:, 0:TC][:, ::-1], 0.0,
                           ALU.add, ALU.min, [k4], [k1])
                    k.scan(X1[:, TC:S][:, ::-1], X4[:, TC:S][:, ::-1], X4[:, TC:S][:, ::-1], X1[:, 0:1],
                           ALU.add, ALU.min, [k4, k1], [k1])
                k.tt(X2, X0, X1, ALU.subtract, [k0, k1], [k2])
                k.reduce(cm, X2.rearrange("p (c t) -> p c t", t=128), ALU.max, [k2], [kcm])
                if d == 0:
                    k.scan(ri, cm, cm, 0.0, ALU.max, ALU.max, [kcm], [kri])
                    k.memset(rr[:, 0:1], 0.0, [krr], eng="dve")
                    k.copy(rr[:, 1:NT], ri[:, 0:NT - 1], [kri], [krr])
                else:
                    k.scan(ri[:, 0:NCT][:, ::-1], cm[:, 0:NCT][:, ::-1], cm[:, 0:NCT][:, ::-1], 0.0, ALU.max, ALU.max, [kcm], [kri])
                    k.scan(ri[:, NCT:NT][:, ::-1], cm[:, NCT:NT][:, ::-1], cm[:, NCT:NT][:, ::-1], ri[:, 0:1], ALU.max, ALU.max,
                           [kcm, kri], [kri])
                    k.memset(rr[:, NCT - 1:NCT], 0.0, [krr], eng="dve")
                    k.copy(rr[:, 0:NCT - 1], ri[:, 1:NCT], [kri], [krr])
                    k.copy(rr[:, NT - 1:NT], ri[:, 0:1], [kri], [krr])
                    k.copy(rr[:, NCT:NT - 1], ri[:, NCT + 1:NT], [kri], [krr])
                rrb = rr.unsqueeze(2).to_broadcast([4, NT, 128])
                k.tt(X3.rearrange("p (c t) -> p c t", t=128), X2.rearrange("p (c t) -> p c t", t=128), rrb, ALU.subtract, [k2, krr], [k3])
                k.act(X3, X3, AF.Exp, [k3], [k3])
                k.tt(X0.rearrange("p (c t) -> p c t", t=128), X1.rearrange("p (c t) -> p c t", t=128), rrb, ALU.add, [k1, krr], [k0])
                k.act(X0, X0, AF.Exp, [k0], [k0], scale=-1.0)
                k.tt(gd, rr, ri, ALU.subtract, [krr, kri], [kgd])
                k.act(gd, gd, AF.Exp, [kgd], [kgd])
                for (src, ks, dstp) in ((X3, k3, kwT[d]), (X0, k0, thT[d])):
                    ps = PS[2]; pk = PK[2]
                    for c in range(NT):
                        k.tr(ps[:, c * 4:(c + 1) * 4], src[:, c * 128:(c + 1) * 128], identF[0:4, 0:4], [ks, kc], [pk])
                    k.copy(dstp[0], ps[:, 0:NT * 4].rearrange("p (c h) -> p c h", h=4), [pk], [dstp[1]])
                ps = PS[3]; pk = PK[3]
                for h in range(4):
                    k.mm(ps[:, h * NT:(h + 1) * NT], eselt[:, h * 128:(h + 1) * 128], gd, True, True, [kes, kgd], [pk])
                k.copy(gbc[d][0], ps[:, 0:4 * NT].rearrange("p (h c) -> p h c", h=4), [pk], [gbc[d][1]])
            P.barrier(); A.release(m1)

            gmlb, kgm = A.alloc([512], F32)
            k.dma(gmlb, gml[l:l + 1, :].to_broadcast([128, 512]), (), [kgm])
            for h in range(4 if on("ml_heads") else 0):
                m2 = A.mark()
                wq, kwq = A.alloc([8, 128], BF16); wk_, kwk = A.alloc([8, 128], BF16)
                wkvo, kwkvo = A.alloc([8, 384], BF16)
                load_w(wq, kwq, w_in[l][:, C_MQ + h * 128:C_MQ + (h + 1) * 128])
                load_w(wk_, kwk, w_in[l][:, C_MK + h * 128:C_MK + (h + 1) * 128])
                load_w(wkvo[:, :, 0:128], kwkvo, w_in[l][:, C_MK + h * 128:C_MK + (h + 1) * 128])
                load_w(wkvo[:, :, 128:256], kwkvo, w_in[l][:, C_MV + h * 128:C_MV + (h + 1) * 128])
                load_w(wkvo[:, :, 256:384], kwkvo, w_in[l][:, C_MO + h * 128:C_MO + (h + 1) * 128])
                QT, kQT = A.alloc([S], BF16); KT, kKT = A.alloc([S], BF16)
                Ktm, kKtm = A.alloc([NT, 128], BF16); Va, kVa = A.alloc([NT, 130], BF16)
                Osg, kOs = A.alloc([NT, 128], BF16); Hacc, kH = A.alloc([NT, 128], F32)
                k.memset(Va[:, :, 128:129], 1.0, [kVa])
                k.memset(Hacc, 0.0, [kH + ".%d" % c for c in range(NT)])
                for ci, (t0, tn) in enumerate(TOKCH if on("ml_fm") else []):
                    for (wt, kwt, dst, kd, sc, pi) in ((wq, kwq, QT, kQT, 1.0, 0), (wk_, kwk, KT, kKT, 128.0 ** -0.5, 1)):
                        ps = PS[pi + 2 * (ci % 2)]; pk = PK[pi + 2 * (ci % 2)]
                        for kk in range(8):
                            k.mm(ps[:, 0:tn], wt[:, kk, :], hT[:, kk, t0:t0 + tn], kk == 0, kk == 7, [kwt, khT], [pk])
                        k.act(dst[:, t0:t0 + tn], ps[:, 0:tn], AF.Identity, [pk], [kd], scale=sc)
                for i in range(NT if on("ml_tm") else 0):
                    ps = PS[4 + i % 2]; pk = PK[4 + i % 2]
                    for kk in range(8):
                        k.mm(ps[:, 0:384], hT[:, kk, i * 128:(i + 1) * 128], wkvo[:, kk, :], kk == 0, kk == 7, [khT, kwkvo], [pk])
                    k.act(Ktm[:, i, :], ps[:, 0:128], AF.Identity, [pk], [kKtm], scale=128.0 ** -0.5)
                    k.copy(Va[:, i, 0:128], ps[:, 128:256], [pk], [kVa])
                    k.act(Osg[:, i, :], ps[:, 256:384], AF.Sigmoid, [pk], [kOs])
                Cf = [A.alloc([129], F32) for _ in range(2)]; Cb = [A.alloc([130], BF16) for _ in range(2)]
                PmT = [A.alloc([128], BF16) for _ in range(2)]; Kp = [A.alloc([128], BF16) for _ in range(2)]
                dd = [A.alloc([2], F32) for _ in range(2)]
                for d in range(2):
                    k.memset(Cf[d][0], 0.0, [Cf[d][1]]); k.memset(Cb[d][0], 0.0, [Cb[d][1]])
                order = [list(range(NT)), [1, 0] + list(range(NT - 1, NCT - 1, -1))]
                for step in range(NT if on("ml_rec") else 0):
                    cc = [order[d][step] for d in range(2)]
                    notlast = step < NT - 1
                    for d in range(2):
                        c = cc[d]
                        if notlast:
                            k.act(Kp[d][0], Ktm[:, c, :], AF.Identity, [kKtm, kwT[d][1]], [Kp[d][1]], scale=kwT[d][0][:, c, h:h + 1])
                    for d in range(2):
                        c = cc[d]; cs = slice(c * 128, (c + 1) * 128)
                        k.mm(PS[0 + d][:, 0:128], KT[:, cs], QT[:, cs], True, True, [kKT, kQT], [PK[0 + d]])
                    for d in range(2):
                        c = cc[d]
                        if notlast:
                            k.mm(PS[6 + d][:, 0:129], Kp[d][0], Va[:, c, 0:129], True, True, [Kp[d][1], kVa], [PK[6 + d]])
                    for d in range(2):
                        c = cc[d]
                        k.stt(PmT[d][0], PS[0 + d][:, 0:128], kwT[d][0][:, c, h:h + 1], maskd[d], ALU.mult, ALU.mult,
                              [PK[0 + d], kwT[d][1], kc], [PmT[d][1]])
                    for d in range(2):
                        c = cc[d]; cs = slice(c * 128, (c + 1) * 128)
                        pO, kO = PS[2 + d], PK[2 + d]
                        k.mm(pO[:, 0:129], PmT[d][0], Va[:, c, 0:129], True, False, [PmT[d][1], kVa], [kO])
                        k.mm(pO[:, 0:129], QT[:, cs], Cb[d][0][:, 0:129], False, True, [kQT, Cb[d][1]], [kO])
                    for d in range(2):
                        c = cc[d]
                        pO, kO = PS[2 + d], PK[2 + d]
                        k.act(dd[d][0][:, 0:1], pO[:, 128:129], AF.Abs, [kO], [dd[d][1]])
                        k.tt(dd[d][0][:, 0:1], dd[d][0][:, 0:1], thT[d][0][:, c, h:h + 1], ALU.max, [dd[d][1], thT[d][1]], [dd[d][1]])
                        k.recip(dd[d][0][:, 1:2], dd[d][0][:, 0:1], [dd[d][1]], [dd[d][1]])
                        k.stt(Hacc[:, c, :], pO[:, 0:128], dd[d][0][:, 1:2], Hacc[:, c, :], ALU.mult, ALU.add,
                              [kO, dd[d][1], kH + ".%d" % c], [kH + ".%d" % c])
                    for d in range(2):
                        c = cc[d]
                        if notlast:
                            pU, kU = PS[6 + d], PK[6 + d]
                            k.tt(Cf[d][0], pU[:, 0:129], Cf[d][0], ALU.add, [kU, Cf[d][1]], [Cf[d][1]])
                            k.ts(Cf[d][0], Cf[d][0], gbc[d][0][:, h, c:c + 1], ALU.mult, [Cf[d][1], gbc[d][1]], [Cf[d][1]])
                            k.act(Cb[d][0][:, 0:129], Cf[d][0], AF.Identity, [Cf[d][1]], [Cb[d][1]])
                ssq, kssq = A.alloc([NT], F32)
                sq, ksq = A.alloc([8, 128], F32); yb, kyb = A.alloc([8, 128], BF16); ys, kys = A.alloc([8 * 128], BF16)
                hk_all = [kH + ".%d" % c for c in range(NT)]
                for g0 in range(0, NT if on("ml_fin") else 0, 8):
                    gn = min(8, NT - g0)
                    hv = Hacc[:, g0:g0 + gn, :]
                    k.tt(sq[:, 0:gn, :], hv, hv, ALU.mult, hk_all, [ksq])
                    k.reduce(ssq[:, g0:g0 + gn], sq[:, 0:gn, :], ALU.add, [ksq], [kssq])
                if on("ml_fin"):
                    k.ts(ssq, ssq, 1.0 / 128, ALU.mult, [kssq], [kssq], s2=EPS, op1=ALU.add)
                    k.act(ssq, ssq, AF.Sqrt, [kssq], [kssq])
                    k.recip(ssq, ssq, [kssq], [kssq])
                for g0 in range(0, NT if on("ml_fin") else 0, 8):
                    gn = min(8, NT - g0)
                    hv = Hacc[:, g0:g0 + gn, :]
                    k.tt(sq[:, 0:gn, :], hv, ssq[:, g0:g0 + gn].unsqueeze(2).to_broadcast([128, gn, 128]), ALU.mult, hk_all + [kssq], [ksq])
                    k.tt(sq[:, 0:gn, :], sq[:, 0:gn, :], gmlb[:, h * 128:(h + 1) * 128].unsqueeze(1).to_broadcast([128, gn, 128]),
                         ALU.mult, [ksq, kgm], [ksq])
                    k.tt(yb[:, 0:gn, :], sq[:, 0:gn, :], Osg[:, g0:g0 + gn, :], ALU.mult, [ksq, kOs], [kyb])
                    ps = PS[4 + (g0 // 8) % 2]; pk = PK[4 + (g0 // 8) % 2]
                    psb = ps[:].bitcast(BF16)
                    for q in range(gn):
                        k.tr(psb[:, q * 128:(q + 1) * 128], yb[:, q, :], identB, [kyb, kib], [pk])
                    k.copy(ys[:, 0:gn * 128], psb[:, 0:gn * 128], [pk], [kys])
                    k.dma(YT[0][h * 128:(h + 1) * 128, g0 * 128:(g0 + gn) * 128], ys[:, 0:gn * 128], [kys], ["YT0"])
                P.barrier(); A.release(m2)
            P.barrier(); A.release(m)

        def attn_finalize(pO, kO, n, yb_ap, kyb, tmpf, pB, kB):
            rden, krd, osb, kosb = tmpf
            k.act(rden[64:65, 0:n], pO[64:65, 0:n], AF.Ln, [kO], [krd])
            k.act(rden[64:65, 0:n], rden[64:65, 0:n], AF.Exp, [krd], [krd], scale=-1.0)
            k.mm(pB[0:64, 0:n], onesF[64:65, 0:64], rden[64:65, 0:n], True, True, [kc, krd], [kB])
            k.act(osb[0:64, 0:n], pO[0:64, 0:n], AF.Identity, [kO], [kosb])
            k.tt(yb_ap, osb[0:64, 0:n], pB[0:64, 0:n], ALU.mult, [kosb, kB], [kyb])

        def phase_gqa(l, last):
            m = A.mark()
            rc, krc = A.alloc([32, 64], F32); rs, krs = A.alloc([32, 64], F32)
            k.dma(rc, ropec.rearrange("p (a b) -> p a b", b=64), (), [krc])
            k.dma(rs, ropes.rearrange("p (a b) -> p a b", b=64), (), [krs])
            gqf, kgq = A.alloc([384], F32)
            k.dma(gqf, gqk[l:l + 1, :].to_broadcast([128, 384]), (), [kgq])
            gq = gqf.rearrange("p (a b) -> p a b", b=64)
            rden, krd = A.alloc([512], F32); osb, kosb = A.alloc([512], F32)
            for g in range(2):
                m2 = A.mark()
                w, kw_ = A.alloc([8, 448], BF16)
                load_w(w[:, :, 0:256], kw_, w_in[l][:, C_AQ + g * 256:C_AQ + (g + 1) * 256])
                load_w(w[:, :, 256:320], kw_, w_in[l][:, C_AK + g * 64:C_AK + (g + 1) * 64])
                load_w(w[:, :, 320:384], kw_, w_in[l][:, C_AK + g * 64:C_AK + (g + 1) * 64])
                load_w(w[:, :, 384:448], kw_, w_in[l][:, C_AV + g * 64:C_AV + (g + 1) * 64])
                QTK, kQ = A.alloc([3, S], BF16); Vg, kV = A.alloc([NT, 66], BF16)
                k.memset(Vg[:, :, 64:65], 1.0, [kV])
                sq, ksq = A.alloc([6, 64], F32); t1, kt1 = A.alloc([6, 64], F32); xr, kxr = A.alloc([6, 64], F32)
                sw, ksw = A.alloc([6, 64], F32); qr, kqr = A.alloc([384], BF16); st5, kst = A.alloc([6], F32)
                def g_proj(i):
                    ps = PS[i % 2]; pk = PK[i % 2]
                    for kk in range(8):
                        k.mm(ps[:, 0:448], hT[:, kk, i * 128:(i + 1) * 128], w[:, kk, :], kk == 0, kk == 7, [khT, kw_], [pk])
                g_proj(0)
                for i in range(NT):
                    ps = PS[i % 2]; pk = PK[i % 2]
                    if i + 1 < NT:
                        g_proj(i + 1)
                    psv = ps[:, 0:384].rearrange("p (a b) -> p a b", b=64)
                    k.act(sq, psv, AF.Square, [pk], [ksq])
                    k.reduce(st5, sq, ALU.add, [ksq], [kst])
                    k.ts(st5, st5, 1.0 / 64, ALU.mult, [kst], [kst], s2=EPS, op1=ALU.add)
                    k.act(st5, st5, AF.Ln, [kst], [kst])
                    k.act(st5, st5, AF.Exp, [kst], [kst], scale=-0.5)
                    k.tt(t1, psv, st5.unsqueeze(2).to_broadcast([128, 6, 64]), ALU.mult, [pk, kst], [kt1])
                    k.tt(t1, t1, gq, ALU.mult, [kt1, kgq], [kt1])
                    k.copy(Vg[:, i, 0:64], ps[:, 384:448], [pk], [kV], eng="dve")
                    if i >= NCT:
                        lt = i - NCT
                        cb_ = rc[:, lt, :].unsqueeze(1).to_broadcast([128, 6, 64])
                        k.tt(xr, t1, cb_, ALU.mult, [kt1, krc], [kxr])
                        t1v = t1.rearrange("p h (a q e) -> p h a q e", a=2, q=2)
                        swv = sw.rearrange("p h (a q e) -> p h a q e", a=2, q=2)
                        rsv = rs[:, lt, :].rearrange("p (a q e) -> p a q e", a=2, q=2)
                        for qd in range(2):
                            k.tt(swv[:, :, :, qd, :], t1v[:, :, :, 1 - qd, :],
                                 rsv[:, :, qd, :].unsqueeze(1).to_broadcast([128, 6, 2, 16]), ALU.mult, [kt1, krs], [ksw])
                        k.tt(qr.rearrange("p (a b) -> p a b", b=64), xr, sw, ALU.add, [kxr, ksw], [kqr])
                    else:
                        k.copy(qr.rearrange("p (a b) -> p a b", b=64), t1, [kt1], [kqr])
                    pt = PS[2 + i % 2]; pkt = PK[2 + i % 2]
                    ptb = pt[:].bitcast(BF16)
                    for q in range(3):
                        k.tr(ptb[:, q * 128:(q + 1) * 128], qr[:, q * 128:(q + 1) * 128], identB, [kqr, kib], [pkt])
                    k.act(QTK[:, :, i * 128:(i + 1) * 128], ptb[:, 0:384].rearrange("p (a b) -> p a b", b=128), AF.Identity, [pkt], [kQ])
                PT = [A.alloc([512], BF16) for _ in range(4)]
                yb, kyb = A.alloc([512], BF16)
                jobs = []
                if not last:
                    for qh in range(4):
                        jobs.append((qh, 0, TC, list(range(NCT))))
                for qh in range(4):
                    for qc in range(TL // 512):
                        jobs.append((qh, TC + qc * 512, 512, list(range(NT))))
                items = []
                for ji, (qh, q0, qn, kts) in enumerate(jobs):
                    for ki, kt in enumerate(kts):
                        items.append((ji, ki, kt))

                def g_qk(idx):
                    ji, ki, kt = items[idx]
                    qh, q0, qn, kts = jobs[ji]
                    j = qh // 2; hb = 64 * (qh % 2)
                    pS, kS = PS[idx % 4], PK[idx % 4]
                    k.mm(pS[:, 0:qn], QTK[hb:hb + 64, 2, kt * 128:(kt + 1) * 128], QTK[hb:hb + 64, j, q0:q0 + qn], True, True, [kQ], [kS])

                def g_rest(idx):
                    ji, ki, kt = items[idx]
                    qh, q0, qn, kts = jobs[ji]
                    pS, kS = PS[idx % 4], PK[idx % 4]
                    ptile = PT[idx % 4]
                    pO, kO = PS[4 + ji % 2], PK[4 + ji % 2]
                    k.act(ptile[0][:, 0:qn], pS[:, 0:qn], AF.Exp, [kS], [ptile[1]], scale=0.125)
                    k.mm(pO[0:65, 0:qn], Vg[:, kt, 0:65], ptile[0][:, 0:qn], ki == 0, ki == len(kts) - 1, [kV, ptile[1]], [kO])

                def g_fin(ji):
                    qh, q0, qn, kts = jobs[ji]
                    pO, kO = PS[4 + ji % 2], PK[4 + ji % 2]
                    attn_finalize(pO, kO, qn, yb[0:64, 0:qn], kyb, (rden, krd, osb, kosb), PS[6 + ji % 2], PK[6 + ji % 2])
                    hd = 4 * g + qh
                    k.dma(YT[1][hd * 64:(hd + 1) * 64, q0:q0 + qn], yb[0:64, 0:qn], [kyb], ["YT1"])

                pending = None
                g_qk(0)
                if len(items) > 1:
                    g_qk(1)
                for idx in range(len(items)):
                    if idx + 2 < len(items):
                        g_qk(idx + 2)
                    g_rest(idx)
                    ji, ki, kt = items[idx]
                    if pending is not None and pending[1] == idx:
                        g_fin(pending[0]); pending = None
                    if ki == len(jobs[ji][3]) - 1:
                        if idx + 2 < len(items):
                            pending = (ji, idx + 2)
                        else:
                            if pending is not None:
                                g_fin(pending[0]); pending = None
                            g_fin(ji)
                if pending is not None:
                    g_fin(pending[0]); pending = None
                P.barrier(); A.release(m2)
            P.barrier(); A.release(m)

        def phase_na(l, last):
            m = A.mark()
            rden, krd = A.alloc([512], F32); osb, kosb = A.alloc([512], F32)
            VnA, kVn = A.alloc([NT, 8, 66], BF16)
            k.memset(VnA[:, :, :, 64:65], 1.0, [kVn])
            mv_ = A.mark()
            wv, kwv = A.alloc([8, 512], BF16)
            load_w(wv, kwv, w_in[l][:, C_NV:C_NV + 512])
            for i in range(NT):
                ps = PS[4 + i % 2]; pk = PK[4 + i % 2]
                for kk in range(8):
                    k.mm(ps[:, 0:512], hT[:, kk, i * 128:(i + 1) * 128], wv[:, kk, :], kk == 0, kk == 7, [khT, kwv], [pk])
                k.copy(VnA[:, i, :, 0:64], ps[:, 0:512].rearrange("p (a b) -> p a b", b=64), [pk], [kVn])
            P.barrier(); A.release(mv_)
            for hp in range(4):
                m2 = A.mark()
                wq, kwq = A.alloc([8, 128], BF16); wk_, kwk = A.alloc([8, 128], BF16)
                load_w(wq, kwq, w_in[l][:, C_NQ + hp * 128:C_NQ + (hp + 1) * 128])
                load_w(wk_, kwk, w_in[l][:, C_NK + hp * 128:C_NK + (hp + 1) * 128])
                Vn = VnA[:, :, 2 * hp:2 * hp + 2, :]
                Tb, kTb = A.alloc([2, 7, 128], F32)
                k.dma(Tb, rpbT[l, hp].rearrange("p (a b c) -> p a b c", a=2, b=7), (), [kTb])
                QT, kQT = A.alloc([S], BF16); KT, kKT = A.alloc([S], BF16)
                for ci, (t0, tn) in enumerate(TOKCH):
                    for (wt, kwt, dst, kd, sc, pi) in ((wq, kwq, QT, kQT, 0.125, 0), (wk_, kwk, KT, kKT, 1.0, 1)):
                        ps = PS[pi + 2 * (ci % 2)]; pk = PK[pi + 2 * (ci % 2)]
                        for kk in range(8):
                            k.mm(ps[:, 0:tn], wt[:, kk, :], hT[:, kk, t0:t0 + tn], kk == 0, kk == 7, [kwt, khT], [pk])
                        k.act(dst[:, t0:t0 + tn], ps[:, 0:tn], AF.Identity, [pk], [kd], scale=sc)
                sbs = [A.alloc([640], F32) for _ in range(2)]
                PT = [A.alloc([896], BF16) for _ in range(2)]
                ynb, kyn = A.alloc([2, TL], BF16, parts=64)
                ycb, kyc = A.alloc([2, TC], BF16, parts=64)
                it = 0
                if not last:
                    for hh in range(2):
                        hb = 64 * hh
                        pS, kS = PS[0], PK[0]; pO, kO = PS[4 + hh], PK[4 + hh]
                        for kt in range(NCT):
                            k.mm(pS[:, kt * 256:(kt + 1) * 256], KT[hb:hb + 64, kt * 128:(kt + 1) * 128], QT[hb:hb + 64, 0:TC], True, True, [kKT, kQT], [kS])
                        ptile = PT[it % 2]; it += 1
                        k.act(ptile[0][:, 0:512], pS[:, 0:512], AF.Exp, [kS], [ptile[1]])
                        for kt in range(NCT):
                            k.mm(pO[0:65, 0:TC], Vn[:, kt, hh, 0:65], ptile[0][:, kt * 256:(kt + 1) * 256], kt == 0, kt == NCT - 1, [kVn, ptile[1]], [kO])
                        attn_finalize(pO, kO, TC, ycb[:, hh, :], kyc, (rden, krd, osb, kosb), PS[6 + hh], PK[6 + hh])
                    for hh in range(2):
                        hd = 2 * hp + hh
                        k.dma(YT[2][hd * 64:(hd + 1) * 64, 0:TC], ycb[:, hh, :], [kyc], ["YT2"])
                def blk(ji):
                    rb, hh = ji // 2, ji % 2
                    q0 = TC + rb * 128
                    r0a, r0b = r0_of(2 * rb), r0_of(2 * rb + 1)
                    tiles = list(range(r0a // 2, (r0b + 7) // 2 + 1))
                    nw = len(tiles)
                    di0 = (2 * tiles[0] - 2 * rb + 6) // 2
                    assert 0 <= di0 and di0 + nw <= 7 and nw <= 5
                    return rb, hh, q0, tiles, nw, di0

                def bufs(ji):
                    return (PS[2 * (ji % 2)], PK[2 * (ji % 2)], PS[2 * (ji % 2) + 1], PK[2 * (ji % 2) + 1],
                            PS[4 + ji % 2], PK[4 + ji % 2], sbs[ji % 2], PT[ji % 2])

                def n_qk(ji):
                    rb, hh, q0, tiles, nw, di0 = blk(ji)
                    hb = 64 * hh
                    pSa, kSa, pSb, kSb, pO, kO, sb_, ptile = bufs(ji)
                    for jw, t in enumerate(tiles):
                        pp, kp, co = (pSa, kSa, jw * 128) if jw < 4 else (pSb, kSb, (jw - 4) * 128)
                        kt = NCT + t
                        k.mm(pp[:, co:co + 128], KT[hb:hb + 64, kt * 128:(kt + 1) * 128], QT[hb:hb + 64, q0:q0 + 128], True, True, [kKT, kQT], [kp])
                    for kt in range(NCT):
                        k.mm(pSb[:, 128 + kt * 128:256 + kt * 128], KT[hb:hb + 64, kt * 128:(kt + 1) * 128], QT[hb:hb + 64, q0:q0 + 128], True, True, [kKT, kQT], [kSb])

                def n_soft(ji):
                    rb, hh, q0, tiles, nw, di0 = blk(ji)
                    pSa, kSa, pSb, kSb, pO, kO, sb_, ptile = bufs(ji)
                    n4 = min(nw, 4)
                    k.tt(sb_[0][:, 0:n4 * 128], pSa[:, 0:n4 * 128], Tb[:, hh, di0:di0 + n4, :].rearrange("p a b -> p (a b)"), ALU.add, [kSa, kTb], [sb_[1]])
                    if nw > 4:
                        k.tt(sb_[0][:, 512:640], pSb[:, 0:128], Tb[:, hh, di0 + 4, :], ALU.add, [kSb, kTb], [sb_[1]])
                    k.act(ptile[0][:, 0:nw * 128], sb_[0][:, 0:nw * 128], AF.Exp, [sb_[1]], [ptile[1]])
                    k.act(ptile[0][:, 640:896], pSb[:, 128:384], AF.Exp, [kSb], [ptile[1]])
                    for jw, t in enumerate(tiles):
                        for a in range(2):
                            for b in range(2):
                                kr = 2 * t + a; qrow = 2 * rb + b
                                if not (r0_of(qrow) <= kr <= r0_of(qrow) + 7):
                                    k.memset(ptile[0][64 * a:64 * a + 64, jw * 128 + 64 * b:jw * 128 + 64 * b + 64], 0.0, [ptile[1]])

                def n_pv(ji):
                    rb, hh, q0, tiles, nw, di0 = blk(ji)
                    pSa, kSa, pSb, kSb, pO, kO, sb_, ptile = bufs(ji)
                    for jw, t in enumerate(tiles):
                        k.mm(pO[0:65, 0:128], Vn[:, NCT + t, hh, 0:65], ptile[0][:, jw * 128:(jw + 1) * 128], jw == 0, False, [kVn, ptile[1]], [kO])
                    for kt in range(NCT):
                        k.mm(pO[0:65, 0:128], Vn[:, kt, hh, 0:65], ptile[0][:, 640 + kt * 128:768 + kt * 128], False, kt == NCT - 1, [kVn, ptile[1]], [kO])

                def n_fin(ji):
                    rb, hh, q0, tiles, nw, di0 = blk(ji)
                    pSa, kSa, pSb, kSb, pO, kO, sb_, ptile = bufs(ji)
                    attn_finalize(pO, kO, 128, ynb[:, hh, rb * 128:(rb + 1) * 128], kyn, (rden, krd, osb, kosb), PS[6 + ji % 2], PK[6 + ji % 2])

                NJ = 64
                n_qk(0)
                for ji in range(NJ):
                    if ji + 1 < NJ:
                        n_qk(ji + 1)
                    n_soft(ji)
                    n_pv(ji)
                    if ji >= 1:
                        n_fin(ji - 1)
                n_fin(NJ - 1)
                for hh in range(2):
                    hd = 2 * hp + hh
                    k.dma(YT[2][hd * 64:(hd + 1) * 64, TC:S], ynb[:, hh, :], [kyn], ["YT2"])
                P.barrier(); A.release(m2)
            P.barrier(); A.release(m)

        def phase_merge(l):
            m = A.mark()
            wz = [A.alloc([8, 3, 128], BF16) for _ in range(2)]
            wb = [A.alloc([3, 4, 128], BF16) for _ in range(2)]
            yt = [A.alloc([3, 4, 512], BF16) for _ in range(2)]
            sg = [A.alloc([512], F32) for _ in range(2)]
            acc, kacc = A.alloc([512], F32); tmp, ktmp = A.alloc([512], F32)
            mo = [A.alloc([512], BF16) for _ in range(2)]
            it = 0
            def ld_w(ct):
                b = ct % 2
                for n in range(3):
                    load_w(wz[b][0][:, :, n, :], wz[b][1], w_in[l][:, C_ZG + n * D + ct * 128:C_ZG + n * D + (ct + 1) * 128])
                    load_w(wb[b][0][:, n, :, :], wb[b][1], w_branch[l, n][:, ct * 128:(ct + 1) * 128])
            def ld_y(itx):
                t0, tn = TOKCH[itx % len(TOKCH)]
                yb_ = yt[itx % 2]
                for n in range(3):
                    k.dma(yb_[0][:, n, :, 0:tn], YT[n][:, t0:t0 + tn].rearrange("(k p) c -> p k c", p=128), ["YT%d" % n], [yb_[1]])
            ld_w(0)
            ld_y(0)
            for ct in range(8):
                b = ct % 2
                if ct + 1 < 8:
                    ld_w(ct + 1)
                for ci, (t0, tn) in enumerate(TOKCH):
                    yb_ = yt[it % 2]; mo_ = mo[it % 2]; it += 1
                    if it < 8 * len(TOKCH):
                        ld_y(it)
                    for n in range(3):
                        pg, kg = PS[2 * (n % 2)], PK[2 * (n % 2)]
                        pp, kp = PS[2 * (n % 2) + 1], PK[2 * (n % 2) + 1]
                        for kk in range(8):
                            k.mm(pg[:, 0:tn], wz[b][0][:, kk, n, :], hT[:, kk, t0:t0 + tn], kk == 0, kk == 7, [wz[b][1], khT], [kg])
                        for kk in range(4):
                            k.mm(pp[:, 0:tn], wb[b][0][:, n, kk, :], yb_[0][:, n, kk, 0:tn], kk == 0, kk == 3, [wb[b][1], yb_[1]], [kp])
                        sg_ = sg[n % 2]
                        k.act(sg_[0][:, 0:tn], pg[:, 0:tn], AF.Sigmoid, [kg], [sg_[1]])
                        if n == 0:
                            k.tt(acc[:, 0:tn], sg_[0][:, 0:tn], pp[:, 0:tn], ALU.mult, [sg_[1], kp], [kacc])
                        else:
                            k.tt(tmp[:, 0:tn], sg_[0][:, 0:tn], pp[:, 0:tn], ALU.mult, [sg_[1], kp], [ktmp])
                            if n == 1:
                                k.tt(acc[:, 0:tn], acc[:, 0:tn], tmp[:, 0:tn], ALU.add, [kacc, ktmp], [kacc])
                            else:
                                k.tt(mo_[0][:, 0:tn], acc[:, 0:tn], tmp[:, 0:tn], ALU.add, [kacc, ktmp], [mo_[1]])
                    k.dma(MTm[ct * 128:(ct + 1) * 128, t0:t0 + tn], mo_[0][:, 0:tn], [mo_[1]], ["MTm"])
            P.barrier(); A.release(m)

        def phase_proj_res(MT, mtkey, nk, wsrc, which, tiles):
            m = A.mark()
            wd, kwd = A.alloc([nk, D], BF16)
            for kk0 in range(0, nk, 8):
                kn = min(8, nk - kk0)
                load_w(wd[:, kk0:kk0 + kn, :], kwd, wsrc[kk0 * 128:(kk0 + kn) * 128, :])
            mt = [A.alloc([nk, 128], BF16) for _ in range(2)]
            xt = [A.alloc([D], F32) for _ in range(2)]
            tt_ = [A.alloc([D], F32) for _ in range(2)]
            junk, kj = A.alloc([512], F32)
            ssv = [A.alloc([4], F32) for _ in range(2)]
            def ld_t(ii):
                i = tiles[ii]; b = ii % 2
                k.dma(mt[b][0], MT[:, i * 128:(i + 1) * 128].rearrange("(k p) c -> p k c", p=128), [mtkey], [mt[b][1]])
                k.dma(xt[b][0], X[i * 128:(i + 1) * 128, :], ["X.%d" % i], [xt[b][1]])
            ld_t(0)
            for ii, i in enumerate(tiles):
                b = ii % 2
                r = 1 if i < NCT else 0
                if ii + 1 < len(tiles):
                    ld_t(ii + 1)
                ph = [(PS[4 * b + hf], PK[4 * b + hf]) for hf in range(2)]
                for hf in range(2):
                    for kk in range(nk):
                        k.mm(ph[hf][0][:], mt[b][0][:, kk, :], wd[:, kk, hf * 512:(hf + 1) * 512], kk == 0, kk == nk - 1, [mt[b][1], kwd], [ph[hf][1]])
                sv, ksv = ssv[b]
                for hf in range(2):
                    k.act(junk, ph[hf][0][:], AF.Square, [ph[hf][1]], [kj, ksv], accum=sv[:, hf:hf + 1])
                k.tt(sv[:, 2:3], sv[:, 0:1], sv[:, 1:2], ALU.add, [ksv], [ksv])
                k.ts(sv[:, 2:3], sv[:, 2:3], 1.0 / D, ALU.mult, [ksv], [ksv], s2=EPS, op1=ALU.add)
                k.act(sv[:, 3:4], sv[:, 2:3], AF.Ln, [ksv], [ksv])
                k.act(sv[:, 3:4], sv[:, 3:4], AF.Exp, [ksv], [ksv], scale=-0.5)
                for hf in range(2):
                    cs = slice(hf * 512, (hf + 1) * 512)
                    k.stt(tt_[b][0][:, cs], ph[hf][0][:], sv[:, 3:4], Gbc[:, r, which, cs], ALU.mult, ALU.mult, [ph[hf][1], ksv, kG], [tt_[b][1]])
                k.tt(tt_[b][0], tt_[b][0], xt[b][0], ALU.add, [tt_[b][1], xt[b][1]], [tt_[b][1]], eng="pool")
                k.dma(X[i * 128:(i + 1) * 128, :], tt_[b][0], [tt_[b][1]], ["X.%d" % i, "X"])
            P.barrier(); A.release(m)

        def phase_ffn_up(l, lo_tok):
            m = A.mark()
            LB = S + 4
            cw, kcw = A.alloc([2 * NFT, 3], F32); cbb, kcb = A.alloc([2 * NFT], F32)
            k.dma(cw, cwfm[l].rearrange("p (a b) -> p a b", b=3), (), [kcw])
            k.dma(cbb, cbfm[l], (), [kcb])
            UU = [[A.alloc([LB], BF16) for _ in range(2)] for _ in range(2)]
            T = [A.alloc([LB], F32) for _ in range(2)]
            mb, kmb = A.alloc([LB], BF16)
            w = [A.alloc([8, 2, 128], BF16) for _ in range(2)]
            for bb_ in range(2):
                for z in range(2):
                    k.memset(UU[bb_][z][0], 0.0, [UU[bb_][z][1]])
            chunks = []
            if lo_tok == 0:
                chunks.append((0, TC, 1))
            for qc in range(TL // 512):
                chunks.append((TC + qc * 512, 512, 3 + TC + qc * 512))
            lo_c = 1 if lo_tok == 0 else 3 + TC
            hi_c = LB - 1
            it = 0
            for j in range(NFT):
                b = j % 2
                U = UU[b]
                load_w(w[b][0][:, :, 0, :], w[b][1], w_up[l][:, j * 128:(j + 1) * 128])
                load_w(w[b][0][:, :, 1, :], w[b][1], w_up[l][:, FFN + j * 128:FFN + (j + 1) * 128])
                for (t0, tn, c0) in chunks:
                    for z in range(2):
                        ps, pk = PS[it % 4], PK[it % 4]; it += 1
                        for kk in range(8):
                            k.mm(ps[:, 0:tn], w[b][0][:, kk, z, :], hT[:, kk, t0:t0 + tn], kk == 0, kk == 7, [w[b][1], khT], [pk])
                        k.act(U[z][0][:, c0:c0 + tn], ps[:, 0:tn], AF.Identity, [pk], [U[z][1]])
                n = hi_c - lo_c
                for z in range(2):
                    ch = z * NFT + j
                    k.act(T[z][0][:, lo_c:hi_c], U[z][0][:, lo_c:hi_c], AF.Identity, [U[z][1], kcw, kcb], [T[z][1]],
                          scale=cw[:, ch, 1:2], bias=cbb[:, ch:ch + 1])
                    k.stt(T[z][0][:, lo_c:hi_c], U[z][0][:, lo_c - 1:hi_c - 1], cw[:, ch, 0:1], T[z][0][:, lo_c:hi_c], ALU.mult, ALU.add,
                          [U[z][1], kcw, T[z][1]], [T[z][1]])
                    k.stt(T[z][0][:, lo_c:hi_c], U[z][0][:, lo_c + 1:hi_c + 1], cw[:, ch, 2:3], T[z][0][:, lo_c:hi_c], ALU.mult, ALU.add,
                          [U[z][1], kcw, T[z][1]], [T[z][1]])
                k.act(T[1][0][:, lo_c:hi_c], T[1][0][:, lo_c:hi_c], AF.Silu, [T[1][1]], [T[1][1]])
                k.tt(mb[:, lo_c:hi_c], T[0][0][:, lo_c:hi_c], T[1][0][:, lo_c:hi_c], ALU.mult, [T[0][1], T[1][1]], [kmb])
                if lo_tok == 0:
                    k.dma(MTf[j * 128:(j + 1) * 128, 0:TC], mb[:, 1:1 + TC], [kmb], ["MTf"])
                k.dma(MTf[j * 128:(j + 1) * 128, TC:S], mb[:, 3 + TC:3 + S], [kmb], ["MTf"])
            P.barrier(); A.release(m)

        for l in range(n_layers):
            last = (l == DEPTH - 1) or force_last
            P.new_epoch()
            if on("mod"):
                P.label = "mod%d" % l; phase_mod(l)
            if on("norm"):
                P.label = "norm%d" % l; phase_norm(0)
            if on("mlstm"):
                P.label = "mlstm%d" % l; phase_mlstm(l)
            if on("gqa"):
                P.label = "gqa%d" % l; phase_gqa(l, last)
            if on("na"):
                P.label = "na%d" % l; phase_na(l, last)
            if on("merge"):
                P.label = "merge%d" % l; phase_merge(l)
            tiles = list(range(NCT if last else 0, NT))
            if on("res1"):
                P.label = "res1_%d" % l; phase_proj_res(MTm, "MTm", 8, w_out[l], 0, tiles)
            if on("norm2"):
                P.label = "norm2%d" % l; phase_norm(1)
            if on("ffn"):
                P.label = "ffn%d" % l; phase_ffn_up(l, TC if last else 0)
            if on("res2"):
                P.label = "res2_%d" % l; phase_proj_res(MTf, "MTf", NFT, w_down[l], 1, tiles)
        P.barrier()
        for q in range(8):
            k.dma(yout[q * 512:(q + 1) * 512, :], X[TC + q * 512:TC + (q + 1) * 512, :], ["X"], ["yout%d" % q])
        P.emit()
        build.last_prog = P
        print("ops emitted:", P.nops, {e: len(v) for e, v in P.ops.items()})
    return nc


def _consts():
    ident = np.eye(128, dtype=np.float32)
    ones = np.ones((128, 128), np.float32)
    s = np.arange(128)[:, None]; t = np.arange(128)[None, :]
    mf = (s <= t).astype(np.float32); mb = (s >= t).astype(np.float32)
    cst = np.concatenate([ident, ones, mf, mb, np.zeros((128, 128), np.float32)], axis=1)
    esel = np.zeros((4, 4, 128), np.float32)
    for h in range(4):
        esel[h, h, :] = 1.0
    half = 32
    tpos = np.arange(TL)
    inv = (10000.0 ** (-np.arange(0, half, 2, dtype=np.float32) / half)).astype(np.float32)
    ang_r = (tpos // GRID).astype(np.float32)[:, None] * inv
    ang_c = (tpos % GRID).astype(np.float32)[:, None] * inv
    ang = np.concatenate([ang_r, ang_r, ang_c, ang_c], axis=-1)
    cos = np.cos(ang).astype(np.float32); sin = np.sin(ang).astype(np.float32)
    sgn = np.concatenate([-np.ones(16), np.ones(16), -np.ones(16), np.ones(16)]).astype(np.float32)
    sins = sin * sgn[None, :]
    ropec = cos.reshape(32, 128, 64).transpose(1, 0, 2).reshape(128, 32 * 64)
    ropes = sins.reshape(32, 128, 64).transpose(1, 0, 2).reshape(128, 32 * 64)
    return cst, esel.reshape(4, 512), np.ascontiguousarray(ropec), np.ascontiguousarray(ropes)


def _rpb_tiles(na_rpb):
    L = na_rpb.shape[0]
    cols = np.arange(GRID)
    c0 = np.clip(cols - 8, 0, GRID - 16)
    kc = np.arange(GRID)[:, None]; qc = np.arange(GRID)[None, :]
    inwin = (kc >= c0[None, :]) & (kc < c0[None, :] + 16)
    dc = np.clip(kc - qc + 15, 0, 30)
    out = np.full((L, 4, 128, 2, 7, 128), NEG, np.float32)
    for di in range(7):
        delta = 2 * di - 6
        for a in range(2):
            for b in range(2):
                dr = delta + a - b + 7
                if not (0 <= dr <= 14):
                    continue
                vals = na_rpb[:, :, dr, :][:, :, dc]
                vals = np.where(inwin[None, None], vals, np.float32(NEG))
                v = vals.reshape(L, 4, 2, GRID, GRID).transpose(0, 1, 3, 2, 4)
                out[:, :, 64 * a:64 * a + 64, :, di, 64 * b:64 * b + 64] = v
    return out.reshape(L, 4, 128, 2 * 7 * 128)


_NC_CACHE = {}


def prep_inputs(inp):
    f = lambda a: np.ascontiguousarray(np.asarray(a, dtype=np.float32))
    cst, esel, ropec, ropes = _consts()
    L = DEPTH
    gfm = np.stack([f(inp["g_pre_mix"]).reshape(L, 8, 128), f(inp["g_pre_ffn"]).reshape(L, 8, 128)], axis=1)
    gfm = np.ascontiguousarray(gfm.transpose(0, 3, 1, 2))
    gpost = np.ascontiguousarray(np.stack([f(inp["g_post_mix"]), f(inp["g_post_ffn"])], axis=1))
    bgate = np.ascontiguousarray(f(inp["b_ml_gates"]).reshape(L, 4, 4).transpose(0, 2, 1))
    gq = f(inp["g_q"]); gk = f(inp["g_k"])
    gqk = np.ascontiguousarray(np.concatenate([gq, gq, gq, gq, gk, gk], axis=1))
    cw = f(inp["conv_w"])
    cwfm = np.ascontiguousarray(cw.reshape(L, 3, 2 * NFT, 128).transpose(0, 3, 2, 1).reshape(L, 128, 2 * NFT * 3))
    cbfm = np.ascontiguousarray(f(inp["conv_b"]).reshape(L, 2 * NFT, 128).transpose(0, 2, 1))
    shared = {
        "w_mod": f(inp["w_mod"]), "b_mod": f(inp["b_mod"]), "gfm": gfm, "gpost": gpost, "w_in": f(inp["w_in"]),
        "bgate": bgate, "gml": f(inp["g_ml_out"]), "gqk": gqk, "rpbT": _rpb_tiles(f(inp["na_rpb"])),
        "w_branch": f(inp["w_branch"]), "w_out": f(inp["w_out"]), "w_up": f(inp["w_up"]), "w_down": f(inp["w_down"]),
        "cwfm": cwfm, "cbfm": cbfm, "ropec": ropec, "ropes": ropes, "cst": cst, "esel": esel,
    }
    x = f(inp["x"]); c = f(inp["c"]); ctx = f(inp["ctx"]); cc = f(inp["c_ctx"])
    maps = []
    for b in range(x.shape[0]):
        cv = np.stack([c[b], cc], axis=0)
        cT = np.ascontiguousarray(cv.reshape(2, 8, 128).transpose(2, 0, 1))
        mp = dict(shared)
        mp.update({"x_in": x[b], "ctx_in": ctx[b], "cT": cT})
        maps.append(mp)
    return maps


def kernel(**inputs):
    maps = prep_inputs(inputs)
    if "nc" not in _NC_CACHE:
        _NC_CACHE["nc"] = build()
    nc = _NC_CACHE["nc"]
    res = run_bass_kernel_spmd(nc, maps, core_ids=list(range(len(maps))))
    return np.stack([np.asarray(r["y"], dtype=np.float32) for r in res.results], axis=0)
```

```python
import numpy as np
from contextlib import ExitStack
import concourse.bass as bass
import concourse.mybir as mybir
from concourse.bass_utils import run_bass_kernel_spmd

F32 = mybir.dt.float32
BF16 = mybir.dt.bfloat16
AF = mybir.ActivationFunctionType
ALU = mybir.AluOpType
AX = mybir.AxisListType

D = 1024; KC = 8; TL = 4096; TC = 256; S = TL + TC; NT = S // 128; NCT = TC // 128
DEPTH = 4; FFN = 2816; NFT = FFN // 128; D_IN = 7440
GRID = 64; EPS = 1e-6
C_MQ, C_MK, C_MV, C_MO, C_MG = 0, 512, 1024, 1536, 2048
C_AQ, C_AK, C_AV = 2064, 2576, 2704
C_NQ, C_NK, C_NV, C_ZG = 2832, 3344, 3856, 4368
ENGS = ("pe", "act", "dve", "pool", "sp")
N_DMA_SEMS = 14
NEG = -30000.0


class Prog:
    def __init__(self, nc):
        self.nc = nc
        self.ops = {e: [] for e in ENGS}
        self.cnt = {e: 0 for e in ENGS}
        self.res = {}
        self.seen = {e: {} for e in ENGS}
        self.dma_i = 0
        self.dma_hist = {}
        self.nops = 0
        self.ep = 0
        self.label = "init"
        self.name2label = {}
        self.allsems = set()
        self.final = []

    def sn(self, base):
        n = "%s_e%d" % (base, self.ep)
        self.allsems.add(n)
        return n

    def new_epoch(self):
        self.barrier()
        self.ep += 1
        self.cnt = {e: 0 for e in ENGS}
        self.dma_hist = {}
        self.seen = {e: {} for e in ENGS}

    def _deps(self, reads, writes):
        ev = set()
        for r in reads:
            st = self.res.get(r)
            if st and st[0]:
                ev.add(st[0])
        for w in writes:
            st = self.res.get(w)
            if st:
                if st[0]:
                    ev.add(st[0])
                ev.update(st[1])
        return ev

    def _commit(self, event, reads, writes):
        for r in reads:
            st = self.res.setdefault(r, [None, []])
            st[1].append(event)
        for w in writes:
            self.res[w] = [event, []]

    def _filter(self, eng, evs, own_sem=None):
        seen = self.seen[eng]
        best = {}
        for (s, v) in evs:
            if s == own_sem and eng == "pe":
                continue
            if seen.get(s, 0) >= v:
                continue
            if best.get(s, 0) < v:
                best[s] = v
        for s, v in best.items():
            seen[s] = v
        return list(best.items())

    def op(self, eng, fn, reads=(), writes=()):
        writes = tuple(writes) + tuple(r for r in reads if r.startswith("ps"))
        reads = tuple(r for r in reads if not r.startswith("ps"))
        evs = self._deps(reads, writes)
        own = self.sn("c_" + eng)
        waits = self._filter(eng, evs, own)
        self.cnt[eng] += 1
        event = (own, self.cnt[eng])
        self.ops[eng].append((waits, fn, (own, 1), self.label))
        self._commit(event, reads, writes)
        self.nops += 1

    def dma(self, eng, fn, reads=(), writes=()):
        reads = tuple(reads); writes = tuple(writes)
        evs = self._deps(reads, writes)
        j = self.dma_i % N_DMA_SEMS
        sem = self.sn("d_%d" % j)
        prev = self.dma_hist.get(j, 0)
        if prev:
            evs.add((sem, prev))
        val = prev + 16
        self.dma_hist[j] = val
        self.dma_i += 1
        waits = self._filter(eng, evs, None)
        self.ops[eng].append((waits, fn, (sem, 16), self.label))
        self._commit((sem, val), reads, writes)
        self.nops += 1

    def barrier(self):
        evs = [(self.sn("c_" + e), self.cnt[e]) for e in ENGS if self.cnt[e]]
        evs += [(self.sn("d_%d" % j), v) for j, v in self.dma_hist.items()]
        self.final = list(evs)
        for e in ENGS:
            waits = self._filter(e, evs, None)
            if waits:
                self.ops[e].append((waits, None, None, self.label))
        self.res = {}

    def emit(self):
        nc = self.nc
        with ExitStack() as st:
            sems = {}
            for n in sorted(self.allsems):
                sems[n] = st.enter_context(nc.semaphore(n))
            final = [(self.sn("c_" + e), self.cnt[e]) for e in ENGS if self.cnt[e] and e != "sp"]
            final += [(self.sn("d_%d" % j), v) for j, v in self.dma_hist.items()]
            block = st.enter_context(nc.Block())

            def run(engname):
                def body(eng):
                    for waits, fn, inc, lab in self.ops[engname]:
                        for (ws, wv) in waits:
                            eng.wait_ge(sems[ws], wv)
                        if fn is not None:
                            ins = fn(eng)
                            ins.then_inc(sems[inc[0]], inc[1])
                            try:
                                self.name2label[ins.ins.name] = lab
                            except Exception:
                                pass
                    if engname == "sp":
                        for (ws, wv) in final:
                            eng.wait_ge(sems[ws], wv)
                return body

            block.tensor(run("pe"))
            block.scalar(run("act"))
            block.vector(run("dve"))
            block.gpsimd(run("pool"))
            block.sync(run("sp"))


class Arena:
    def __init__(self, ap, nwords):
        self.ap = ap; self.n = nwords; self.off = 0; self.uid = 0

    def mark(self):
        return self.off

    def release(self, m):
        self.off = m

    def alloc(self, shape, dt, parts=128):
        n = int(np.prod(shape))
        words = n if dt == F32 else (n + 1) // 2
        words = (words + 7) // 8 * 8
        assert self.off + words <= self.n, ("SBUF arena overflow", self.off, words, self.n)
        a = self.ap[0:parts, self.off:self.off + words]
        self.off += words
        if dt != F32:
            a = a.bitcast(dt)
        a = a[:, 0:n]
        if len(shape) > 1:
            names = [chr(ord('a') + i) for i in range(len(shape))]
            kw = {names[i]: int(shape[i]) for i in range(len(shape))}
            a = a.rearrange("p (%s) -> p %s" % (" ".join(names), " ".join(names)), **kw)
        self.uid += 1
        return a, "b%d" % self.uid


class K:
    def __init__(self, P):
        self.P = P

    def mm(self, out, lhsT, rhs, start, stop, r, w):
        self.P.op("pe", lambda e: e.matmul(out, lhsT=lhsT, rhs=rhs, start=start, stop=stop), r, w)

    def tr(self, out, in_, ident, r, w):
        self.P.op("pe", lambda e: e.transpose(out, in_, ident), r, w)

    def act(self, out, in_, func, r, w, scale=None, bias=None, accum=None, eng="act"):
        kw = {}
        if scale is not None: kw["scale"] = scale
        if bias is not None: kw["bias"] = bias
        if accum is not None: kw["accum_out"] = accum
        self.P.op("act", lambda e: e.activation(out=out, in_=in_, func=func, **kw), r, w)

    def tt(self, out, in0, in1, op, r, w, eng="dve"):
        self.P.op(eng, lambda e: e.tensor_tensor(out=out, in0=in0, in1=in1, op=op), r, w)

    def ts(self, out, in0, s1, op0, r, w, s2=None, op1=None, eng="dve"):
        if op1 is None:
            self.P.op(eng, lambda e: e.tensor_scalar(out=out, in0=in0, scalar1=s1, scalar2=None, op0=op0), r, w)
        else:
            self.P.op(eng, lambda e: e.tensor_scalar(out=out, in0=in0, scalar1=s1, scalar2=s2, op0=op0, op1=op1), r, w)

    def stt(self, out, in0, scalar, in1, op0, op1, r, w):
        self.P.op("dve", lambda e: e.scalar_tensor_tensor(out=out, in0=in0, scalar=scalar, in1=in1, op0=op0, op1=op1), r, w)

    def copy(self, out, in_, r, w, eng="dve"):
        self.P.op(eng, lambda e: e.tensor_copy(out=out, in_=in_), r, w)

    def recip(self, out, in_, r, w):
        self.P.op("dve", lambda e: e.reciprocal(out=out, in_=in_), r, w)

    def reduce(self, out, in_, op, r, w):
        self.P.op("dve", lambda e: e.tensor_reduce(out=out, in_=in_, axis=AX.X, op=op), r, w)

    def scan(self, out, d0, d1, init, op0, op1, r, w):
        self.P.op("dve", lambda e: e.tensor_tensor_scan(out=out, data0=d0, data1=d1, initial=init, op0=op0, op1=op1), r, w)

    def ttr(self, out, in0, in1, accum, r, w):
        self.P.op("dve", lambda e: e.tensor_tensor_reduce(out=out, in0=in0, in1=in1, scale=1.0, scalar=0.0,
                                                          op0=ALU.mult, op1=ALU.add, accum_out=accum), r, w)

    def memset(self, ap, v, w, eng="pool"):
        self.P.op(eng, lambda e: e.memset(ap, v), (), w)

    def dma(self, out, in_, r, w, eng="sp"):
        self.P.dma(eng, lambda e: e.dma_start(out=out, in_=in_), r, w)


def r0_of(r):
    return min(max(r - 4, 0), GRID - 8)


def build(n_layers=DEPTH, debug=False, arena_words=51200, phases=None, force_last=False):
    nc = bass.Bass("TRN2", target_bir_lowering=False)

    def din(name, shape, dt=F32):
        return nc.dram_tensor(name, list(shape), dt, kind="ExternalInput").ap()

    x_in = din("x_in", [TL, D]); ctx_in = din("ctx_in", [TC, D])
    cT = din("cT", [128, 2, 8])
    w_mod = din("w_mod", [DEPTH, D, 6 * D]); b_mod = din("b_mod", [DEPTH, 6 * D])
    gfm = din("gfm", [DEPTH, 128, 2, 8])
    gpost = din("gpost", [DEPTH, 2, D])
    w_in = din("w_in", [DEPTH, D, D_IN])
    bgate = din("bgate", [DEPTH, 4, 4])
    gml = din("gml", [DEPTH, 512])
    gqk = din("gqk", [DEPTH, 384])
    rpbT = din("rpbT", [DEPTH, 4, 128, 2 * 7 * 128])
    w_branch = din("w_branch", [DEPTH, 3, 512, D]); w_out = din("w_out", [DEPTH, D, D])
    w_up = din("w_up", [DEPTH, D, 2 * FFN]); w_down = din("w_down", [DEPTH, FFN, D])
    cwfm = din("cwfm", [DEPTH, 128, 2 * NFT * 3]); cbfm = din("cbfm", [DEPTH, 128, 2 * NFT])
    ropec = din("ropec", [128, 32 * 64]); ropes = din("ropes", [128, 32 * 64])
    cst = din("cst", [128, 5 * 128])
    yout = nc.dram_tensor("y", [TL, D], F32, kind="ExternalOutput").ap()
    okind = "ExternalOutput" if debug else "Internal"
    X = nc.dram_tensor("Xres", [S, D], F32, kind=okind).ap()
    YT = [nc.dram_tensor("YT%d" % n, [512, S], BF16, kind=okind).ap() for n in range(3)]
    MTm = nc.dram_tensor("MTm", [D, S], BF16, kind="Internal").ap()
    MTf = nc.dram_tensor("MTf", [FFN, S], BF16, kind="Internal").ap()

    with ExitStack() as st:
        arena_t = st.enter_context(nc.sbuf_tensor("arena", [128, arena_words], F32))
        A = Arena(arena_t, arena_words)
        PS = [st.enter_context(nc.psum_tensor("ps%d" % i, [128, 512], F32)) for i in range(8)]
        PK = ["ps%d" % i for i in range(8)]
        P = Prog(nc)
        k = K(P)

        cstt, kc = A.alloc([5 * 128], F32)
        identF = cstt[:, 0:128]; onesF = cstt[:, 128:256]
        maskd = [cstt[:, 256:384], cstt[:, 384:512]]
        eselt, kes = A.alloc([4 * 128], F32, parts=4)
        identB, kib = A.alloc([128], BF16)
        srep, ksr = A.alloc([2, 8, 128], F32)
        hT, khT = A.alloc([KC, S], BF16)
        Gbc, kG = A.alloc([2, 2, D], F32)
        fmv, kfm = A.alloc([2, 4, 8], F32)
        k.dma(cstt, cst, (), [kc])
        k.copy(identB, identF, [kc], [kib])

        eselin = din("esel", [4, 4 * 128])
        k.dma(eselt, eselin, (), [kes])

        k.dma(X[0:TC, :], ctx_in, (), ["Xi"])
        for q in range(8):
            k.dma(X[TC + q * 512:TC + (q + 1) * 512, :], x_in[q * 512:(q + 1) * 512, :], (), ["Xi%d" % q])

        m0 = A.mark()
        ct_t, kct = A.alloc([2, 8], F32)
        k.dma(ct_t, cT, (), [kct])
        k.act(ct_t, ct_t, AF.Silu, [kct], [kct])
        for r in range(2):
            for kk in range(8):
                k.ts(srep[:, r, kk, :], onesF, ct_t[:, r, kk:kk + 1], ALU.mult, [kct, kc], [ksr])
        P.barrier(); A.release(m0)

        on = lambda nm: phases is None or nm in phases

        def phase_mod(l):
            m = A.mark()
            wm = [A.alloc([8, 512], F32) for _ in range(2)]
            brow = [A.alloc([512], F32, parts=1) for _ in range(2)]
            gp = [A.alloc([512], F32) for _ in range(2)]
            tmp = [A.alloc([512], F32) for _ in range(2)]
            junk, kj = A.alloc([4, 128], F32)
            gf, kgf = A.alloc([2, 8], F32)
            k.dma(gf, gfm[l], (), [kgf])
            for cb in range(12):
                b = cb % 2
                seg = cb // 2; half = cb % 2
                k.dma(wm[b][0], w_mod[l][:, cb * 512:(cb + 1) * 512].rearrange("(k p) c -> p k c", p=128), (), [wm[b][1]])
                k.dma(brow[b][0], b_mod[l:l + 1, cb * 512:(cb + 1) * 512], (), [brow[b][1]])
                if seg in (2, 5):
                    which = 0 if seg == 2 else 1
                    k.dma(gp[b][0], gpost[l, which:which + 1, half * 512:(half + 1) * 512].to_broadcast([128, 512]), (), [gp[b][1]])
                for r in range(2):
                    ps = PS[r + 2 * b]; pk = PK[r + 2 * b]
                    for kk in range(8):
                        k.mm(ps[:], srep[:, r, kk, :], wm[b][0][:, kk, :], kk == 0, False, [ksr, wm[b][1]], [pk])
                    k.mm(ps[:], onesF[0:1, :], brow[b][0], False, True, [kc, brow[b][1]], [pk])
                    if seg in (2, 5):
                        which = 0 if seg == 2 else 1
                        k.tt(Gbc[:, r, which, half * 512:(half + 1) * 512], ps[:], gp[b][0], ALU.mult, [pk, gp[b][1]], [kG])
                    else:
                        slot = {0: 1, 1: 0, 3: 3, 4: 2}[seg]
                        tb = tmp[r]
                        k.copy(tb[0], ps[:], [pk], [tb[1]])
                        k.tt(junk, tb[0].rearrange("p (a b) -> p a b", b=128), identF.unsqueeze(1).to_broadcast([128, 4, 128]),
                             ALU.mult, [tb[1], kc], [kj])
                        k.reduce(fmv[:, r, slot, half * 4:half * 4 + 4], junk, ALU.add, [kj], [kfm])
            for r in range(2):
                for (slot, gi) in ((0, 0), (2, 1)):
                    k.stt(fmv[:, r, slot, :], fmv[:, r, slot, :], 1.0, gf[:, gi, :], ALU.add, ALU.mult, [kfm, kgf], [kfm])
            return m

        def phase_norm(which, mod_ps=False):
            pso = 4 if mod_ps else 0
            m = A.mark()
            xt = [A.alloc([D], F32) for _ in range(2)]
            xn = [A.alloc([D], BF16) for _ in range(2)]
            junk, kj = A.alloc([D], F32)
            ss, kss = A.alloc([NT], F32)
            for i in range(NT):
                b = i % 2
                k.dma(xt[b][0], X[i * 128:(i + 1) * 128, :], ["X"], [xt[b][1]])
                k.act(junk, xt[b][0], AF.Square, [xt[b][1]], [kj, kss], accum=ss[:, i:i + 1])
            k.ts(ss, ss, 1.0 / D, ALU.mult, [kss], [kss], s2=EPS, op1=ALU.add)
            k.act(ss, ss, AF.Sqrt, [kss], [kss])
            k.recip(ss, ss, [kss], [kss])
            for i in range(NT):
                b = i % 2
                r = 1 if i < NCT else 0
                k.dma(xt[b][0], X[i * 128:(i + 1) * 128, :], ["X"], [xt[b][1]])
                k.ts(xn[b][0], xt[b][0], ss[:, i:i + 1], ALU.mult, [xt[b][1], kss], [xn[b][1]])
                ps = PS[pso + b]; pk = PK[pso + b]
                psb = ps[:].bitcast(BF16).rearrange("p (a b) -> p a b", a=8)
                for kk in range(8):
                    k.tr(psb[:, kk, :], xn[b][0][:, kk * 128:(kk + 1) * 128], identB, [xn[b][1], kib], [pk])
                for kk in range(8):
                    k.act(hT[:, kk, i * 128:(i + 1) * 128], psb[:, kk, :], AF.Identity, [pk, kfm], [khT],
                          scale=fmv[:, r, 2 * which, kk:kk + 1], bias=fmv[:, r, 2 * which + 1, kk:kk + 1])
            P.barrier(); A.release(m)

        def load_w(dst, key, src_cols_ap):
            k.dma(dst, src_cols_ap.rearrange("(k p) c -> p k c", p=128), (), [key], eng="pool")

        TOKCH = [(c * 512, min(512, S - c * 512)) for c in range((S + 511) // 512)]

        def phase_mlstm(l):
            m = A.mark()
            kwT = [A.alloc([NT, 4], F32) for _ in range(2)]
            thT = [A.alloc([NT, 4], F32) for _ in range(2)]
            gbc = [A.alloc([4, NT], F32) for _ in range(2)]
            m1 = A.mark()
            wg, kwg = A.alloc([8, 16], BF16)
            load_w(wg, kwg, w_in[l][:, C_MG:C_MG + 16])
            bg, kbg = A.alloc([4], F32, parts=4)
            k.dma(bg, bgate[l], (), [kbg])
            X0, k0 = A.alloc([S], F32, parts=4); X1, k1 = A.alloc([S], F32, parts=4)
            X2, k2 = A.alloc([S], F32, parts=4); X3, k3 = A.alloc([S], F32, parts=4)
            X4, k4 = A.alloc([S], F32, parts=4)
            cm, kcm = A.alloc([NT], F32, parts=4); ri, kri = A.alloc([NT], F32, parts=4)
            rr, krr = A.alloc([NT], F32, parts=4); gd, kgd = A.alloc([NT], F32, parts=4)
            for d in range(2):
                for (j, dst, kd) in ((2 * d, X0, k0), (2 * d + 1, X1, k1)):
                    for ci, (t0, tn) in enumerate(TOKCH):
                        ps = PS[ci % 2]; pk = PK[ci % 2]
                        for kk in range(8):
                            k.mm(ps[0:4, 0:tn], wg[:, kk, 4 * j:4 * j + 4], hT[:, kk, t0:t0 + tn], kk == 0, kk == 7, [kwg, khT], [pk])
                        k.act(dst[:, t0:t0 + tn], ps[0:4, 0:tn], AF.Identity, [pk, kbg], [kd], bias=bg[:, j:j + 1], scale=1.0)
                k.act(X1, X1, AF.Exp, [k1], [k1], scale=-1.0)
                k.ts(X2, X1, 2.0, ALU.add, [k1], [k2])
                k.recip(X2, X2, [k2], [k2])
                k.tt(X2, X2, X1, ALU.mult, [k2, k1], [k2])
                k.tt(X3, X2, X2, ALU.mult, [k2], [k3])
                k.ts(X4, X3, 0.2, ALU.mult, [k3], [k4], s2=1.0 / 3.0, op1=ALU.add)
                k.tt(X4, X4, X3, ALU.mult, [k4, k3], [k4])
                k.ts(X4, X4, 1.0, ALU.add, [k4], [k4], s2=-2.0, op1=ALU.mult)
                k.tt(X4, X4, X2, ALU.mult, [k4, k2], [k4])
                if d == 0:
                    k.scan(X1, X4, X4, 0.0, ALU.add, ALU.min, [k4], [k1])
                else:
                    k.scan(X1[:, 0:TC][:, ::-1], X4[:, 0:TC][:, ::-1], X4[:, 0:TC][:, ::-1], 0.0,
                           ALU.add, ALU.min, [k4], [k1])
                    k.scan(X1[:, TC:S][:, ::-1], X4[:, TC:S][:, ::-1], X4[:, TC:S][:, ::-1], X1[:, 0:1],
                           ALU.add, ALU.min, [k4, k1], [k1])
                k.tt(X2, X0, X1, ALU.subtract, [k0, k1], [k2])
                k.reduce(cm, X2.rearrange("p (c t) -> p c t", t=128), ALU.max, [k2], [kcm])
                if d == 0:
                    k.scan(ri, cm, cm, 0.0, ALU.max, ALU.max, [kcm], [kri])
                    k.memset(rr[:, 0:1], 0.0, [krr], eng="dve")
                    k.copy(rr[:, 1:NT], ri[:, 0:NT - 1], [kri], [krr])
                else:
                    k.scan(ri[:, 0:NCT][:, ::-1], cm[:, 0:NCT][:, ::-1], cm[:, 0:NCT][:, ::-1], 0.0, ALU.max, ALU.max, [kcm], [kri])
                    k.scan(ri[:, NCT:NT][:, ::-1], cm[:, NCT:NT][:, ::-1], cm[:, NCT:NT][:, ::-1], ri[:, 0:1], ALU.max, ALU.max,
                           [kcm, kri], [kri])
                    k.memset(rr[:, NCT - 1:NCT], 0.0, [krr], eng="dve")
                    k.copy(rr[:, 0:NCT - 1], ri[:, 1:NCT], [kri], [krr])
                    k.copy(rr[:, NT - 1:NT], ri[:, 0:1], [kri], [krr])
                    k.copy(rr[:, NCT:NT - 1], ri[:, NCT + 1:NT], [kri], [krr])
                rrb = rr.unsqueeze(2).to_broadcast([4, NT, 128])
                k.tt(X3.rearrange("p (c t) -> p c t", t=128), X2.rearrange("p (c t) -> p c t", t=128), rrb, ALU.subtract, [k2, krr], [k3])
                k.act(X3, X3, AF.Exp, [k3], [k3])
                k.tt(X0.rearrange("p (c t) -> p c t", t=128), X1.rearrange("p (c t) -> p c t", t=128), rrb, ALU.add, [k1, krr], [k0])
                k.act(X0, X0, AF.Exp, [k0], [k0], scale=-1.0)
                k.tt(gd, rr, ri, ALU.subtract, [krr, kri], [kgd])
                k.act(gd, gd, AF.Exp, [kgd], [kgd])
                for (src, ks, dstp) in ((X3, k3, kwT[d]), (X0, k0, thT[d])):
                    ps = PS[2]; pk = PK[2]
                    for c in range(NT):
                        k.tr(ps[:, c * 4:(c + 1) * 4], src[:, c * 128:(c + 1) * 128], identF[0:4, 0:4], [ks, kc], [pk])
                    k.copy(dstp[0], ps[:, 0:NT * 4].rearrange("p (c h) -> p c h", h=4), [pk], [dstp[1]])
                ps = PS[3]; pk = PK[3]
                for h in range(4):
                    k.mm(ps[:, h * NT:(h + 1) * NT], eselt[:, h * 128:(h + 1) * 128], gd, True, True, [kes, kgd], [pk])
                k.copy(gbc[d][0], ps[:, 0:4 * NT].rearrange("p (h c) -> p h c", h=4), [pk], [gbc[d][1]])
            P.barrier(); A.release(m1)

            gmlb, kgm = A.alloc([512], F32)
            k.dma(gmlb, gml[l:l + 1, :].to_broadcast([128, 512]), (), [kgm])
            for h in range(4 if on("ml_heads") else 0):
                m2 = A.mark()
                wq, kwq = A.alloc([8, 128], BF16); wk_, kwk = A.alloc([8, 128], BF16)
                wkvo, kwkvo = A.alloc([8, 384], BF16)
                load_w(wq, kwq, w_in[l][:, C_MQ + h * 128:C_MQ + (h + 1) * 128])
                load_w(wk_, kwk, w_in[l][:, C_MK + h * 128:C_MK + (h + 1) * 128])
                load_w(wkvo[:, :, 0:128], kwkvo, w_in[l][:, C_MK + h * 128:C_MK + (h + 1) * 128])
                load_w(wkvo[:, :, 128:256], kwkvo, w_in[l][:, C_MV + h * 128:C_MV + (h + 1) * 128])
                load_w(wkvo[:, :, 256:384], kwkvo, w_in[l][:, C_MO + h * 128:C_MO + (h + 1) * 128])
                QT, kQT = A.alloc([S], BF16); KT, kKT = A.alloc([S], BF16)
                Ktm, kKtm = A.alloc([NT, 128], BF16); Va, kVa = A.alloc([NT, 130], BF16)
                Osg, kOs = A.alloc([NT, 128], BF16); Hacc, kH = A.alloc([NT, 128], F32)
                k.memset(Va[:, :, 128:129], 1.0, [kVa])
                k.memset(Hacc, 0.0, [kH + ".%d" % c for c in range(NT)])
                for ci, (t0, tn) in enumerate(TOKCH if on("ml_fm") else []):
                    for (wt, kwt, dst, kd, sc, pi) in ((wq, kwq, QT, kQT, 1.0, 0), (wk_, kwk, KT, kKT, 128.0 ** -0.5, 1)):
                        ps = PS[pi + 2 * (ci % 2)]; pk = PK[pi + 2 * (ci % 2)]
                        for kk in range(8):
                            k.mm(ps[:, 0:tn], wt[:, kk, :], hT[:, kk, t0:t0 + tn], kk == 0, kk == 7, [kwt, khT], [pk])
                        k.act(dst[:, t0:t0 + tn], ps[:, 0:tn], AF.Identity, [pk], [kd], scale=sc)
                for i in range(NT if on("ml_tm") else 0):
                    ps = PS[4 + i % 2]; pk = PK[4 + i % 2]
                    for kk in range(8):
                        k.mm(ps[:, 0:384], hT[:, kk, i * 128:(i + 1) * 128], wkvo[:, kk, :], kk == 0, kk == 7, [khT, kwkvo], [pk])
                    k.act(Ktm[:, i, :], ps[:, 0:128], AF.Identity, [pk], [kKtm], scale=128.0 ** -0.5)
                    k.copy(Va[:, i, 0:128], ps[:, 128:256], [pk], [kVa])
                    k.act(Osg[:, i, :], ps[:, 256:384], AF.Sigmoid, [pk], [kOs])
                Cf = [A.alloc([129], F32) for _ in range(2)]; Cb = [A.alloc([130], BF16) for _ in range(2)]
                PmT = [A.alloc([128], BF16) for _ in range(2)]; Kp = [A.alloc([128], BF16) for _ in range(2)]
                dd = [A.alloc([2], F32) for _ in range(2)]
                for d in range(2):
                    k.memset(Cf[d][0], 0.0, [Cf[d][1]]); k.memset(Cb[d][0], 0.0, [Cb[d][1]])
                order = [list(range(NT)), [1, 0] + list(range(NT - 1, NCT - 1, -1))]
                for step in range(NT if on("ml_rec") else 0):
                    cc = [order[d][step] for d in range(2)]
                    notlast = step < NT - 1
                    for d in range(2):
                        c = cc[d]
                        if notlast:
                            k.act(Kp[d][0], Ktm[:, c, :], AF.Identity, [kKtm, kwT[d][1]], [Kp[d][1]], scale=kwT[d][0][:, c, h:h + 1])
                    for d in range(2):
                        c = cc[d]; cs = slice(c * 128, (c + 1) * 128)
                        k.mm(PS[0 + d][:, 0:128], KT[:, cs], QT[:, cs], True, True, [kKT, kQT], [PK[0 + d]])
                    for d in range(2):
                        c = cc[d]
                        if notlast:
                            k.mm(PS[6 + d][:, 0:129], Kp[d][0], Va[:, c, 0:129], True, True, [Kp[d][1], kVa], [PK[6 + d]])
                    for d in range(2):
                        c = cc[d]
                        k.stt(PmT[d][0], PS[0 + d][:, 0:128], kwT[d][0][:, c, h:h + 1], maskd[d], ALU.mult, ALU.mult,
                              [PK[0 + d], kwT[d][1], kc], [PmT[d][1]])
                    for d in range(2):
                        c = cc[d]; cs = slice(c * 128, (c + 1) * 128)
                        pO, kO = PS[2 + d], PK[2 + d]
                        k.mm(pO[:, 0:129], PmT[d][0], Va[:, c, 0:129], True, False, [PmT[d][1], kVa], [kO])
                        k.mm(pO[:, 0:129], QT[:, cs], Cb[d][0][:, 0:129], False, True, [kQT, Cb[d][1]], [kO])
                    for d in range(2):
                        c = cc[d]
                        pO, kO = PS[2 + d], PK[2 + d]
                        k.act(dd[d][0][:, 0:1], pO[:, 128:129], AF.Abs, [kO], [dd[d][1]])
                        k.tt(dd[d][0][:, 0:1], dd[d][0][:, 0:1], thT[d][0][:, c, h:h + 1], ALU.max, [dd[d][1], thT[d][1]], [dd[d][1]])
                        k.recip(dd[d][0][:, 1:2], dd[d][0][:, 0:1], [dd[d][1]], [dd[d][1]])
                        k.stt(Hacc[:, c, :], pO[:, 0:128], dd[d][0][:, 1:2], Hacc[:, c, :], ALU.mult, ALU.add,
                              [kO, dd[d][1], kH + ".%d" % c], [kH + ".%d" % c])
                    for d in range(2):
                        c = cc[d]
                        if notlast:
                            pU, kU = PS[6 + d], PK[6 + d]
                            k.tt(Cf[d][0], pU[:, 0:129], Cf[d][0], ALU.add, [kU, Cf[d][1]], [Cf[d][1]])
                            k.ts(Cf[d][0], Cf[d][0], gbc[d][0][:, h, c:c + 1], ALU.mult, [Cf[d][1], gbc[d][1]], [Cf[d][1]])
                            k.act(Cb[d][0][:, 0:129], Cf[d][0], AF.Identity, [Cf[d][1]], [Cb[d][1]])
                ssq, kssq = A.alloc([NT], F32)
                sq, ksq = A.alloc([8, 128], F32); yb, kyb = A.alloc([8, 128], BF16); ys, kys = A.alloc([8 * 128], BF16)
                hk_all = [kH + ".%d" % c for c in range(NT)]
                for g0 in range(0, NT if on("ml_fin") else 0, 8):
                    gn = min(8, NT - g0)
                    hv = Hacc[:, g0:g0 + gn, :]
                    k.tt(sq[:, 0:gn, :], hv, hv, ALU.mult, hk_all, [ksq])
                    k.reduce(ssq[:, g0:g0 + gn], sq[:, 0:gn, :], ALU.add, [ksq], [kssq])
                if on("ml_fin"):
                    k.ts(ssq, ssq, 1.0 / 128, ALU.mult, [kssq], [kssq], s2=EPS, op1=ALU.add)
                    k.act(ssq, ssq, AF.Sqrt, [kssq], [kssq])
                    k.recip(ssq, ssq, [kssq], [kssq])
                for g0 in range(0, NT if on("ml_fin") else 0, 8):
                    gn = min(8, NT - g0)
                    hv = Hacc[:, g0:g0 + gn, :]
                    k.tt(sq[:, 0:gn, :], hv, ssq[:, g0:g0 + gn].unsqueeze(2).to_broadcast([128, gn, 128]), ALU.mult, hk_all + [kssq], [ksq])
                    k.tt(sq[:, 0:gn, :], sq[:, 0:gn, :], gmlb[:, h * 128:(h + 1) * 128].unsqueeze(1).to_broadcast([128, gn, 128]),
                         ALU.mult, [ksq, kgm], [ksq])
                    k.tt(yb[:, 0:gn, :], sq[:, 0:gn, :], Osg[:, g0:g0 + gn, :], ALU.mult, [ksq, kOs], [kyb])
                    ps = PS[4 + (g0 // 8) % 2]; pk = PK[4 + (g0 // 8) % 2]
                    psb = ps[:].bitcast(BF16)
                    for q in range(gn):
                        k.tr(psb[:, q * 128:(q + 1) * 128], yb[:, q, :], identB, [kyb, kib], [pk])
                    k.copy(ys[:, 0:gn * 128], psb[:, 0:gn * 128], [pk], [kys])
                    k.dma(YT[0][h * 128:(h + 1) * 128, g0 * 128:(g0 + gn) * 128], ys[:, 0:gn * 128], [kys], ["YT0"])
                P.barrier(); A.release(m2)
            P.barrier(); A.release(m)

        def attn_finalize(pO, kO, n, yb_ap, kyb, tmpf, pB, kB):
            rden, krd, osb, kosb = tmpf
            k.act(rden[64:65, 0:n], pO[64:65, 0:n], AF.Ln, [kO], [krd])
            k.act(rden[64:65, 0:n], rden[64:65, 0:n], AF.Exp, [krd], [krd], scale=-1.0)
            k.mm(pB[0:64, 0:n], onesF[64:65, 0:64], rden[64:65, 0:n], True, True, [kc, krd], [kB])
            k.act(osb[0:64, 0:n], pO[0:64, 0:n], AF.Identity, [kO], [kosb])
            k.tt(yb_ap, osb[0:64, 0:n], pB[0:64, 0:n], ALU.mult, [kosb, kB], [kyb])

        def phase_gqa(l, last):
            m = A.mark()
            rc, krc = A.alloc([32, 64], F32); rs, krs = A.alloc([32, 64], F32)
            k.dma(rc, ropec.rearrange("p (a b) -> p a b", b=64), (), [krc])
            k.dma(rs, ropes.rearrange("p (a b) -> p a b", b=64), (), [krs])
            gqf, kgq = A.alloc([384], F32)
            k.dma(gqf, gqk[l:l + 1, :].to_broadcast([128, 384]), (), [kgq])
            gq = gqf.rearrange("p (a b) -> p a b", b=64)
            rden, krd = A.alloc([512], F32); osb, kosb = A.alloc([512], F32)
            for g in range(2):
                m2 = A.mark()
                w, kw_ = A.alloc([8, 384], BF16)
                load_w(w[:, :, 0:256], kw_, w_in[l][:, C_AQ + g * 256:C_AQ + (g + 1) * 256])
                load_w(w[:, :, 256:320], kw_, w_in[l][:, C_AK + g * 64:C_AK + (g + 1) * 64])
                load_w(w[:, :, 320:384], kw_, w_in[l][:, C_AV + g * 64:C_AV + (g + 1) * 64])
                QTK, kQ = A.alloc([4, S], BF16); Vg, kV = A.alloc([NT, 128], BF16)
                k.memset(Vg, 0.0, [kV])
                k.memset(Vg[:, :, 64:65], 1.0, [kV])
                sq, ksq = A.alloc([5, 64], F32); t1, kt1 = A.alloc([5, 64], F32); xr, kxr = A.alloc([5, 64], F32)
                sw, ksw = A.alloc([5, 64], F32); qr, kqr = A.alloc([320], BF16); st5, kst = A.alloc([5], F32)
                kz = [A.alloc([256], BF16) for _ in range(2)]
                for z_ in range(2):
                    k.memset(kz[z_][0], 0.0, [kz[z_][1]])
                gq5 = gq[:, 0:5, :]

                def g_proj(i):
                    ps = PS[i % 2]; pk = PK[i % 2]
                    for kk in range(8):
                        k.mm(ps[:, 0:384], hT[:, kk, i * 128:(i + 1) * 128], w[:, kk, :], kk == 0, kk == 7, [khT, kw_], [pk])
                g_proj(0)
                for i in range(NT):
                    ps = PS[i % 2]; pk = PK[i % 2]
                    if i + 1 < NT:
                        g_proj(i + 1)
                    psv = ps[:, 0:320].rearrange("p (a b) -> p a b", b=64)
                    k.act(sq, psv, AF.Square, [pk], [ksq])
                    k.reduce(st5, sq, ALU.add, [ksq], [kst])
                    k.ts(st5, st5, 1.0 / 64, ALU.mult, [kst], [kst], s2=EPS, op1=ALU.add)
                    k.act(st5, st5, AF.Ln, [kst], [kst])
                    k.act(st5, st5, AF.Exp, [kst], [kst], scale=-0.5)
                    k.tt(t1, psv, st5.unsqueeze(2).to_broadcast([128, 5, 64]), ALU.mult, [pk, kst], [kt1])
                    k.tt(t1, t1, gq5, ALU.mult, [kt1, kgq], [kt1])
                    k.copy(Vg[:, i, 0:64], ps[:, 320:384], [pk], [kV], eng="dve")
                    qrv = qr.rearrange("p (a b) -> p a b", b=64)
                    if i >= NCT:
                        lt = i - NCT
                        cb_ = rc[:, lt, :].unsqueeze(1).to_broadcast([128, 5, 64])
                        k.tt(xr, t1, cb_, ALU.mult, [kt1, krc], [kxr])
                        t1v = t1.rearrange("p h (a q e) -> p h a q e", a=2, q=2)
                        swv = sw.rearrange("p h (a q e) -> p h a q e", a=2, q=2)
                        rsv = rs[:, lt, :].rearrange("p (a q e) -> p a q e", a=2, q=2)
                        for qd in range(2):
                            k.tt(swv[:, :, :, qd, :], t1v[:, :, :, 1 - qd, :],
                                 rsv[:, :, qd, :].unsqueeze(1).to_broadcast([128, 5, 2, 16]), ALU.mult, [kt1, krs], [ksw])
                        k.tt(qrv, xr, sw, ALU.add, [kxr, ksw], [kqr])
                    else:
                        k.copy(qrv, t1, [kt1], [kqr])
                    kzz = kz[i % 2]
                    k.copy(kzz[0][:, 0:64], qr[:, 256:320], [kqr], [kzz[1]])
                    k.copy(kzz[0][:, 192:256], qr[:, 256:320], [kqr], [kzz[1]])
                    pt = PS[2 + i % 2]; pkt = PK[2 + i % 2]
                    ptb = pt[:].bitcast(BF16)
                    for q in range(2):
                        k.tr(ptb[:, q * 128:(q + 1) * 128], qr[:, q * 128:(q + 1) * 128], identB, [kqr, kib], [pkt])
                    for q in range(2):
                        k.tr(ptb[:, (2 + q) * 128:(3 + q) * 128], kzz[0][:, q * 128:(q + 1) * 128], identB, [kzz[1], kib], [pkt])
                    k.act(QTK[:, :, i * 128:(i + 1) * 128], ptb[:, 0:512].rearrange("p (a b) -> p a b", b=128), AF.Identity, [pkt], [kQ])
                PT = [A.alloc([512], BF16) for _ in range(4)]
                yb, kyb = A.alloc([512], BF16)
                jobs = []
                if not last:
                    for qh in range(4):
                        jobs.append((qh, 0, TC, list(range(NCT))))
                for qh in range(4):
                    for qc in range(TL // 512):
                        jobs.append((qh, TC + qc * 512, 512, list(range(NT))))
                items = []
                for ji, (qh, q0, qn, kts) in enumerate(jobs):
                    for ki, kt in enumerate(kts):
                        items.append((ji, ki, kt))

                def g_qk(idx):
                    ji, ki, kt = items[idx]
                    qh, q0, qn, kts = jobs[ji]
                    j = qh // 2; hb = 64 * (qh % 2)
                    pS, kS = PS[idx % 4], PK[idx % 4]
                    k.mm(pS[:, 0:qn], QTK[:, 2 + qh % 2, kt * 128:(kt + 1) * 128], QTK[:, j, q0:q0 + qn], True, True, [kQ], [kS])

                def g_rest(idx):
                    ji, ki, kt = items[idx]
                    qh, q0, qn, kts = jobs[ji]
                    pS, kS = PS[idx % 4], PK[idx % 4]
                    ptile = PT[idx % 4]
                    pO, kO = PS[4 + ji % 2], PK[4 + ji % 2]
                    k.act(ptile[0][:, 0:qn], pS[:, 0:qn], AF.Exp, [kS], [ptile[1]], scale=0.125)
                    k.mm(pO[0:65, 0:qn], Vg[:, kt, 0:65], ptile[0][:, 0:qn], ki == 0, ki == len(kts) - 1, [kV, ptile[1]], [kO])

                def g_fin(ji):
                    qh, q0, qn, kts = jobs[ji]
                    pO, kO = PS[4 + ji % 2], PK[4 + ji % 2]
                    attn_finalize(pO, kO, qn, yb[0:64, 0:qn], kyb, (rden, krd, osb, kosb), PS[6 + ji % 2], PK[6 + ji % 2])
                    hd = 4 * g + qh
                    k.dma(YT[1][hd * 64:(hd + 1) * 64, q0:q0 + qn], yb[0:64, 0:qn], [kyb], ["YT1"])

                def g_exp(idx):
                    ji, ki, kt = items[idx]
                    qh, q0, qn, kts = jobs[ji]
                    k.act(PT[idx % 4][0][:, 0:qn], PS[idx % 4][:, 0:qn], AF.Exp, [PK[idx % 4]], [PT[idx % 4][1]], scale=0.125)

                def g_pv(idx, extra):
                    ji, ki, kt = items[idx]
                    qh, q0, qn, kts = jobs[ji]
                    pO, kO = PS[4 + ji % 2], PK[4 + ji % 2]
                    k.mm(pO[:, 0:qn], Vg[:, kt, :], PT[idx % 4][0][:, 0:qn], ki == 0, ki == len(kts) - 1,
                         [kV, PT[idx % 4][1]] + extra, [kO])

                pending = None
                g_qk(0)
                if len(items) > 1:
                    g_qk(1)
                for idx in range(len(items)):
                    if idx % 2 == 0:
                        for q in (idx + 2, idx + 3):
                            if q < len(items):
                                g_qk(q)
                        g_exp(idx)
                        if idx + 1 < len(items):
                            g_exp(idx + 1)
                            g_pv(idx, [PT[(idx + 1) % 4][1]])
                        else:
                            g_pv(idx, [])
                    else:
                        g_pv(idx, [])
                    ji, ki, kt = items[idx]
                    if pending is not None and pending[1] == idx:
                        g_fin(pending[0]); pending = None
                    if ki == len(jobs[ji][3]) - 1:
                        if idx + 2 < len(items):
                            pending = (ji, idx + 2)
                        else:
                            if pending is not None:
                                g_fin(pending[0]); pending = None
                            g_fin(ji)
                if pending is not None:
                    g_fin(pending[0]); pending = None
                P.barrier(); A.release(m2)
            P.barrier(); A.release(m)

        def phase_na(l, last):
            m = A.mark()
            rden, krd = A.alloc([512], F32); osb, kosb = A.alloc([512], F32)
            VnA, kVn = A.alloc([NT, 8, 66], BF16)
            k.memset(VnA[:, :, :, 64:65], 1.0, [kVn])
            mv_ = A.mark()
            wv, kwv = A.alloc([8, 512], BF16)
            load_w(wv, kwv, w_in[l][:, C_NV:C_NV + 512])
            for i in range(NT):
                ps = PS[4 + i % 2]; pk = PK[4 + i % 2]
                for kk in range(8):
                    k.mm(ps[:, 0:512], hT[:, kk, i * 128:(i + 1) * 128], wv[:, kk, :], kk == 0, kk == 7, [khT, kwv], [pk])
                k.copy(VnA[:, i, :, 0:64], ps[:, 0:512].rearrange("p (a b) -> p a b", b=64), [pk], [kVn])
            P.barrier(); A.release(mv_)
            for hp in range(4):
                m2 = A.mark()
                wq, kwq = A.alloc([8, 128], BF16); wk_, kwk = A.alloc([8, 128], BF16)
                load_w(wq, kwq, w_in[l][:, C_NQ + hp * 128:C_NQ + (hp + 1) * 128])
                load_w(wk_, kwk, w_in[l][:, C_NK + hp * 128:C_NK + (hp + 1) * 128])
                Vn = VnA[:, :, 2 * hp:2 * hp + 2, :]
                Tb, kTb = A.alloc([2, 7, 128], F32)
                k.dma(Tb, rpbT[l, hp].rearrange("p (a b c) -> p a b c", a=2, b=7), (), [kTb])
                QT, kQT = A.alloc([S], BF16); KT, kKT = A.alloc([S], BF16)
                for ci, (t0, tn) in enumerate(TOKCH):
                    for (wt, kwt, dst, kd, sc, pi) in ((wq, kwq, QT, kQT, 0.125, 0), (wk_, kwk, KT, kKT, 1.0, 1)):
                        ps = PS[pi + 2 * (ci % 2)]; pk = PK[pi + 2 * (ci % 2)]
                        for kk in range(8):
                            k.mm(ps[:, 0:tn], wt[:, kk, :], hT[:, kk, t0:t0 + tn], kk == 0, kk == 7, [kwt, khT], [pk])
                        k.act(dst[:, t0:t0 + tn], ps[:, 0:tn], AF.Identity, [pk], [kd], scale=sc)
                sbs = [A.alloc([640], F32) for _ in range(2)]
                PT = [A.alloc([896], BF16) for _ in range(2)]
                ynb, kyn = A.alloc([2, TL], BF16, parts=64)
                ycb, kyc = A.alloc([2, TC], BF16, parts=64)
                it = 0
                if not last:
                    for hh in range(2):
                        hb = 64 * hh
                        pS, kS = PS[0], PK[0]; pO, kO = PS[4 + hh], PK[4 + hh]
                        for kt in range(NCT):
                            k.mm(pS[:, kt * 256:(kt + 1) * 256], KT[hb:hb + 64, kt * 128:(kt + 1) * 128], QT[hb:hb + 64, 0:TC], True, True, [kKT, kQT], [kS])
                        ptile = PT[it % 2]; it += 1
                        k.act(ptile[0][:, 0:512], pS[:, 0:512], AF.Exp, [kS], [ptile[1]])
                        for kt in range(NCT):
                            k.mm(pO[0:65, 0:TC], Vn[:, kt, hh, 0:65], ptile[0][:, kt * 256:(kt + 1) * 256], kt == 0, kt == NCT - 1, [kVn, ptile[1]], [kO])
                        attn_finalize(pO, kO, TC, ycb[:, hh, :], kyc, (rden, krd, osb, kosb), PS[6 + hh], PK[6 + hh])
                    for hh in range(2):
                        hd = 2 * hp + hh
                        k.dma(YT[2][hd * 64:(hd + 1) * 64, 0:TC], ycb[:, hh, :], [kyc], ["YT2"])
                def blk(ji):
                    rb, hh = ji // 2, ji % 2
                    q0 = TC + rb * 128
                    r0a, r0b = r0_of(2 * rb), r0_of(2 * rb + 1)
                    tiles = list(range(r0a // 2, (r0b + 7) // 2 + 1))
                    nw = len(tiles)
                    di0 = (2 * tiles[0] - 2 * rb + 6) // 2
                    assert 0 <= di0 and di0 + nw <= 7 and nw <= 5
                    return rb, hh, q0, tiles, nw, di0

                def bufs(ji):
                    return (PS[2 * (ji % 2)], PK[2 * (ji % 2)], PS[2 * (ji % 2) + 1], PK[2 * (ji % 2) + 1],
                            PS[4 + ji % 2], PK[4 + ji % 2], sbs[ji % 2], PT[ji % 2])

                def n_qk(ji):
                    rb, hh, q0, tiles, nw, di0 = blk(ji)
                    hb = 64 * hh
                    pSa, kSa, pSb, kSb, pO, kO, sb_, ptile = bufs(ji)
                    for jw, t in enumerate(tiles):
                        pp, kp, co = (pSa, kSa, jw * 128) if jw < 4 else (pSb, kSb, (jw - 4) * 128)
                        kt = NCT + t
                        k.mm(pp[:, co:co + 128], KT[hb:hb + 64, kt * 128:(kt + 1) * 128], QT[hb:hb + 64, q0:q0 + 128], True, True, [kKT, kQT], [kp])
                    for kt in range(NCT):
                        k.mm(pSb[:, 128 + kt * 128:256 + kt * 128], KT[hb:hb + 64, kt * 128:(kt + 1) * 128], QT[hb:hb + 64, q0:q0 + 128], True, True, [kKT, kQT], [kSb])

                def n_soft(ji):
                    rb, hh, q0, tiles, nw, di0 = blk(ji)
                    pSa, kSa, pSb, kSb, pO, kO, sb_, ptile = bufs(ji)
                    n4 = min(nw, 4)
                    k.tt(sb_[0][:, 0:n4 * 128], pSa[:, 0:n4 * 128], Tb[:, hh, di0:di0 + n4, :].rearrange("p a b -> p (a b)"), ALU.add, [kSa, kTb], [sb_[1]])
                    if nw > 4:
                        k.tt(sb_[0][:, 512:640], pSb[:, 0:128], Tb[:, hh, di0 + 4, :], ALU.add, [kSb, kTb], [sb_[1]])
                    k.act(ptile[0][:, 0:nw * 128], sb_[0][:, 0:nw * 128], AF.Exp, [sb_[1]], [ptile[1]])
                    k.act(ptile[0][:, 640:896], pSb[:, 128:384], AF.Exp, [kSb], [ptile[1]])
                    for jw, t in enumerate(tiles):
                        for a in range(2):
                            for b in range(2):
                                kr = 2 * t + a; qrow = 2 * rb + b
                                if not (r0_of(qrow) <= kr <= r0_of(qrow) + 7):
                                    k.memset(ptile[0][64 * a:64 * a + 64, jw * 128 + 64 * b:jw * 128 + 64 * b + 64], 0.0, [ptile[1]])

                def n_pv(ji):
                    rb, hh, q0, tiles, nw, di0 = blk(ji)
                    pSa, kSa, pSb, kSb, pO, kO, sb_, ptile = bufs(ji)
                    for jw, t in enumerate(tiles):
                        k.mm(pO[0:65, 0:128], Vn[:, NCT + t, hh, 0:65], ptile[0][:, jw * 128:(jw + 1) * 128], jw == 0, False, [kVn, ptile[1]], [kO])
                    for kt in range(NCT):
                        k.mm(pO[0:65, 0:128], Vn[:, kt, hh, 0:65], ptile[0][:, 640 + kt * 128:768 + kt * 128], False, kt == NCT - 1, [kVn, ptile[1]], [kO])

                def n_fin(ji):
                    rb, hh, q0, tiles, nw, di0 = blk(ji)
                    pSa, kSa, pSb, kSb, pO, kO, sb_, ptile = bufs(ji)
                    attn_finalize(pO, kO, 128, ynb[:, hh, rb * 128:(rb + 1) * 128], kyn, (rden, krd, osb, kosb), PS[6 + ji % 2], PK[6 + ji % 2])

                NJ = 64
                n_qk(0)
                for ji in range(NJ):
                    if ji + 1 < NJ:
                        n_qk(ji + 1)
                    n_soft(ji)
                    n_pv(ji)
                    if ji >= 1:
                        n_fin(ji - 1)
                n_fin(NJ - 1)
                for hh in range(2):
                    hd = 2 * hp + hh
                    k.dma(YT[2][hd * 64:(hd + 1) * 64, TC:S], ynb[:, hh, :], [kyn], ["YT2"])
                P.barrier(); A.release(m2)
            P.barrier(); A.release(m)

        def phase_merge(l):
            m = A.mark()
            wz = [A.alloc([8, 3, 128], BF16) for _ in range(2)]
            wb = [A.alloc([3, 4, 128], BF16) for _ in range(2)]
            yt = [A.alloc([3, 4, 512], BF16) for _ in range(2)]
            sg = [A.alloc([512], F32) for _ in range(2)]
            acc, kacc = A.alloc([512], F32); tmp, ktmp = A.alloc([512], F32)
            mo = [A.alloc([512], BF16) for _ in range(2)]
            it = 0
            def ld_w(ct):
                b = ct % 2
                for n in range(3):
                    load_w(wz[b][0][:, :, n, :], wz[b][1], w_in[l][:, C_ZG + n * D + ct * 128:C_ZG + n * D + (ct + 1) * 128])
                    load_w(wb[b][0][:, n, :, :], wb[b][1], w_branch[l, n][:, ct * 128:(ct + 1) * 128])
            def ld_y(itx):
                t0, tn = TOKCH[itx % len(TOKCH)]
                yb_ = yt[itx % 2]
                for n in range(3):
                    k.dma(yb_[0][:, n, :, 0:tn], YT[n][:, t0:t0 + tn].rearrange("(k p) c -> p k c", p=128), ["YT%d" % n], [yb_[1]])
            ld_w(0)
            ld_y(0)
            for ct in range(8):
                b = ct % 2
                if ct + 1 < 8:
                    ld_w(ct + 1)
                for ci, (t0, tn) in enumerate(TOKCH):
                    yb_ = yt[it % 2]; mo_ = mo[it % 2]; it += 1
                    if it < 8 * len(TOKCH):
                        ld_y(it)
                    for n in range(3):
                        pg, kg = PS[2 * (n % 2)], PK[2 * (n % 2)]
                        pp, kp = PS[2 * (n % 2) + 1], PK[2 * (n % 2) + 1]
                        for kk in range(8):
                            k.mm(pg[:, 0:tn], wz[b][0][:, kk, n, :], hT[:, kk, t0:t0 + tn], kk == 0, kk == 7, [wz[b][1], khT], [kg])
                        for kk in range(4):
                            k.mm(pp[:, 0:tn], wb[b][0][:, n, kk, :], yb_[0][:, n, kk, 0:tn], kk == 0, kk == 3, [wb[b][1], yb_[1]], [kp])
                        sg_ = sg[n % 2]
                        k.act(sg_[0][:, 0:tn], pg[:, 0:tn], AF.Sigmoid, [kg], [sg_[1]])
                        if n == 0:
                            k.tt(acc[:, 0:tn], sg_[0][:, 0:tn], pp[:, 0:tn], ALU.mult, [sg_[1], kp], [kacc])
                        else:
                            k.tt(tmp[:, 0:tn], sg_[0][:, 0:tn], pp[:, 0:tn], ALU.mult, [sg_[1], kp], [ktmp])
                            if n == 1:
                                k.tt(acc[:, 0:tn], acc[:, 0:tn], tmp[:, 0:tn], ALU.add, [kacc, ktmp], [kacc])
                            else:
                                k.tt(mo_[0][:, 0:tn], acc[:, 0:tn], tmp[:, 0:tn], ALU.add, [kacc, ktmp], [mo_[1]])
                    k.dma(MTm[ct * 128:(ct + 1) * 128, t0:t0 + tn], mo_[0][:, 0:tn], [mo_[1]], ["MTm"])
            P.barrier(); A.release(m)

        def phase_proj_res(MT, mtkey, nk, wsrc, which, tiles):
            m = A.mark()
            wd, kwd = A.alloc([nk, D], BF16)
            for kk0 in range(0, nk, 8):
                kn = min(8, nk - kk0)
                load_w(wd[:, kk0:kk0 + kn, :], kwd, wsrc[kk0 * 128:(kk0 + kn) * 128, :])
            mt = [A.alloc([nk, 128], BF16) for _ in range(2)]
            xt = [A.alloc([D], F32) for _ in range(2)]
            tt_ = [A.alloc([D], F32) for _ in range(2)]
            junk, kj = A.alloc([512], F32)
            ssv = [A.alloc([4], F32) for _ in range(2)]
            def ld_t(ii):
                i = tiles[ii]; b = ii % 2
                k.dma(mt[b][0], MT[:, i * 128:(i + 1) * 128].rearrange("(k p) c -> p k c", p=128), [mtkey], [mt[b][1]])
                k.dma(xt[b][0], X[i * 128:(i + 1) * 128, :], ["X.%d" % i], [xt[b][1]])
            ld_t(0)
            for ii, i in enumerate(tiles):
                b = ii % 2
                r = 1 if i < NCT else 0
                if ii + 1 < len(tiles):
                    ld_t(ii + 1)
                ph = [(PS[4 * b + hf], PK[4 * b + hf]) for hf in range(2)]
                for hf in range(2):
                    for kk in range(nk):
                        k.mm(ph[hf][0][:], mt[b][0][:, kk, :], wd[:, kk, hf * 512:(hf + 1) * 512], kk == 0, kk == nk - 1, [mt[b][1], kwd], [ph[hf][1]])
                sv, ksv = ssv[b]
                for hf in range(2):
                    k.act(junk, ph[hf][0][:], AF.Square, [ph[hf][1]], [kj, ksv], accum=sv[:, hf:hf + 1])
                k.tt(sv[:, 2:3], sv[:, 0:1], sv[:, 1:2], ALU.add, [ksv], [ksv])
                k.ts(sv[:, 2:3], sv[:, 2:3], 1.0 / D, ALU.mult, [ksv], [ksv], s2=EPS, op1=ALU.add)
                k.act(sv[:, 3:4], sv[:, 2:3], AF.Ln, [ksv], [ksv])
                k.act(sv[:, 3:4], sv[:, 3:4], AF.Exp, [ksv], [ksv], scale=-0.5)
                for hf in range(2):
                    cs = slice(hf * 512, (hf + 1) * 512)
                    k.stt(tt_[b][0][:, cs], ph[hf][0][:], sv[:, 3:4], Gbc[:, r, which, cs], ALU.mult, ALU.mult, [ph[hf][1], ksv, kG], [tt_[b][1]])
                k.tt(tt_[b][0], tt_[b][0], xt[b][0], ALU.add, [tt_[b][1], xt[b][1]], [tt_[b][1]], eng="pool")
                k.dma(X[i * 128:(i + 1) * 128, :], tt_[b][0], [tt_[b][1]], ["X.%d" % i, "X"])
            P.barrier(); A.release(m)

        def phase_ffn_up(l, lo_tok):
            m = A.mark()
            LB = S + 4
            cw, kcw = A.alloc([2 * NFT, 3], F32); cbb, kcb = A.alloc([2 * NFT], F32)
            k.dma(cw, cwfm[l].rearrange("p (a b) -> p a b", b=3), (), [kcw])
            k.dma(cbb, cbfm[l], (), [kcb])
            UU = [[A.alloc([LB], BF16) for _ in range(2)] for _ in range(2)]
            T = [A.alloc([LB], F32) for _ in range(2)]
            mb, kmb = A.alloc([LB], BF16)
            w = [A.alloc([8, 2, 128], BF16) for _ in range(2)]
            for bb_ in range(2):
                for z in range(2):
                    k.memset(UU[bb_][z][0], 0.0, [UU[bb_][z][1]])
            chunks = []
            if lo_tok == 0:
                chunks.append((0, TC, 1))
            for qc in range(TL // 512):
                chunks.append((TC + qc * 512, 512, 3 + TC + qc * 512))
            lo_c = 1 if lo_tok == 0 else 3 + TC
            hi_c = LB - 1
            it = 0

            def conv_ops(j):
                U = UU[j % 2]
                ops = []
                for z in range(2):
                    ch = z * NFT + j
                    ops.append(lambda z=z, ch=ch: k.act(T[z][0][:, lo_c:hi_c], U[z][0][:, lo_c:hi_c], AF.Identity, [U[z][1], kcw, kcb], [T[z][1]],
                                                        scale=cw[:, ch, 1:2], bias=cbb[:, ch:ch + 1]))
                    ops.append(lambda z=z, ch=ch: k.stt(T[z][0][:, lo_c:hi_c], U[z][0][:, lo_c - 1:hi_c - 1], cw[:, ch, 0:1], T[z][0][:, lo_c:hi_c],
                                                        ALU.mult, ALU.add, [U[z][1], kcw, T[z][1]], [T[z][1]]))
                    ops.append(lambda z=z, ch=ch: k.stt(T[z][0][:, lo_c:hi_c], U[z][0][:, lo_c + 1:hi_c + 1], cw[:, ch, 2:3], T[z][0][:, lo_c:hi_c],
                                                        ALU.mult, ALU.add, [U[z][1], kcw, T[z][1]], [T[z][1]]))
                ops.append(lambda: k.act(T[1][0][:, lo_c:hi_c], T[1][0][:, lo_c:hi_c], AF.Silu, [T[1][1]], [T[1][1]]))
                ops.append(lambda: k.tt(mb[:, lo_c:hi_c], T[0][0][:, lo_c:hi_c], T[1][0][:, lo_c:hi_c], ALU.mult, [T[0][1], T[1][1]], [kmb]))
                if lo_tok == 0:
                    ops.append(lambda: k.dma(MTf[j * 128:(j + 1) * 128, 0:TC], mb[:, 1:1 + TC], [kmb], ["MTf"]))
                ops.append(lambda: k.dma(MTf[j * 128:(j + 1) * 128, TC:S], mb[:, 3 + TC:3 + S], [kmb], ["MTf"]))
                return ops

            pend = []
            for j in range(NFT):
                b = j % 2
                U = UU[b]
                load_w(w[b][0][:, :, 0, :], w[b][1], w_up[l][:, j * 128:(j + 1) * 128])
                load_w(w[b][0][:, :, 1, :], w[b][1], w_up[l][:, FFN + j * 128:FFN + (j + 1) * 128])
                gi = 0
                for (t0, tn, c0) in chunks:
                    for z in range(2):
                        ps, pk = PS[it % 4], PK[it % 4]; it += 1
                        for kk in range(8):
                            k.mm(ps[:, 0:tn], w[b][0][:, kk, z, :], hT[:, kk, t0:t0 + tn], kk == 0, kk == 7, [w[b][1], khT], [pk])
                        k.act(U[z][0][:, c0:c0 + tn], ps[:, 0:tn], AF.Identity, [pk], [U[z][1]])
                        gi += 1
                        if pend and gi % 2 == 0:
                            pend.pop(0)()
                while pend:
                    pend.pop(0)()
                pend = conv_ops(j)
            while pend:
                pend.pop(0)()
            P.barrier(); A.release(m)

        for l in range(n_layers):
            last = (l == DEPTH - 1) or force_last
            P.new_epoch()
            m_mod = None
            if on("mod"):
                P.label = "mod%d" % l; m_mod = phase_mod(l)
            if on("norm"):
                P.label = "norm%d" % l; phase_norm(0, mod_ps=True)
            if m_mod is not None:
                P.barrier(); A.release(m_mod)
            if on("mlstm"):
                P.label = "mlstm%d" % l; phase_mlstm(l)
            if on("gqa"):
                P.label = "gqa%d" % l; phase_gqa(l, last)
            if on("na"):
                P.label = "na%d" % l; phase_na(l, last)
            if on("merge"):
                P.label = "merge%d" % l; phase_merge(l)
            tiles = list(range(NCT if last else 0, NT))
            if on("res1"):
                P.label = "res1_%d" % l; phase_proj_res(MTm, "MTm", 8, w_out[l], 0, tiles)
            if on("norm2"):
                P.label = "norm2%d" % l; phase_norm(1)
            if on("ffn"):
                P.label = "ffn%d" % l; phase_ffn_up(l, TC if last else 0)
            if on("res2"):
                P.label = "res2_%d" % l; phase_proj_res(MTf, "MTf", NFT, w_down[l], 1, tiles)
        P.barrier()
        for q in range(8):
            k.dma(yout[q * 512:(q + 1) * 512, :], X[TC + q * 512:TC + (q + 1) * 512, :], ["X"], ["yout%d" % q])
        P.emit()
        build.last_prog = P
        print("ops emitted:", P.nops, {e: len(v) for e, v in P.ops.items()})
    return nc


def _consts():
    ident = np.eye(128, dtype=np.float32)
    ones = np.ones((128, 128), np.float32)
    s = np.arange(128)[:, None]; t = np.arange(128)[None, :]
    mf = (s <= t).astype(np.float32); mb = (s >= t).astype(np.float32)
    cst = np.concatenate([ident, ones, mf, mb, np.zeros((128, 128), np.float32)], axis=1)
    esel = np.zeros((4, 4, 128), np.float32)
    for h in range(4):
        esel[h, h, :] = 1.0
    half = 32
    tpos = np.arange(TL)
    inv = (10000.0 ** (-np.arange(0, half, 2, dtype=np.float32) / half)).astype(np.float32)
    ang_r = (tpos // GRID).astype(np.float32)[:, None] * inv
    ang_c = (tpos % GRID).astype(np.float32)[:, None] * inv
    ang = np.concatenate([ang_r, ang_r, ang_c, ang_c], axis=-1)
    cos = np.cos(ang).astype(np.float32); sin = np.sin(ang).astype(np.float32)
    sgn = np.concatenate([-np.ones(16), np.ones(16), -np.ones(16), np.ones(16)]).astype(np.float32)
    sins = sin * sgn[None, :]
    ropec = cos.reshape(32, 128, 64).transpose(1, 0, 2).reshape(128, 32 * 64)
    ropes = sins.reshape(32, 128, 64).transpose(1, 0, 2).reshape(128, 32 * 64)
    return cst, esel.reshape(4, 512), np.ascontiguousarray(ropec), np.ascontiguousarray(ropes)


def _rpb_tiles(na_rpb):
    L = na_rpb.shape[0]
    cols = np.arange(GRID)
    c0 = np.clip(cols - 8, 0, GRID - 16)
    kc = np.arange(GRID)[:, None]; qc = np.arange(GRID)[None, :]
    inwin = (kc >= c0[None, :]) & (kc < c0[None, :] + 16)
    dc = np.clip(kc - qc + 15, 0, 30)
    out = np.full((L, 4, 128, 2, 7, 128), NEG, np.float32)
    for di in range(7):
        delta = 2 * di - 6
        for a in range(2):
            for b in range(2):
                dr = delta + a - b + 7
                if not (0 <= dr <= 14):
                    continue
                vals = na_rpb[:, :, dr, :][:, :, dc]
                vals = np.where(inwin[None, None], vals, np.float32(NEG))
                v = vals.reshape(L, 4, 2, GRID, GRID).transpose(0, 1, 3, 2, 4)
                out[:, :, 64 * a:64 * a + 64, :, di, 64 * b:64 * b + 64] = v
    return out.reshape(L, 4, 128, 2 * 7 * 128)


_NC_CACHE = {}


def prep_inputs(inp):
    f = lambda a: np.ascontiguousarray(np.asarray(a, dtype=np.float32))
    cst, esel, ropec, ropes = _consts()
    L = DEPTH
    gfm = np.stack([f(inp["g_pre_mix"]).reshape(L, 8, 128), f(inp["g_pre_ffn"]).reshape(L, 8, 128)], axis=1)
    gfm = np.ascontiguousarray(gfm.transpose(0, 3, 1, 2))
    gpost = np.ascontiguousarray(np.stack([f(inp["g_post_mix"]), f(inp["g_post_ffn"])], axis=1))
    bgate = np.ascontiguousarray(f(inp["b_ml_gates"]).reshape(L, 4, 4).transpose(0, 2, 1))
    gq = f(inp["g_q"]); gk = f(inp["g_k"])
    gqk = np.ascontiguousarray(np.concatenate([gq, gq, gq, gq, gk, gk], axis=1))
    cw = f(inp["conv_w"])
    cwfm = np.ascontiguousarray(cw.reshape(L, 3, 2 * NFT, 128).transpose(0, 3, 2, 1).reshape(L, 128, 2 * NFT * 3))
    cbfm = np.ascontiguousarray(f(inp["conv_b"]).reshape(L, 2 * NFT, 128).transpose(0, 2, 1))
    shared = {
        "w_mod": f(inp["w_mod"]), "b_mod": f(inp["b_mod"]), "gfm": gfm, "gpost": gpost, "w_in": f(inp["w_in"]),
        "bgate": bgate, "gml": f(inp["g_ml_out"]), "gqk": gqk, "rpbT": _rpb_tiles(f(inp["na_rpb"])),
        "w_branch": f(inp["w_branch"]), "w_out": f(inp["w_out"]), "w_up": f(inp["w_up"]), "w_down": f(inp["w_down"]),
        "cwfm": cwfm, "cbfm": cbfm, "ropec": ropec, "ropes": ropes, "cst": cst, "esel": esel,
    }
    x = f(inp["x"]); c = f(inp["c"]); ctx = f(inp["ctx"]); cc = f(inp["c_ctx"])
    maps = []
    for b in range(x.shape[0]):
        cv = np.stack([c[b], cc], axis=0)
        cT = np.ascontiguousarray(cv.reshape(2, 8, 128).transpose(2, 0, 1))
        mp = dict(shared)
        mp.update({"x_in": x[b], "ctx_in": ctx[b], "cT": cT})
        maps.append(mp)
    return maps


def kernel(**inputs):
    maps = prep_inputs(inputs)
    if "nc" not in _NC_CACHE:
        _NC_CACHE["nc"] = build()
    nc = _NC_CACHE["nc"]
    res = run_bass_kernel_spmd(nc, maps, core_ids=list(range(len(maps))))
    return np.stack([np.asarray(r["y"], dtype=np.float32) for r in res.results], axis=0)
```
